# Optimizing a Trainium2 kernel written in Bass

```python
import math
import jax, jax.numpy as jnp
from jax import lax
import numpy as np

D_MODEL = 1024
BATCH = 8
SEQ = 2048
DEPTH = 2

HEAD_DIM = 64
Q_BLOCK = 128
ROPE_THETA = 10000.0
LN_EPS = 1e-5
RMS_EPS = 1e-5
DEEPNORM_ALPHA = (2 * DEPTH) ** 0.25
DEEPNORM_BETA = (8 * DEPTH) ** -0.25

CONV_CH = D_MODEL // 2
CONV_WIDTH = 31
DIFF_VDIM = 2 * HEAD_DIM
DIFF_HEADS = (D_MODEL // 2) // DIFF_VDIM
EVEN_IN = 2 * CONV_CH + DIFF_HEADS * (4 * HEAD_DIM + DIFF_VDIM)
EVEN_MIX = CONV_CH + DIFF_HEADS * DIFF_VDIM

SB_HEADS = (D_MODEL // 2) // HEAD_DIM
SB_WIDTH = SB_HEADS * HEAD_DIM
POOL_WINDOWS = (2, 4, 8, 16)
POOL_GROUPS = len(POOL_WINDOWS)
POOL_CH = (D_MODEL // 2) // POOL_GROUPS
ODD_IN = 3 * SB_WIDTH + POOL_GROUPS * POOL_CH
ODD_MIX = SB_WIDTH + POOL_GROUPS * POOL_CH

N_EXPERTS = 16
N_GROUPS = 4
EXPERTS_PER_GROUP = N_EXPERTS // N_GROUPS
TOP_K = 2
D_FF_EXPERT = 512

kernel_name = 'hybrid_conv_diffattn_stickbreak_pool_moe'


def _layer_norm(x, g, b):
    xf = x.astype(jnp.float32)
    mu = xf.mean(-1, keepdims=True)
    var = jnp.square(xf - mu).mean(-1, keepdims=True)
    y = (xf - mu) * lax.rsqrt(var + LN_EPS)
    return (y * g.astype(jnp.float32) + b.astype(jnp.float32)).astype(x.dtype)


def _rope_tables(n_pos):
    inv = ROPE_THETA ** (-jnp.arange(0, HEAD_DIM, 2, dtype=jnp.float32) / HEAD_DIM)
    ang = jnp.arange(n_pos, dtype=jnp.float32)[:, None] * inv[None, :]
    ang = jnp.concatenate([ang, ang], axis=-1)
    return jnp.cos(ang), jnp.sin(ang)


def _rope(u, cos, sin):
    u1, u2 = jnp.split(u, 2, axis=-1)
    return u * cos + jnp.concatenate([-u2, u1], axis=-1) * sin


def _sweep_query_blocks(block_fn, n_pos):
    return jnp.concatenate(
        [block_fn(i * Q_BLOCK, (i + 1) * Q_BLOCK) for i in range(n_pos // Q_BLOCK)], axis=2)


def _differential_attention(q1, q2, k1, k2, v, lam, lam_init, subln_g):
    scale = HEAD_DIM ** -0.5

    def block(start, end):
        mask = jnp.arange(end)[None, :] <= (start + jnp.arange(Q_BLOCK))[:, None]

        def probs(q, k):
            s = jnp.einsum('bhqd,bhkd->bhqk', q[:, :, start:end], k[:, :, :end]) * scale
            return jax.nn.softmax(jnp.where(mask, s, -jnp.inf), axis=-1)

        w = probs(q1, k1) - lam * probs(q2, k2)
        return jnp.einsum('bhqk,bhkd->bhqd', w, v[:, :, :end])

    o = _sweep_query_blocks(block, q1.shape[2])
    o = o * lax.rsqrt(jnp.mean(jnp.square(o), -1, keepdims=True) + RMS_EPS)
    return o * subln_g.astype(jnp.float32) * (1.0 - lam_init)


def _stick_breaking_attention(q, k, v):
    scale = HEAD_DIM ** -0.5

    def block(start, end):
        mask = jnp.arange(end)[None, :] < (start + jnp.arange(Q_BLOCK))[:, None]
        z = jnp.einsum('bhqd,bhkd->bhqk', q[:, :, start:end], k[:, :, :end]) * scale
        log_stay = jnp.where(mask, jax.nn.log_sigmoid(-z), 0.0)
        suffix = lax.cumsum(log_stay, axis=3, reverse=True) - log_stay
        a = jnp.where(mask, jnp.exp(jax.nn.log_sigmoid(z) + suffix), 0.0)
        return jnp.einsum('bhqk,bhkd->bhqd', a, v[:, :, :end])

    return _sweep_query_blocks(block, q.shape[2])


def _causal_depthwise_conv(u, w, b):
    kern = w[:, None, :].astype(u.dtype)
    y = lax.conv_general_dilated(
        u, kern, window_strides=(1,), padding=[(CONV_WIDTH - 1, 0)],
        dimension_numbers=('NWC', 'WIO', 'NWC'), feature_group_count=u.shape[-1])
    return y + b.astype(u.dtype)


def _multiscale_pool(u, pool_w, pool_scale):
    bsz, n_pos, _ = u.shape
    ug = u.astype(jnp.float32).reshape(bsz, n_pos, POOL_GROUPS, POOL_CH)
    pos = jnp.arange(n_pos)
    outs = []
    for g, win in enumerate(POOL_WINDOWS):
        xg = ug[:, :, g]
        c_pad = jnp.pad(jnp.cumsum(xg, axis=1), ((0, 0), (win, 0), (0, 0)))
        window_sum = c_pad[:, win:] - c_pad[:, :n_pos]
        count = jnp.minimum(pos + 1, win).astype(jnp.float32)
        outs.append(window_sum / count[None, :, None] - xg)
    pooled = jnp.stack(outs, axis=2)
    mixed = jnp.einsum('bsgc,gcd->bsgd', pooled, pool_w.astype(jnp.float32))
    out = mixed.reshape(bsz, n_pos, POOL_GROUPS * POOL_CH) * pool_scale.astype(jnp.float32)
    return out.astype(u.dtype)


def _lambda_init(layer_idx):
    return 0.8 - 0.6 * math.exp(-0.3 * layer_idx)


def _even_mixer(x, in_proj, conv_w, conv_b, conv_ln_g, conv_ln_b,
                lambda_q1, lambda_k1, lambda_q2, lambda_k2, subln_g, out_proj,
                lam_init, cos, sin):
    bsz, n_pos, _ = x.shape
    p = x @ in_proj
    qk_w = DIFF_HEADS * 2 * HEAD_DIM
    a_val, a_gate, dq, dk, dv = jnp.split(
        p, [CONV_CH, 2 * CONV_CH, 2 * CONV_CH + qk_w, 2 * CONV_CH + 2 * qk_w], axis=-1)
    a = a_val * jax.nn.sigmoid(a_gate)
    a = _causal_depthwise_conv(a, conv_w, conv_b)
    a = jax.nn.silu(_layer_norm(a, conv_ln_g, conv_ln_b))
    def split_qk(t):
        return t.reshape(bsz, n_pos, DIFF_HEADS, 2, HEAD_DIM).transpose(3, 0, 2, 1, 4).astype(jnp.float32)
    q = split_qk(dq)
    k = split_qk(dk)
    v = dv.reshape(bsz, n_pos, DIFF_HEADS, DIFF_VDIM).transpose(0, 2, 1, 3).astype(jnp.float32)
    q1, q2 = _rope(q[0], cos, sin), _rope(q[1], cos, sin)
    k1, k2 = _rope(k[0], cos, sin), _rope(k[1], cos, sin)
    f32 = jnp.float32
    lam = (jnp.exp(jnp.sum(lambda_q1.astype(f32) * lambda_k1.astype(f32)))
           - jnp.exp(jnp.sum(lambda_q2.astype(f32) * lambda_k2.astype(f32))) + lam_init)
    o = _differential_attention(q1, q2, k1, k2, v, lam, lam_init, subln_g)
    o = o.transpose(0, 2, 1, 3).reshape(bsz, n_pos, DIFF_HEADS * DIFF_VDIM).astype(x.dtype)
    return jnp.concatenate([a, o], axis=-1) @ out_proj


def _odd_mixer(x, in_proj, pool_w, pool_scale, out_proj):
    bsz, n_pos, _ = x.shape
    p = x @ in_proj
    q, k, v, u = jnp.split(p, [SB_WIDTH, 2 * SB_WIDTH, 3 * SB_WIDTH], axis=-1)

    def heads(t):
        return t.reshape(bsz, n_pos, SB_HEADS, HEAD_DIM).transpose(0, 2, 1, 3).astype(jnp.float32)

    o = _stick_breaking_attention(heads(q), heads(k), heads(v))
    o = o.transpose(0, 2, 1, 3).reshape(bsz, n_pos, SB_WIDTH).astype(x.dtype)
    d = _multiscale_pool(u, pool_w, pool_scale)
    return jnp.concatenate([o, d], axis=-1) @ out_proj


def _grouped_moe(x, router_w, router_bias, w_gate, w_up, w_down):
    bsz, n_pos, dm = x.shape
    t = x.reshape(-1, dm)
    n_tok = t.shape[0]
    affinity = jax.nn.sigmoid(t.astype(jnp.float32) @ router_w.astype(jnp.float32))
    sel = affinity + router_bias.astype(jnp.float32)
    sel_g = sel.reshape(n_tok, N_GROUPS, EXPERTS_PER_GROUP)
    group_score = lax.top_k(sel_g, TOP_K)[0].sum(-1)
    g_best = jnp.argmax(group_score, axis=-1)
    idx = jnp.broadcast_to(g_best[:, None, None], (n_tok, 1, EXPERTS_PER_GROUP))
    in_group = jnp.take_along_axis(sel_g, idx, axis=1)[:, 0]
    _, local = lax.top_k(in_group, TOP_K)
    expert_idx = g_best[:, None] * EXPERTS_PER_GROUP + local
    gate = jnp.take_along_axis(affinity, expert_idx, axis=1)
    gate = gate / gate.sum(-1, keepdims=True)
    combine = (jax.nn.one_hot(expert_idx, N_EXPERTS, dtype=jnp.float32) * gate[..., None]).sum(1)
    h = jax.nn.silu(jnp.einsum('td,edf->tef', t, w_gate)) * jnp.einsum('td,edf->tef', t, w_up)
    h = h * combine[:, :, None].astype(h.dtype)
    y = jnp.einsum('tef,efd->td', h, w_down)
    return y.reshape(bsz, n_pos, dm)


def _normal(key, shape, scale):
    return jax.random.normal(key, shape, jnp.float32) * scale


def setup_inputs(seed: int = 0) -> dict:
    key = jax.random.key(seed)
    ks = iter(jax.random.split(key, 48))
    d = D_MODEL

    def gain(n):
        return 1.0 + _normal(next(ks), (n,), 0.02)

    def bias(n):
        return _normal(next(ks), (n,), 0.01)

    inp = {}
    inp['x'] = _normal(next(ks), (BATCH, SEQ, d), 1.0)
    inp['router_w'] = _normal(next(ks), (d, N_EXPERTS), d ** -0.5)
    inp['router_bias'] = _normal(next(ks), (N_EXPERTS,), 0.01)
    inp['l0_in_proj'] = _normal(next(ks), (d, EVEN_IN), d ** -0.5)
    inp['l0_conv_w'] = _normal(next(ks), (CONV_WIDTH, CONV_CH), CONV_WIDTH ** -0.5)
    inp['l0_conv_b'] = bias(CONV_CH)
    inp['l0_conv_ln_g'] = gain(CONV_CH)
    inp['l0_conv_ln_b'] = bias(CONV_CH)
    inp['l0_lambda_q1'] = _normal(next(ks), (HEAD_DIM,), 0.1)
    inp['l0_lambda_k1'] = _normal(next(ks), (HEAD_DIM,), 0.1)
    inp['l0_lambda_q2'] = _normal(next(ks), (HEAD_DIM,), 0.1)
    inp['l0_lambda_k2'] = _normal(next(ks), (HEAD_DIM,), 0.1)
    inp['l0_subln_g'] = gain(DIFF_VDIM)
    inp['l0_out_proj'] = _normal(next(ks), (EVEN_MIX, d), EVEN_MIX ** -0.5 * DEEPNORM_BETA)
    inp['l0_ln_mix_g'] = gain(d)
    inp['l0_ln_mix_b'] = bias(d)
    inp['l0_w_gate'] = _normal(next(ks), (N_EXPERTS, d, D_FF_EXPERT), d ** -0.5)
    inp['l0_w_up'] = _normal(next(ks), (N_EXPERTS, d, D_FF_EXPERT), d ** -0.5)
    inp['l0_w_down'] = _normal(next(ks), (N_EXPERTS, D_FF_EXPERT, d), D_FF_EXPERT ** -0.5 * DEEPNORM_BETA)
    inp['l0_ln_ffn_g'] = gain(d)
    inp['l0_ln_ffn_b'] = bias(d)
    inp['l1_in_proj'] = _normal(next(ks), (d, ODD_IN), d ** -0.5)
    inp['l1_pool_w'] = _normal(next(ks), (POOL_GROUPS, POOL_CH, POOL_CH), POOL_CH ** -0.5)
    inp['l1_pool_scale'] = 1.0 + _normal(next(ks), (POOL_GROUPS * POOL_CH,), 0.1)
    inp['l1_out_proj'] = _normal(next(ks), (ODD_MIX, d), ODD_MIX ** -0.5 * DEEPNORM_BETA)
    inp['l1_ln_mix_g'] = gain(d)
    inp['l1_ln_mix_b'] = bias(d)
    inp['l1_w_gate'] = _normal(next(ks), (N_EXPERTS, d, D_FF_EXPERT), d ** -0.5)
    inp['l1_w_up'] = _normal(next(ks), (N_EXPERTS, d, D_FF_EXPERT), d ** -0.5)
    inp['l1_w_down'] = _normal(next(ks), (N_EXPERTS, D_FF_EXPERT, d), D_FF_EXPERT ** -0.5 * DEEPNORM_BETA)
    inp['l1_ln_ffn_g'] = gain(d)
    inp['l1_ln_ffn_b'] = bias(d)
    return inp


def reference(x, router_w, router_bias,
              l0_in_proj, l0_conv_w, l0_conv_b, l0_conv_ln_g, l0_conv_ln_b,
              l0_lambda_q1, l0_lambda_k1, l0_lambda_q2, l0_lambda_k2, l0_subln_g,
              l0_out_proj, l0_ln_mix_g, l0_ln_mix_b, l0_w_gate, l0_w_up, l0_w_down,
              l0_ln_ffn_g, l0_ln_ffn_b,
              l1_in_proj, l1_pool_w, l1_pool_scale, l1_out_proj, l1_ln_mix_g, l1_ln_mix_b,
              l1_w_gate, l1_w_up, l1_w_down, l1_ln_ffn_g, l1_ln_ffn_b):
    cos, sin = _rope_tables(x.shape[1])
    mixer_params = [
        (l0_in_proj, l0_conv_w, l0_conv_b, l0_conv_ln_g, l0_conv_ln_b,
         l0_lambda_q1, l0_lambda_k1, l0_lambda_q2, l0_lambda_k2, l0_subln_g, l0_out_proj),
        (l1_in_proj, l1_pool_w, l1_pool_scale, l1_out_proj),
    ]
    mix_norms = [(l0_ln_mix_g, l0_ln_mix_b), (l1_ln_mix_g, l1_ln_mix_b)]
    experts = [(l0_w_gate, l0_w_up, l0_w_down), (l1_w_gate, l1_w_up, l1_w_down)]
    ffn_norms = [(l0_ln_ffn_g, l0_ln_ffn_b), (l1_ln_ffn_g, l1_ln_ffn_b)]
    for i in range(DEPTH):
        if i % 2 == 0:
            mix = _even_mixer(x, *mixer_params[i], lam_init=_lambda_init(i), cos=cos, sin=sin)
        else:
            mix = _odd_mixer(x, *mixer_params[i])
        x = _layer_norm(DEEPNORM_ALPHA * x + mix, *mix_norms[i])
        ffn = _grouped_moe(x, router_w, router_bias, *experts[i])
        x = _layer_norm(DEEPNORM_ALPHA * x + ffn, *ffn_norms[i])
    return x
```

```python
import contextlib
import math
import numpy as np
import concourse.bass as bass
import concourse.mybir as mybir
from concourse.alu_op_type import AluOpType as ALU
from concourse.bass_utils import run_bass_kernel_spmd

F32 = mybir.dt.float32
BF16 = mybir.dt.bfloat16
AF = mybir.ActivationFunctionType
AX = mybir.AxisListType

S = 2048
D = 1024
NT = 16
NE = 16
ALPHA = 4.0 ** 0.25
LN_EPS = 1e-5
RMS_EPS = 1e-5
LAM_INIT0 = 0.8 - 0.6 * math.exp(0.0)
POOL_WINDOWS = (2, 4, 8, 16)
NFILL = 3


import os as _os
TRACE = bool(_os.environ.get("KTRACE"))


class Sem:
    def __init__(self, h):
        self.h = h
        self.cnt = 0


class Buf:
    __slots__ = ("last_w", "readers", "name", "excl")

    def __init__(self, name="", excl=False):
        self.last_w = None
        self.readers = []
        self.name = name
        self.excl = excl


class Eng:
    def __init__(self, name, obj, sem, same_wait=True):
        self.name = name
        self.obj = obj
        self.sem = sem
        self.waited = {}
        self.same_wait = same_wait


class FW:
    def __init__(self, nc, stack, n_dma_sems=32):
        self.nc = nc
        def mk(n):
            s_ = Sem(stack.enter_context(nc.semaphore(n)))
            s_.name = n
            return s_
        self.pe = Eng("pe", nc.tensor, mk("s_pe"), same_wait=False)
        self.act = Eng("act", nc.scalar, mk("s_act"))
        self.dve = Eng("dve", nc.vector, mk("s_dve"))
        self.pool = Eng("pool", nc.gpsimd, mk("s_pool"))
        self.sp = Eng("sp", nc.sync, mk("s_sp"))
        self.engs = [self.pe, self.act, self.dve, self.pool, self.sp]
        self.dma_sems = [mk(f"s_dma{i}") for i in range(n_dma_sems)]
        self.dma_i = 0
        self.dma_qi = [0, 0]
        self.n_instr = 0
        self.pe_pending = False

    def _wait(self, eng, tok):
        sem, val = tok
        if sem is eng.sem and not eng.same_wait:
            return
        if eng.waited.get(id(sem), 0) >= val:
            return
        eng.obj.wait_ge(sem.h, val)
        eng.waited[id(sem)] = val
        if TRACE:
            print("   wait", eng.name, "on", getattr(sem, "name", "?"), val)

    def _deps(self, eng, reads, writes):
        for b in reads:
            if b.last_w is not None:
                self._wait(eng, b.last_w)
            if b.excl:
                for t in b.readers:
                    if t[0] is not eng.sem:
                        self._wait(eng, t)
        for b in writes:
            if b.last_w is not None:
                self._wait(eng, b.last_w)
            for t in b.readers:
                self._wait(eng, t)

    @staticmethod
    def _commit(tok, reads, writes):
        for b in reads:
            if len(b.readers) > 24:
                best = {}
                for s_, v_ in b.readers:
                    if best.get(id(s_), (None, -1))[1] < v_:
                        best[id(s_)] = (s_, v_)
                b.readers = list(best.values())
            b.readers.append(tok)
        for b in writes:
            b.last_w = tok
            b.readers = []

    def op(self, eng, fn, reads=(), writes=(), inc=True):
        if TRACE:
            print("op", eng.name, "cnt", eng.sem.cnt, "reads", [b.name for b in reads], "writes", [b.name for b in writes], "inc", inc)
        self._deps(eng, reads, writes)
        ins = fn()
        self.n_instr += 1
        if inc:
            eng.sem.cnt += 1
            ins.then_inc(eng.sem.h, 1)
            tok = (eng.sem, eng.sem.cnt)
            if eng is self.pe:
                self.pe_pending = False
        else:
            assert eng is self.pe
            tok = (eng.sem, eng.sem.cnt + 1)
            self.pe_pending = True
        self._commit(tok, reads, writes)
        return ins

    def dma(self, q, out, in_, reads=(), writes=(), **kw):
        half = len(self.dma_sems) // 2
        k = 0 if q is self.sp else 1
        sem = self.dma_sems[k * half + self.dma_qi[k] % half]
        self.dma_qi[k] += 1
        self.dma_i += 1
        if sem.cnt:
            self._wait(q, (sem, sem.cnt))
        self._deps(q, reads, writes)
        ins = q.obj.dma_start(out=out, in_=in_, **kw)
        sem.cnt += 16
        ins.then_inc(sem.h, 16)
        self.n_instr += 1
        tok = (sem, sem.cnt)
        self._commit(tok, reads, writes)
        return tok

    def barrier(self):
        assert not self.pe_pending
        for e in self.engs:
            for o in self.engs:
                if o is not e and o.sem.cnt:
                    self._wait(e, (o.sem, o.sem.cnt))
            for s in self.dma_sems:
                if s.cnt:
                    self._wait(e, (s, s.cnt))


class Ring:
    def __init__(self, aps, name="r"):
        self.items = [(ap, Buf(f"{name}{i}")) for i, ap in enumerate(aps)]
        self.i = 0

    def next(self):
        it = self.items[self.i % len(self.items)]
        self.i += 1
        return it


class Prog:
    def __init__(self, stop=None, taps=()):
        self.stop = stop
        self.taps = set(taps)
        self.dbg_specs = {}

    def build(self):
        nc = bass.Bass("TRN2", target_bir_lowering=False)
        self.nc = nc
        di = lambda n, s: nc.dram_tensor(n, list(s), F32, kind="ExternalInput").ap()
        self.x_d = di("x", [S, D])
        self.router_w = di("router_w", [D, NE])
        self.win = [di("w_in0", [D, 2560]), di("w_in1", [D, 2048])]
        self.wout = [di("w_out0", [D, D]), di("w_out1", [D, D])]
        self.wg = [di("w_gate0", [NE, D, 512]), di("w_gate1", [NE, D, 512])]
        self.wu = [di("w_up0", [NE, D, 512]), di("w_up1", [NE, D, 512])]
        self.wd = [di("w_down0", [NE, 512, D]), di("w_down1", [NE, 512, D])]
        self.pool_w = di("pool_w", [4, 128, 128])
        self.pp_d = di("pp", [128, 144])
        self.rowp_d = di("rowp", [272])
        self.lnp_d = di("lnp", [8, D])
        self.cst_d = di("cst", [128, 128 * 5 + 64])
        self.rope_d = di("rope", [2, 128, S])
        self.out_d = nc.dram_tensor("out", [S, D], F32, kind="ExternalOutput").ap()
        self.spill_d = nc.dram_tensor("xspill", [S, D], F32, kind="Internal").ap()
        with contextlib.ExitStack() as st:
            self.st = st
            self.fw = FW(nc, st)
            self._alloc()
            self._run()
        return nc

    def _alloc(self):
        nc, st = self.nc, self.st
        sb = lambda n, s, d: st.enter_context(nc.sbuf_tensor("sb_" + n, s, d))
        self.XT = sb("xt", [128, 8 * S], BF16)[:].rearrange("p (c t) -> p c t", c=8)
        self.XTb = [Buf(f"xt{t}") for t in range(NT)]
        bigf = sb("big", [128, 16640], F32)[:]
        self.X = bigf[:, 0:16384].rearrange("p (t d) -> p t d", t=NT)
        self.Xb = [Buf(f"x{t}") for t in range(NT)]
        bigb = bigf.bitcast(BF16)
        self.QK = bigb[:, 0:16384].rearrange("p (c t) -> p c t", c=8)
        self.QKb = [[Buf(f"qk{c}_{tb}") for tb in range(4)] for c in range(8)]
        self.V = bigb[:, 16384:24576].rearrange("p (t f) -> p t f", t=NT)
        self.Vb = [Buf(f"v{t}") for t in range(NT)]
        self.AU = bigb[:, 24576:32896].rearrange("p (c t) -> p c t", c=4)
        self.AUb = [Buf(f"au{c}") for c in range(4)]
        wb = sb("w", [128, 24576], BF16)[:]
        self.Wblk = [wb[:, i * 4096:(i + 1) * 4096] for i in range(6)]
        self.Wb = [Buf(f"w{i}") for i in range(6)]
        scr = sb("scr", [128, 4096], F32)[:]
        self.SCR = [scr[:, 0:2048], scr[:, 2048:4096]]
        self.SCRb = [Buf("scrA"), Buf("scrB")]
        tf = sb("tmpf", [128, 8 * 512], F32)[:]
        self.tmpf = Ring([tf[:, i * 512:(i + 1) * 512] for i in range(8)], "tf")
        tb_ = sb("tmpb", [128, 10 * 512], BF16)[:]
        self.tmpb = Ring([tb_[:, i * 512:(i + 1) * 512] for i in range(6)], "tb")
        self.tmpb2 = Ring([tb_[:, i * 512:(i + 1) * 512] for i in range(6, 10)], "tb2")
        self.tf_full, self.tb_full = tf, tb_
        self.ht = sb("ht", [128, 2 * 2048], BF16)[:]
        self.htr = Ring([self.ht[:, i * 2048:(i + 1) * 2048].rearrange("p (c t) -> p c t", c=4) for i in range(2)], "ht")
        self.cstf = sb("cstf", [128, 128 * 5 + 64], F32)[:]
        self.cstb = sb("cstb", [128, 128 * 7], BF16)[:]
        self.Bc = Buf("const")
        self.pp = sb("pp", [128, 144], F32)[:]
        self.rowp = sb("rowp", [128, 272], F32)[:]
        self.small = sb("small", [128, 64], F32)[:]
        self.Bsmall = Buf("small")
        self.comb = sb("comb", [128, NT * NE], F32)[:]
        self.Bcomb = Buf("comb")
        self.zeros_b = sb("zeros", [128, 128], BF16)[:]
        self.rt_halves = [scr[0:16, 1024:2048], scr[0:16, 3072:4096]]
        self.lgt = scr[:, 1024:1024 + NT * NE]
        self.Brt = Buf("rt")
        stt = sb("stats", [128, 4 * 16], F32)[:]
        self.statr = Ring([stt[:, i * 16:(i + 1) * 16] for i in range(4)], "stat")
        self.wr = sb("wr", [128, 8 * NE], F32)[:].rearrange("p (c e) -> p c e", c=8)
        self.pwb = sb("pwb", [128, 4 * 128], BF16)[:].rearrange("p (g d) -> p g d", g=4)
        self.PS = []
        self.PSb = []
        self.PSpair = []
        for k in range(4):
            pair = st.enter_context(nc.psum_tensor(f"pp{k}", [128, 1024], F32))[:]
            self.PSpair.append(pair)
            for h in range(2):
                self.PS.append(pair[:, h * 512:(h + 1) * 512])
                self.PSb.append(Buf(f"ps{2 * k + h}", excl=True))

    def psring(self, idxs):
        r = Ring([self.PS[i] for i in idxs])
        r.items = [(self.PS[i], self.PSb[i]) for i in idxs]
        return r

    def mm(self, out, lhsT, rhs, start, stop, reads, writes, inc=None):
        nc = self.nc
        inc = stop if inc is None else inc
        self.fw.op(self.fw.pe, lambda: nc.tensor.matmul(out, lhsT, rhs, start=start, stop=stop), reads, writes, inc=inc)

    def tr(self, out, in_, ident, reads, writes, inc=True):
        nc = self.nc
        self.fw.op(self.fw.pe, lambda: nc.tensor.transpose(out, in_, ident), reads, writes, inc=inc)

    def act(self, out, in_, func, reads, writes, **kw):
        nc = self.nc
        self.fw.op(self.fw.act, lambda: nc.scalar.activation(out, in_, func, **kw), reads, writes)

    def dve(self, fn, reads, writes):
        self.fw.op(self.fw.dve, fn, reads, writes)

    def pool(self, fn, reads, writes):
        self.fw.op(self.fw.pool, fn, reads, writes)

    def tap(self, name, ap, bufs, dtype=F32):
        if name not in self.taps:
            return
        nc = self.nc
        shape = list(ap.shape)
        d = nc.dram_tensor("dbg_" + name, shape, dtype, kind="ExternalOutput").ap()
        self.dbg_specs[name] = (shape, dtype)
        self.fw.barrier()
        self.fw.dma(self.fw.sp, d, ap, reads=bufs)
        self.fw.barrier()

    def _run(self):
        self._consts()
        for L in range(2):
            if L == 0:
                self._xt_from_dram()
            else:
                self._xt_from_x(router=False, spill=True)
                self.fw.barrier()
            if self.stop == f"l{L}_xt":
                return self._finish()
            self._in_proj(L)
            if self.stop in (f"l{L}_inproj", "l0_glu", "l0_qk"):
                return self._finish()
            self._load_wout(L)
            if L == 0:
                if self.stop == "l0_conv":
                    self._conv_branch()
                    return self._finish()
                self._diff_attn(self._conv_pe())
                self._conv_ln()
            else:
                self._pool_mix()
                self._sb_attn()
            if self.stop == f"l{L}_mix":
                return self._finish()
            self.fw.barrier()
            W0 = self._mid(L)
            self._routing()
            if self.stop == f"l{L}_route":
                return self._finish()
            self._experts(L, W0)
            self._final_ln(L)
            if self.stop == f"l{L}_moe":
                return self._finish()
        self._finish()

    def _finish(self):
        self.fw.barrier()
        allb = self.XTb + self.Xb + self.Vb + self.AUb + [b for r in self.QKb for b in r] + [self.Bcomb, self.Brt, self.Bsmall]
        self.tap("XT", self.XT, allb, BF16)
        self.tap("QK", self.QK, allb, BF16)
        self.tap("V", self.V, allb, BF16)
        self.tap("AU", self.AU, allb, BF16)
        self.tap("X", self.X, allb, F32)
        self.tap("comb", self.comb, allb, F32)
        self.tap("small", self.small, allb, F32)
        self.fw.barrier()

    def _consts(self):
        nc, fw = self.nc, self.fw
        fw.dma(fw.sp, self.cstf, self.cst_d[:, :], writes=[self.Bc])
        fw.dma(fw.sp, self.pp, self.pp_d[:, :], writes=[self.Bc])
        fw.dma(fw.sp, self.rowp, self.rowp_d.partition_broadcast(128), writes=[self.Bc])
        fw.dma(fw.sp, self.wr, self.router_w.rearrange("(c p) e -> p c e", p=128), writes=[self.Bc])
        fw.dma(fw.pool, self.pwb, self.pool_w.rearrange("g c d -> c g d"), writes=[self.Bc])
        self.dve(lambda: nc.vector.tensor_copy(self.cstb[:, 0:640], self.cstf[:, 0:640]), [self.Bc], [self.Bc])
        self.dve(lambda: nc.vector.memset(self.cstb[:, 640:768], 1.0), [], [self.Bc])
        self.dve(lambda: nc.vector.tensor_scalar(self.cstb[:, 768:896], self.cstf[:, 128:256], -8.0, None, ALU.mult), [self.Bc], [self.Bc])
        self.ident_f = self.cstf[:, 0:128]
        self.ident_b = self.cstb[:, 0:128]
        self.tri_b = self.cstb[:, 128:256]
        self.mle_b = self.cstb[:, 256:384]
        self.mlt_b = self.cstb[:, 384:512]
        self.pm_b = self.cstb[:, 512:640]
        self.ones_b = self.cstb[:, 640:768]
        self.ntri_b = self.cstb[:, 768:896]
        self.invc = self.cstf[:, 640:704].rearrange("p (g t) -> p g t", g=4)
        self.cw = self.pp[:, 0:124].rearrange("p (c w) -> p c w", c=4)
        self.conv_b = self.pp[:, 124:128]
        self.cln_g = self.pp[:, 128:132]
        self.cln_b = self.pp[:, 132:136]
        self.subln = self.pp[:, 136:137]
        self.pscale = self.pp[:, 137:141]
        sm = self.small
        lam4 = self.rowp[:, 0:256].rearrange("p (a d) -> p a d", a=4)
        tmp, tmpb_ = self.tmpf.next()
        self.dve(lambda: nc.vector.tensor_tensor(tmp[:, 0:128].rearrange("p (a d) -> p a d", a=2), lam4[:, 0:2, :], lam4[:, 2:4, :], ALU.mult), [self.Bc], [tmpb_])
        self.dve(lambda: nc.vector.tensor_reduce(sm[:, 2:4], tmp[:, 0:128].rearrange("p (a d) -> p a d", a=2), AX.X, ALU.add), [tmpb_], [self.Bsmall])
        self.act(sm[:, 4:6], sm[:, 2:4], AF.Exp, [self.Bsmall], [self.Bsmall])
        self.dve(lambda: nc.vector.tensor_tensor(sm[:, 6:7], sm[:, 5:6], sm[:, 4:5], ALU.subtract), [self.Bsmall], [self.Bsmall])
        self.dve(lambda: nc.vector.tensor_scalar(sm[:, 0:1], sm[:, 6:7], -LAM_INIT0, None, ALU.add), [self.Bsmall], [self.Bsmall])
        self.dve(lambda: nc.vector.tensor_scalar(sm[:, 1:2], self.subln, 1.0 - LAM_INIT0, None, ALU.mult), [self.Bc], [self.Bsmall])
        self.neglam = sm[:, 0:1]
        self.gsc = sm[:, 1:2]
        self.rbias = self.rowp[:, 256:272]

    def _xt_from_dram(self):
        nc, fw = self.nc, self.fw
        psr = self.psring([0, 1, 2, 3])
        k = 0
        for tt in range(NT):
            half = tt % 2
            for hh in range(2):
                pass
            xt_ap = self.SCR[half][:, 0:1024]
            fw.dma(fw.sp, xt_ap, self.x_d[tt * 128:(tt + 1) * 128, :], writes=[self.SCRb[half]])
            for dg in range(2):
                ps, psb = psr.next()
                for j in range(4):
                    dc = dg * 4 + j
                    self.tr(ps[:, j * 128:(j + 1) * 128], xt_ap[:, dc * 128:(dc + 1) * 128], self.ident_f,
                            [self.SCRb[half], self.Bc], [psb], inc=(j == 3))
                dst = self.XT[:, dg * 4:(dg + 1) * 4, tt * 128:(tt + 1) * 128]
                src = ps.rearrange("p (c t) -> p c t", c=4)
                if k % 2 == 0:
                    self.act(dst, src, AF.Copy, [psb], [self.XTb[tt]])
                else:
                    self.dve(lambda: nc.vector.tensor_copy(dst, src), [psb], [self.XTb[tt]])
                k += 1

    def _xt_from_x(self, router, spill, inv=1.0):
        nc, fw = self.nc, self.fw
        psr = self.psring([0, 1])
        lg_ps, lg_b = self.PS[2], self.PSb[2]
        if spill:
            for tt in range(NT):
                fw.dma(fw.sp, self.spill_d[tt * 128:(tt + 1) * 128, :], self.X[:, tt, :], reads=[self.Xb[tt]])
        for tb in range(4):
            for dc in range(8):
                ps, psb = psr.next()
                for j in range(4):
                    tt = tb * 4 + j
                    self.tr(ps[:, j * 128:(j + 1) * 128], self.X[:, tt, dc * 128:(dc + 1) * 128], self.ident_f,
                            [self.Xb[tt], self.Bc], [psb], inc=(j == 3))
                dst = self.XT[:, dc, tb * 512:(tb + 1) * 512]
                self.act(dst, ps, AF.Copy, [psb], self.XTb[tb * 4:tb * 4 + 4], scale=inv)
                if router:
                    xf, xfb = self.tmpf.next()
                    self.act(xf, ps, AF.Copy, [psb], [xfb], scale=inv)
                    self.mm(lg_ps[0:16, :], self.wr[:, dc, :], xf, dc == 0, dc == 7, [self.Bc, xfb], [lg_b])
            if router:
                self.dve(lambda: nc.vector.tensor_copy(self.rt_halves[tb // 2][:, (tb % 2) * 512:(tb % 2 + 1) * 512], lg_ps[0:16, :]), [lg_b], [self.Brt])

    @staticmethod
    def _win_slot(L):
        return {0: 0, 1: 1, 2: 2, 3: 3, 4: 4} if L == 0 else {3: 0, 0: 1, 1: 2, 2: 3}

    def _load_w_in(self, L, g, slot):
        fw = self.fw
        src = self.win[L].rearrange("(c p) n -> p c n", p=128)[:, :, g * 512:(g + 1) * 512]
        dst = self.Wblk[slot].rearrange("p (c n) -> p c n", c=8)
        fw.dma(fw.pool, dst, src, writes=[self.Wb[slot]])
        return dst

    def _proj_fm(self, W, wbuf, c, tb, ps, psb):
        for dc in range(8):
            self.mm(ps, W[:, dc, c * 128:(c + 1) * 128], self.XT[:, dc, tb * 512:(tb + 1) * 512], dc == 0, dc == 7,
                    [wbuf] + self.XTb[tb * 4:tb * 4 + 4], [psb])

    def _proj_v(self, W, wbuf):
        nc = self.nc
        psr = self.psring([0, 1, 2, 3])
        for tt in range(NT):
            ps, psb = psr.next()
            for dc in range(8):
                self.mm(ps, self.XT[:, dc, tt * 128:(tt + 1) * 128], W[:, dc, :], dc == 0, dc == 7, [wbuf, self.XTb[tt]], [psb])
            if tt % 2 == 0:
                self.act(self.V[:, tt, :], ps, AF.Copy, [psb], [self.Vb[tt]])
            else:
                self.dve(lambda: nc.vector.tensor_copy(self.V[:, tt, :], ps), [psb], [self.Vb[tt]])

    def _in_proj(self, L):
        nc, fw = self.nc, self.fw
        ng = 5 if L == 0 else 4
        pref = getattr(self, "pref_win", {}) if L == 1 else {}
        slot_of = self._win_slot(L)
        Ws = [pref[g] if g in pref else self._load_w_in(L, g, slot_of[g]) for g in range(ng)]
        Wbs = [self.Wb[slot_of[g]] for g in range(ng)]
        psr = self.psring([0, 1, 2, 3])
        if L == 0:
            fw.dma(fw.sp, self.SCR[0], self.rope_d[0], writes=[self.SCRb[0]])
            fw.dma(fw.sp, self.SCR[1], self.rope_d[1], writes=[self.SCRb[1]])
            for c in range(4):
                self.pool(lambda: nc.gpsimd.memset(self.AU[:, c, 0:32], 0.0), [], [self.AUb[c]])
            for c in range(4):
                for tb in range(4):
                    pv, pvb = psr.next()
                    pg, pgb = psr.next()
                    self._proj_fm(Ws[0], self.Wb[0], c, tb, pv, pvb)
                    self._proj_fm(Ws[1], self.Wb[1], c, tb, pg, pgb)
                    sg, sgb = self.tmpf.next()
                    self.act(sg, pg, AF.Sigmoid, [pgb], [sgb])
                    dst = self.AU[:, c, 32 + tb * 512:32 + (tb + 1) * 512]
                    self.dve(lambda: nc.vector.tensor_tensor(dst, pv, sg, ALU.mult), [pvb, sgb], [self.AUb[c]])
            if self.stop == "l0_glu":
                return
            self._conv_diag()
            pmr = self.psring([4, 5])
            pending = None

            import os
            KD = int(os.environ.get("KDBG", "0"))

            def rope_tail(item):
                c8, tb, p1, p1b, qb, qbb = item
                if KD == 1:
                    return
                if KD == 2:
                    p2, p2b = pmr.next()
                    self.mm(p2, self.pm_b, qb, True, True, [self.Bc, qbb], [p2b])
                    return
                if KD == 3:
                    t1, t1b = self.tmpf.next()
                    sl = slice(tb * 512, (tb + 1) * 512)
                    self.dve(lambda: nc.vector.tensor_tensor(t1, p1, self.SCR[0][:, sl], ALU.mult), [p1b, self.SCRb[0]], [t1b])
                    return
                if KD == 6:
                    t1, t1b = self.tmpf.next()
                    self.dve(lambda: nc.vector.tensor_tensor(t1, self.cstf[:, 0:512], self.cstf[:, 0:512], ALU.mult), [self.Bc], [t1b])
                    return
                if KD == 7:
                    t1, t1b = self.tmpf.next()
                    self.dve(lambda: nc.vector.tensor_copy(t1, p1), [p1b], [t1b])
                    return
                if KD == 8:
                    t1, t1b = self.tmpf.next()
                    self.dve(lambda: nc.vector.tensor_tensor(qb, p1, self.cstf[:, 0:512], ALU.mult), [p1b, self.Bc], [qbb])
                    return
                if KD == 4:
                    t1, t1b = self.tmpf.next()
                    self.dve(lambda: nc.vector.tensor_tensor(t1, p1, self.cstf[:, 0:512], ALU.mult), [p1b, self.Bc], [t1b])
                    return
                if KD == 5:
                    t1, t1b = self.tmpf.next()
                    sl = slice(tb * 512, (tb + 1) * 512)
                    self.dve(lambda: nc.vector.tensor_tensor(t1, p1, self.SCR[0][:, sl], ALU.mult), [p1b, self.SCRb[0]], [t1b])
                    if c8 == 0 and tb == 1:
                        raise StopIteration
                    return
                p2, p2b = pmr.next()
                self.mm(p2, self.pm_b, qb, True, True, [self.Bc, qbb], [p2b])
                t1, t1b = self.tmpf.next()
                t2, t2b = self.tmpf.next()
                sl = slice(tb * 512, (tb + 1) * 512)
                self.dve(lambda: nc.vector.tensor_tensor(t1, p1, self.SCR[0][:, sl], ALU.mult), [p1b, self.SCRb[0]], [t1b])
                self.dve(lambda: nc.vector.tensor_tensor(t2, p2, self.SCR[1][:, sl], ALU.mult), [p2b, self.SCRb[1]], [t2b])
                self.dve(lambda: nc.vector.tensor_tensor(self.QK[:, c8, sl], t1, t2, ALU.add), [t1b, t2b], [self.QKb[c8][tb]])

            try:
                for g in (2, 3):
                    for c in range(4):
                        for tb in range(4):
                            p1, p1b = psr.next()
                            self._proj_fm(Ws[g], self.Wb[g], c, tb, p1, p1b)
                            qb, qbb = self.tmpb.next()
                            self.act(qb, p1, AF.Copy, [p1b], [qbb])
                            if pending is not None:
                                rope_tail(pending)
                            pending = ((g - 2) * 4 + c, tb, p1, p1b, qb, qbb)
                rope_tail(pending)
            except StopIteration:
                pass
            if self.stop == "l0_qk":
                return
            self._proj_v(Ws[4], self.Wb[4])
        else:
            for c in range(4):
                self.pool(lambda: nc.gpsimd.memset(self.AU[:, c, 0:16], 0.0), [], [self.AUb[c]])
            for c in range(4):
                for tb in range(4):
                    p1, p1b = psr.next()
                    self._proj_fm(Ws[3], Wbs[3], c, tb, p1, p1b)
                    self.act(self.AU[:, c, 16 + tb * 512:16 + (tb + 1) * 512], p1, AF.Copy, [p1b], [self.AUb[c]])
            self._pool_prefix()
            for g in (0, 1):
                for c in range(4):
                    for tb in range(4):
                        p1, p1b = psr.next()
                        self._proj_fm(Ws[g], Wbs[g], c, tb, p1, p1b)
                        self.act(self.QK[:, g * 4 + c, tb * 512:(tb + 1) * 512], p1, AF.Copy, [p1b], [self.QKb[g * 4 + c][tb]])
            self._proj_v(Ws[2], Wbs[2])

    def _conv_diag(self):
        nc = self.nc
        homes = [(self.Wblk[0], [self.Wb[0]]), (self.Wblk[1], [self.Wb[1]]), (self.Wblk[5], [self.Wb[5]]),
                 (self.ht, [self.htr.items[0][1], self.htr.items[1][1]])]
        self.Dg = []
        for c in range(4):
            home, bufs = homes[c]
            D3 = home[:, 0:31 * 128].rearrange("p (w j) -> p w j", w=31)
            self.dve(lambda: nc.vector.tensor_tensor(D3, self.ident_b.unsqueeze(1).broadcast_to([128, 31, 128]),
                                                     self.cw[:, c, :].unsqueeze(2).broadcast_to([128, 31, 128]), ALU.mult),
                     [self.Bc], bufs)
            self.Dg.append((D3, bufs))

    def _conv_pe(self):
        MIX = self.XT
        ps, psb = self.PS[3], self.PSb[3]
        for c in range(4):
            D3, dbufs = self.Dg[c]
            for tb in range(4):
                for w in range(31):
                    o = 2 + w + tb * 512
                    self.mm(ps, D3[:, w, :], self.AU[:, c, o:o + 512], w == 0, w == 30, dbufs + [self.AUb[c]], [psb])
                    yield
                self.act(MIX[:, c, tb * 512:(tb + 1) * 512], ps, AF.Identity, [psb, self.Bc], self.XTb[tb * 4:tb * 4 + 4],
                         bias=self.conv_b[:, c:c + 1])
                yield

    def _conv_ln(self):
        nc = self.nc
        MIX = self.XT
        sls = [slice(tb * 512, (tb + 1) * 512) for tb in range(4)]
        for tb in range(4):
            sl = sls[tb]
            xb4 = self.XTb[tb * 4:tb * 4 + 4]
            sum_ps, sum_b = self.PS[tb], self.PSb[tb]
            sq_ps, sq_b = self.PS[4 + tb], self.PSb[4 + tb]
            for c in range(4):
                sq, sqb = self.tmpb.next()
                self.act(sq, MIX[:, c, sl], AF.Square, xb4, [sqb])
                self.mm(sum_ps, self.ones_b, MIX[:, c, sl], c == 0, c == 3, [self.Bc] + xb4, [sum_b])
                self.mm(sq_ps, self.ones_b, sq, c == 0, c == 3, [self.Bc, sqb], [sq_b])
        for tb in range(4):
            sl = sls[tb]
            mean, var = self.SCR[0][:, sl], self.SCR[1][:, sl]
            self.act(mean, self.PS[tb], AF.Copy, [self.PSb[tb]], [self.SCRb[0]], scale=1.0 / 512)
            nmsq, nmsqb = self.tmpf.next()
            self.dve(lambda: nc.vector.scalar_tensor_tensor(nmsq, mean, -1.0, mean, ALU.mult, ALU.mult), [self.SCRb[0]], [nmsqb])
            self.dve(lambda: nc.vector.scalar_tensor_tensor(var, self.PS[4 + tb], 1.0 / 512, nmsq, ALU.mult, ALU.add), [self.PSb[4 + tb], nmsqb], [self.SCRb[1]])
            self.dve(lambda: nc.vector.tensor_scalar(var, var, LN_EPS, None, ALU.add), [self.SCRb[1]], [self.SCRb[1]])
            self.act(var, var, AF.Ln, [self.SCRb[1]], [self.SCRb[1]])
            self.act(var, var, AF.Exp, [self.SCRb[1]], [self.SCRb[1]], scale=-0.5)
        for tb in range(4):
            sl = sls[tb]
            xb4 = self.XTb[tb * 4:tb * 4 + 4]
            mean, var = self.SCR[0][:, sl], self.SCR[1][:, sl]
            for c in range(4):
                d, db = self.tmpf.next()
                self.dve(lambda: nc.vector.tensor_tensor(d, MIX[:, c, sl], mean, ALU.subtract), xb4 + [self.SCRb[0]], [db])
                self.dve(lambda: nc.vector.tensor_tensor(d, d, var, ALU.mult), [db, self.SCRb[1]], [db])
                self.act(MIX[:, c, sl], d, AF.Silu, [db, self.Bc], xb4, scale=self.cln_g[:, c:c + 1], bias=self.cln_b[:, c:c + 1])

    def _conv_branch(self):
        for _ in self._conv_pe():
            pass
        self._conv_ln()

    def _diff_attn(self, side=None):
        nc = self.nc
        MIX = self.XT
        QK, V = self.QK, self.V
        scr = self.psring([0, 1, 2])
        O = [(self.PS[4], self.PSb[4]), (self.PS[6], self.PSb[6])]

        def pull(n):
            if side is not None:
                for _ in range(n):
                    next(side, None)
        Dn = [(self.PS[5], self.PSb[5]), (self.PS[7], self.PSb[7])]
        deferred = []
        for h in range(4):
            for qb in range(4):
                nkt = 4 * (qb + 1)
                q0 = qb * 512
                steps = [(kt, j) for kt in range(nkt) for j in range(2)]

                def stage_a(kt, j):
                    i = kt - 4 * qb
                    c0 = 128 * i if i > 0 else 0
                    sp_, spb = scr.next()
                    pr = slice(64 * j, 64 * j + 64)
                    self.mm(sp_[:, c0:512], QK[pr, 4 + h, kt * 128:(kt + 1) * 128], QK[pr, h, q0 + c0:q0 + 512], True, True,
                            [self.QKb[4 + h][kt // 4], self.QKb[h][qb]], [spb])
                    pt, ptb = self.tmpb.next()
                    self.act(pt[:, c0:512], sp_[:, c0:512], AF.Exp, [spb], [ptb], scale=0.125)
                    if i >= 0:
                        self.pool(lambda: nc.gpsimd.tensor_tensor(pt[:, c0:c0 + 128], pt[:, c0:c0 + 128], self.mle_b, ALU.mult), [ptb, self.Bc], [ptb])
                    return (kt, j, c0, pt, ptb)

                def stage_b(kt, j, c0, pt, ptb):
                    self.mm(O[j][0][:, c0:512], V[:, kt, h * 128:(h + 1) * 128], pt[:, c0:512], kt == 0, kt == nkt - 1,
                            [self.Vb[kt], ptb], [O[j][1]])
                    self.mm(Dn[j][0][:, c0:512], self.ones_b, pt[:, c0:512], kt == 0, kt == nkt - 1, [self.Bc, ptb], [Dn[j][1]])

                pend = []
                for si, (kt, j) in enumerate(steps):
                    pend.append(stage_a(kt, j))
                    if len(pend) > 2:
                        stage_b(*pend.pop(0))
                    pull(1)
                    while deferred and deferred[0][0] <= si:
                        deferred.pop(0)[1]()
                while pend:
                    stage_b(*pend.pop(0))
                while deferred:
                    deferred.pop(0)[1]()
                os_ = []
                for j in range(2):
                    r, rb = self.tmpf.next()
                    self.act(r, Dn[j][0], AF.Ln, [Dn[j][1]], [rb])
                    self.act(r, r, AF.Exp, [rb], [rb], scale=-1.0)
                    o, ob = self.tmpf.next()
                    self.dve(lambda: nc.vector.tensor_tensor(o, O[j][0], r, ALU.mult), [O[j][1], rb], [ob])
                    os_.append((o, ob))
                (o1, o1b), (o2, o2b) = os_
                self.dve(lambda: nc.vector.scalar_tensor_tensor(o1, o2, self.neglam, o1, ALU.mult, ALU.add), [o2b, o1b, self.Bsmall], [o1b])
                st = {}

                def fin_a(o1=o1, o1b=o1b, st=st):
                    st["osq"], st["osqb"] = self.tmpb.next()
                    self.act(st["osq"], o1, AF.Square, [o1b], [st["osqb"]])

                def fin_b(st=st):
                    ss, ssb = scr.next()
                    self.mm(ss, self.ones_b, st["osq"], True, True, [self.Bc, st["osqb"]], [ssb])
                    st["rs"], st["rsb"] = self.tmpf.next()
                    rs = st["rs"]
                    self.dve(lambda: nc.vector.tensor_scalar(rs, ss, 1.0 / 128, RMS_EPS, ALU.mult, ALU.add), [ssb], [st["rsb"]])

                def fin_c(o1=o1, o1b=o1b, st=st, h=h, q0=q0, qb=qb):
                    rs, rsb = st["rs"], st["rsb"]
                    self.act(rs, rs, AF.Ln, [rsb], [rsb])
                    self.act(rs, rs, AF.Exp, [rsb], [rsb], scale=-0.5)
                    self.dve(lambda: nc.vector.scalar_tensor_tensor(MIX[:, 4 + h, q0:q0 + 512], o1, self.gsc, rs, ALU.mult, ALU.mult),
                             [o1b, rsb, self.Bsmall], self.XTb[qb * 4:qb * 4 + 4])

                deferred = [(2, fin_a), (4, fin_b), (6, fin_c)]
                pull(12)
        while deferred:
            deferred.pop(0)[1]()
        if side is not None:
            for _ in side:
                pass

    def _pool_prefix(self):
        nc = self.nc
        for g in range(4):
            U = self.AU[:, g, :]
            bufs = [(self.SCR[0], self.SCRb[0]), (self.SCR[1], self.SCRb[1])]
            cur, curb = bufs[0]
            self.dve(lambda: nc.vector.tensor_tensor(cur, U[:, 16:16 + S], U[:, 15:15 + S], ALU.add), [self.AUb[g]], [curb])
            k = 0
            sh = 2
            while sh < POOL_WINDOWS[g]:
                nxt, nxtb = bufs[(k + 1) % 2]
                self.dve(lambda: nc.vector.tensor_tensor(nxt[:, sh:S], cur[:, sh:S], cur[:, 0:S - sh], ALU.add), [curb], [nxtb])
                self.pool(lambda: nc.gpsimd.tensor_copy(nxt[:, 0:sh], cur[:, 0:sh]), [curb], [nxtb])
                cur, curb = nxt, nxtb
                k += 1
                sh *= 2
            win = POOL_WINDOWS[g]
            t, tb_ = self.tmpf.next()
            self.dve(lambda: nc.vector.tensor_tensor(t[:, 0:16], cur[:, 0:16], self.invc[:, g, :], ALU.mult), [curb, self.Bc], [tb_])
            self.dve(lambda: nc.vector.tensor_tensor(t[:, 0:16], t[:, 0:16], U[:, 16:32], ALU.subtract), [tb_, self.AUb[g]], [tb_])
            for tb in range(4):
                sl = slice(tb * 512, (tb + 1) * 512)
                usl = U[:, 16 + tb * 512:16 + (tb + 1) * 512]
                self.dve(lambda: nc.vector.scalar_tensor_tensor(usl, cur[:, sl], 1.0 / win, usl, ALU.mult, ALU.subtract),
                         [curb, self.AUb[g]], [self.AUb[g]])
            self.dve(lambda: nc.vector.tensor_copy(U[:, 16:32], t[:, 0:16]), [tb_], [self.AUb[g]])

    def _pool_mix(self):
        nc = self.nc
        MIX = self.XT
        pr = self.psring([4, 5, 6, 7])
        for g in range(4):
            for tb in range(4):
                sl = slice(tb * 512, (tb + 1) * 512)
                pm_ps, pm_b = pr.next()
                self.mm(pm_ps, self.pwb[:, g, :], self.AU[:, g, 16 + tb * 512:16 + (tb + 1) * 512], True, True, [self.Bc, self.AUb[g]], [pm_b])
                self.dve(lambda: nc.vector.tensor_scalar(MIX[:, 4 + g, sl], pm_ps, self.pscale[:, g:g + 1], None, ALU.mult),
                         [pm_b, self.Bc], self.XTb[tb * 4:tb * 4 + 4])

    def _sb_attn(self):
        nc = self.nc
        MIX = self.XT
        QK, V = self.QK, self.V
        self.fw.barrier()
        tf, tb_ = self.tf_full, self.tb_full
        fpr = Ring([tf[:, i * 1024:(i + 1) * 1024] for i in range(4)], "tfp")
        spr = Ring([tb_[:, i * 1024:(i + 1) * 1024] for i in range(3)], "tbp")
        apr = Ring([tb_[:, 3072 + i * 1024:3072 + (i + 1) * 1024] for i in range(2)], "tap")
        zpairs = [(self.PSpair[0], [self.PSb[0], self.PSb[1]]), (self.PSpair[1], [self.PSb[2], self.PSb[3]])]
        zi = [0]
        Rp, Rb = self.PSpair[2], [self.PSb[4], self.PSb[5]]
        Or = self.psring([6])
        zeros = self.zeros_b
        self.pool(lambda: nc.gpsimd.memset(zeros, 0.0), [], [self.Bsmall])
        w = lambda ap, c0: ap.rearrange("p (h q) -> p h q", h=2)[:, :, c0:512]
        hv = lambda ap, hh: ap[:, hh * 512:(hh + 1) * 512]
        for c in range(4):
            for qb in range(4):
                q0 = qb * 512
                nkt = 4 * (qb + 1)
                o_ps, o_b = Or.next()
                for hh in range(2):
                    self.mm(hv(Rp, hh), zeros, QK[:, c, q0:q0 + 512], True, True, [self.Bsmall, self.QKb[c][qb]], [Rb[hh]], inc=True)
                kts = list(range(nkt - 1, -1, -1))

                def a_pe(idx, kt):
                    i = kt - 4 * qb
                    c0 = 128 * i if i > 0 else 0
                    zp, zb = zpairs[zi[0] % 2]
                    zi[0] += 1
                    for hh in range(2):
                        pr = slice(64 * hh, 64 * hh + 64)
                        self.mm(hv(zp, hh)[:, c0:512], QK[pr, 4 + c, kt * 128:(kt + 1) * 128], QK[pr, c, q0 + c0:q0 + 512], True, True,
                                [self.QKb[4 + c][kt // 4], self.QKb[c][qb]], [zb[hh]])
                    return dict(idx=idx, kt=kt, i=i, c0=c0, zp=zp, zb=zb)

                def a_act(st):
                    c0 = st["c0"]
                    e, eb = fpr.next()
                    self.act(w(e, c0), w(st["zp"], c0), AF.Exp, st["zb"], [eb], scale=0.125)
                    sp_, spb = spr.next()
                    self.act(w(sp_, c0), w(e, c0), AF.Ln, [eb], [spb], bias=1.0)
                    if st["i"] >= 0:
                        for hh in range(2):
                            blk = hv(sp_, hh)[:, c0:c0 + 128]
                            self.pool(lambda: nc.gpsimd.tensor_tensor(blk, blk, self.mlt_b, ALU.mult), [spb, self.Bc], [spb])
                    st["sp"], st["spb"] = sp_, spb

                def b_copy(st):
                    c0 = st["c0"]
                    rsb_, rsbb = fpr.next()
                    self.dve(lambda: nc.vector.tensor_copy(w(rsb_, c0), w(Rp, c0)), Rb, [rsbb])
                    st["rsb"], st["rsbb"] = rsb_, rsbb

                def b_pe(st):
                    c0 = st["c0"]
                    for hh in range(2):
                        sph = hv(st["sp"], hh)[:, c0:512]
                        self.mm(hv(st["zp"], hh)[:, c0:512], self.ntri_b, sph, False, True, [self.Bc, st["spb"]], [st["zb"][hh]], inc=True)
                        self.mm(hv(Rp, hh)[:, c0:512], self.ones_b, sph, False, True, [self.Bc, st["spb"]], [Rb[hh]], inc=True)

                def b_dve(st):
                    c0 = st["c0"]
                    t, tbf = fpr.next()
                    self.dve(lambda: nc.vector.scalar_tensor_tensor(w(t, c0), w(st["zp"], c0), 0.125, w(st["rsb"], c0), ALU.mult, ALU.subtract),
                             st["zb"] + [st["rsbb"]], [tbf])
                    st["t"], st["tbf"] = t, tbf

                def b_act(st):
                    c0 = st["c0"]
                    a, ab = apr.next()
                    self.act(w(a, c0), w(st["t"], c0), AF.Exp, [st["tbf"]], [ab])
                    if st["i"] >= 0:
                        for hh in range(2):
                            blk = hv(a, hh)[:, c0:c0 + 128]
                            self.pool(lambda: nc.gpsimd.tensor_tensor(blk, blk, self.mlt_b, ALU.mult), [ab, self.Bc], [ab])
                    return (st["idx"], st["kt"], c0, a, ab)

                def stage_c(idx, kt, c0, a, ab):
                    for hh in range(2):
                        pr = slice(64 * hh, 64 * hh + 64)
                        h = 2 * c + hh
                        self.mm(o_ps[pr, c0:512], V[:, kt, h * 64:(h + 1) * 64], hv(a, hh)[:, c0:512], idx == 0, idx == nkt - 1,
                                [self.Vb[kt], ab], [o_b], inc=True)

                pa = None
                pc = None
                for idx in range(nkt + 2):
                    cur = a_pe(idx, kts[idx]) if idx < nkt else None
                    if cur is not None:
                        for _ in range(NFILL):
                            self.mm(self.PS[7], self.ones_b, QK[:, c, q0:q0 + 512], True, True, [self.Bc, self.QKb[c][qb]], [self.PSb[7]], inc=False)
                    if pa is not None:
                        b_copy(pa)
                        b_pe(pa)
                        b_dve(pa)
                    if pc is not None:
                        stage_c(*pc)
                    if cur is not None:
                        a_act(cur)
                    nc_ = b_act(pa) if pa is not None else None
                    pa, pc = cur, nc_
                self.act(MIX[:, c, q0:q0 + 512], o_ps, AF.Copy, [o_b], self.XTb[qb * 4:qb * 4 + 4])

    def _load_ln(self, idx, scale=None):
        nc, fw = self.nc, self.fw
        G = self.SCR[0][:, 0:1024]
        B_ = self.SCR[1][:, 0:1024]
        fw.dma(fw.sp, G, self.lnp_d[2 * idx].partition_broadcast(128), writes=[self.SCRb[0]])
        fw.dma(fw.sp, B_, self.lnp_d[2 * idx + 1].partition_broadcast(128), writes=[self.SCRb[1]])
        if scale is not None:
            self.act(G, G, AF.Copy, [self.SCRb[0]], [self.SCRb[0]], scale=scale)
            self.act(B_, B_, AF.Copy, [self.SCRb[1]], [self.SCRb[1]], scale=scale)
        return G, B_

    def _ln_stats(self, tt):
        nc = self.nc
        xs = self.X[:, tt, :]
        st_, stb = self.statr.next()
        xb = [self.Xb[tt]]
        self.dve(lambda: nc.vector.bn_stats(st_[:, 0:6], xs[:, 0:512]), xb, [stb])
        self.dve(lambda: nc.vector.bn_stats(st_[:, 6:12], xs[:, 512:1024]), xb, [stb])
        self.dve(lambda: nc.vector.bn_aggr(st_[:, 12:14], st_[:, 0:12]), [stb], [stb])
        self.dve(lambda: nc.vector.tensor_scalar(st_[:, 14:15], st_[:, 13:14], LN_EPS, None, ALU.add), [stb], [stb])
        self.act(st_[:, 14:15], st_[:, 14:15], AF.Ln, [stb], [stb])
        self.act(st_[:, 14:15], st_[:, 14:15], AF.Exp, [stb], [stb], scale=-0.5)
        return (tt, st_, stb)

    def _ln_apply(self, tt, st_, stb, G, B_):
        nc = self.nc
        xs = self.X[:, tt, :]
        xb = [self.Xb[tt]]
        self.dve(lambda: nc.vector.scalar_tensor_tensor(xs, xs, st_[:, 12:13], G, ALU.subtract, ALU.mult), xb + [stb, self.SCRb[0]], xb)
        self.dve(lambda: nc.vector.scalar_tensor_tensor(xs, xs, st_[:, 14:15], B_, ALU.mult, ALU.add), xb + [stb, self.SCRb[1]], xb)

    def _ln_tile(self, tt, G, B_):
        self._ln_apply(*self._ln_stats(tt), G, B_)

    def _load_wout(self, L):
        fw = self.fw
        for i in range(2):
            src = self.wout[L].rearrange("(c p) n -> p c n", p=128)[:, i * 4:(i + 1) * 4, :]
            dst = self.Wblk[3 + i].rearrange("p (c n) -> p c n", c=4)
            fw.dma(fw.pool, dst, src, writes=[self.Wb[3 + i]])

    def _mid(self, L):
        nc, fw = self.nc, self.fw
        MIX = self.XT
        inv = 1.0 / ALPHA
        Wo = [self.Wblk[3].rearrange("p (c n) -> p c n", c=4), self.Wblk[4].rearrange("p (c n) -> p c n", c=4)]
        W0 = self._expert_load(L, 0)
        G, B_ = self._load_ln(2 * L, scale=ALPHA)
        xsrc = self.x_d if L == 0 else self.spill_d
        for tt in range(NT):
            fw.dma(fw.sp, self.X[:, tt, :], xsrc[tt * 128:(tt + 1) * 128, :], writes=[self.Xb[tt]])
        psr = self.psring([5, 6])
        tpr = self.psring([0, 1])
        RB = [(self.PS[i], self.PSb[i]) for i in (2, 3, 4, 7)]

        def op_tile(tt):
            for half in range(2):
                ps, psb = psr.next()
                for fc in range(8):
                    self.mm(ps, MIX[:, fc, tt * 128:(tt + 1) * 128], Wo[fc // 4][:, fc % 4, half * 512:(half + 1) * 512], fc == 0, fc == 7,
                            [self.XTb[tt], self.Wb[3 + fc // 4]], [psb])
                xs = self.X[:, tt, half * 512:(half + 1) * 512]
                self.dve(lambda: nc.vector.scalar_tensor_tensor(xs, xs, ALPHA, ps, ALU.mult, ALU.add), [self.Xb[tt], psb], [self.Xb[tt]])
            st = self._ln_stats(tt)
            ln_flush()
            ln_pend.append(st)

        ln_pend = []

        def ln_flush():
            while ln_pend:
                self._ln_apply(*ln_pend.pop(0), G, B_)

        def xt_T(tb, dc):
            ps, psb = tpr.next()
            for j in range(4):
                tt = tb * 4 + j
                self.tr(ps[:, j * 128:(j + 1) * 128], self.X[:, tt, dc * 128:(dc + 1) * 128], self.ident_f,
                        [self.Xb[tt], self.Bc], [psb], inc=(j == 3))
            xf, xfb = self.tmpf.next()
            self.act(xf, ps, AF.Copy, [psb], [xfb], scale=inv)
            self.act(self.XT[:, dc, tb * 512:(tb + 1) * 512], ps, AF.Copy, [psb], self.XTb[tb * 4:tb * 4 + 4], scale=inv)
            return (tb, dc, xf, xfb)

        def xt_R(tb, dc, xf, xfb):
            for j in range(4):
                self.mm(RB[j][0][:, 0:NE], xf[:, j * 128:(j + 1) * 128], self.wr[:, dc, :], dc == 0, dc == 7, [self.Bc, xfb], [RB[j][1]])
            if dc == 7:
                for j in range(4):
                    tt = tb * 4 + j
                    self.act(self.lgt[:, tt * NE:(tt + 1) * NE], RB[j][0][:, 0:NE], AF.Copy, [RB[j][1]], [self.Brt])

        pend = []

        def xt_step(tb, dc):
            pend.append(xt_T(tb, dc))
            if len(pend) > 1:
                xt_R(*pend.pop(0))

        for tt in range(4):
            op_tile(tt)
        for tb in range(4):
            for j in range(4):
                if tb + 1 < 4:
                    op_tile((tb + 1) * 4 + j)
                else:
                    ln_flush()
                xt_step(tb, 2 * j)
                xt_step(tb, 2 * j + 1)
        while pend:
            xt_R(*pend.pop(0))
        return W0

    def _routing(self):
        nc = self.nc
        def wt():
            t, tb_ = self.tmpf.next()
            return t[:, 0:256], tb_
        aff, affb = wt()
        self.act(aff, self.lgt, AF.Sigmoid, [self.Brt], [affb])
        sel, selb = wt()
        v3 = lambda a: a.rearrange("p (t e) -> p t e", t=NT)
        v4 = lambda a: a.rearrange("p (t g j) -> p t g j", t=NT, g=4)
        g3 = lambda a: a.rearrange("p (t g) -> p t g", t=NT)
        self.dve(lambda: nc.vector.tensor_tensor(v3(sel), v3(aff), self.rbias.unsqueeze(1).broadcast_to([128, NT, NE]), ALU.add), [affb, self.Bc], [selb])
        sm = self.small
        m1 = sm[:, 8:8 + 64] if False else None
        m1t, m1b = self.tmpf.next()
        m1 = m1t[:, 0:64]
        m2 = m1t[:, 64:128]
        gs = m1t[:, 128:192]
        gmax = m1t[:, 192:208]
        gmask = m1t[:, 208:272]
        gsum = m1t[:, 272:288]
        self.dve(lambda: nc.vector.tensor_reduce(m1, sel.rearrange("p (tg j) -> p tg j", j=4), AX.X, ALU.max), [selb], [m1b])
        eq, eqb = wt()
        m1bc = m1.unsqueeze(2).broadcast_to([128, 64, 4])
        self.dve(lambda: nc.vector.tensor_tensor(eq.rearrange("p (tg j) -> p tg j", j=4), sel.rearrange("p (tg j) -> p tg j", j=4), m1bc, ALU.is_equal), [selb, m1b], [eqb])
        self.dve(lambda: nc.vector.scalar_tensor_tensor(eq, eq, -1.0e9, sel, ALU.mult, ALU.add), [eqb, selb], [eqb])
        self.dve(lambda: nc.vector.tensor_reduce(m2, eq.rearrange("p (tg j) -> p tg j", j=4), AX.X, ALU.max), [eqb], [m1b])
        self.dve(lambda: nc.vector.tensor_tensor(gs, m1, m2, ALU.add), [m1b], [m1b])
        self.dve(lambda: nc.vector.tensor_reduce(gmax, g3(gs), AX.X, ALU.max), [m1b], [m1b])
        self.dve(lambda: nc.vector.tensor_tensor(g3(gmask), g3(gs), gmax.unsqueeze(2).broadcast_to([128, NT, 4]), ALU.is_equal), [m1b], [m1b])
        ge, geb = wt()
        m2bc = m2.unsqueeze(2).broadcast_to([128, 64, 4])
        gmbc = gmask.unsqueeze(2).broadcast_to([128, 64, 4])
        j4 = lambda a: a.rearrange("p (tg j) -> p tg j", j=4)
        self.dve(lambda: nc.vector.tensor_tensor(j4(ge), j4(sel), m2bc, ALU.is_ge), [selb, m1b], [geb])
        self.dve(lambda: nc.vector.tensor_tensor(j4(ge), j4(ge), gmbc, ALU.mult), [geb, m1b], [geb])
        self.dve(lambda: nc.vector.tensor_tensor(ge, ge, aff, ALU.mult), [geb, affb], [geb])
        self.dve(lambda: nc.vector.tensor_reduce(gsum, v3(ge), AX.X, ALU.add), [geb], [m1b])
        self.dve(lambda: nc.vector.reciprocal(gsum, gsum), [m1b], [m1b])
        self.dve(lambda: nc.vector.tensor_tensor(v3(self.comb), v3(ge), gsum.unsqueeze(2).broadcast_to([128, NT, NE]), ALU.mult), [geb, m1b], [self.Bcomb])

    def _expert_load(self, L, e):
        fw = self.fw
        s = e % 2
        Wg = self.Wblk[3 * s].rearrange("p (c n) -> p c n", c=8)
        Wu = self.Wblk[3 * s + 1].rearrange("p (c n) -> p c n", c=8)
        Wd = self.Wblk[3 * s + 2].rearrange("p (c n) -> p c n", c=4)
        fw.dma(fw.pool, Wg, self.wg[L][e].rearrange("(c p) n -> p c n", p=128), writes=[self.Wb[3 * s]])
        fw.dma(fw.pool, Wu, self.wu[L][e].rearrange("(c p) n -> p c n", p=128), writes=[self.Wb[3 * s + 1]])
        fw.dma(fw.pool, Wd, self.wd[L][e].rearrange("(c p) n -> p c n", p=128), writes=[self.Wb[3 * s + 2]])
        return (Wg, Wu, Wd, s)

    def _experts(self, L, W0):
        nc, fw = self.nc, self.fw
        gr = self.psring([0, 1])
        ur = self.psring([2, 3])
        yr = self.psring([4, 5, 6, 7])
        comb3 = self.comb.rearrange("p (t e) -> p t e", t=NT)

        def load(e):
            s = e % 2
            Wg = self.Wblk[3 * s].rearrange("p (c n) -> p c n", c=8)
            Wu = self.Wblk[3 * s + 1].rearrange("p (c n) -> p c n", c=8)
            Wd = self.Wblk[3 * s + 2].rearrange("p (c n) -> p c n", c=4)
            fw.dma(fw.pool, Wg, self.wg[L][e].rearrange("(c p) n -> p c n", p=128), writes=[self.Wb[3 * s]])
            fw.dma(fw.pool, Wu, self.wu[L][e].rearrange("(c p) n -> p c n", p=128), writes=[self.Wb[3 * s + 1]])
            fw.dma(fw.pool, Wd, self.wd[L][e].rearrange("(c p) n -> p c n", p=128), writes=[self.Wb[3 * s + 2]])
            return (Wg, Wu, Wd, s)

        G_, B_ = self._load_ln(2 * L + 1)

        lnp = []

        def ln_fin():
            while lnp:
                st = lnp.pop(0)
                self._ln_apply(*st, G_, B_)
                if L == 1:
                    tt = st[0]
                    fw.dma(fw.sp, self.out_d[tt * 128:(tt + 1) * 128, :], self.X[:, tt, :], reads=[self.Xb[tt]])

        def ln_out(tt):
            st = self._ln_stats(tt)
            ln_fin()
            lnp.append(st)

        def stage_gu(e, tb, W, ln_tiles=()):
            Wg, Wu, Wd, s = W
            H, Hb = self.htr.next()
            xb4 = self.XTb[tb * 4:tb * 4 + 4]
            for fc in range(4):
                g, gb = gr.next()
                u, ub = ur.next()
                for dc in range(8):
                    self.mm(g, Wg[:, dc, fc * 128:(fc + 1) * 128], self.XT[:, dc, tb * 512:(tb + 1) * 512], dc == 0, dc == 7, [self.Wb[3 * s]] + xb4, [gb])
                for dc in range(8):
                    self.mm(u, Wu[:, dc, fc * 128:(fc + 1) * 128], self.XT[:, dc, tb * 512:(tb + 1) * 512], dc == 0, dc == 7, [self.Wb[3 * s + 1]] + xb4, [ub])
                sg, sgb = self.tmpf.next()
                self.act(sg, g, AF.Silu, [gb], [sgb])
                self.dve(lambda: nc.vector.tensor_tensor(H[:, fc, :], u, sg, ALU.mult), [ub, sgb], [Hb])
                if fc < len(ln_tiles):
                    ln_out(ln_tiles[fc])
            return (e, tb, W, H, Hb)

        def stage_dn(e, tb, W, H, Hb):
            Wg, Wu, Wd, s = W
            for t4 in range(4):
                tt = tb * 4 + t4
                for half in range(2):
                    y, yb = yr.next()
                    for fc in range(4):
                        self.mm(y, H[:, fc, t4 * 128:(t4 + 1) * 128], Wd[:, fc, half * 512:(half + 1) * 512], fc == 0, fc == 3, [Hb, self.Wb[3 * s + 2]], [yb])
                    xs = self.X[:, tt, half * 512:(half + 1) * 512]
                    self.dve(lambda: nc.vector.scalar_tensor_tensor(xs, y, comb3[:, tt, e:e + 1], xs, ALU.mult, ALU.add), [yb, self.Bcomb, self.Xb[tt]], [self.Xb[tt]])

        Wn = W0
        prev = None
        for e in range(NE):
            W = Wn
            for tb in range(4):
                if e == NE - 1:
                    stage_dn(*prev)
                    done = list(range((tb - 1) * 4, tb * 4)) if tb > 0 else []
                    cur = stage_gu(e, tb, W, done)
                else:
                    cur = stage_gu(e, tb, W)
                    if prev is not None:
                        stage_dn(*prev)
                prev = cur
                if tb == 0 and e + 1 < NE:
                    Wn = self._expert_load(L, e + 1)
                if tb == 0 and e + 1 == NE and L == 0:
                    self.pref_win = {g: self._load_w_in(1, g, self._win_slot(1)[g]) for g in (3, 0, 1)}
        stage_dn(*prev)
        for tt in range(12, 16):
            ln_out(tt)
        ln_fin()

    def _final_ln(self, L):
        pass


def _constants():
    j = np.arange(128)[:, None]
    k = np.arange(128)[None, :]
    ident = (j == k).astype(np.float32)
    tri = (j >= k).astype(np.float32)
    mle = (j <= k).astype(np.float32)
    mlt = (j < k).astype(np.float32)
    m = np.arange(128)
    perm = np.where((m % 64) < 32, m + 32, m - 32)
    pm = np.zeros((128, 128), np.float32)
    pm[perm, m] = 1.0
    invc = np.zeros((128, 4, 16), np.float32)
    for g, w in enumerate(POOL_WINDOWS):
        invc[:, g, :] = 1.0 / np.minimum(np.arange(16) + 1, w)
    cst = np.concatenate([ident, tri, mle, mlt, pm, invc.reshape(128, 64)], axis=1).astype(np.float32)
    inv = (10000.0 ** (-np.arange(0, 64, 2, dtype=np.float32) / 64)).astype(np.float32)
    ang = np.arange(S, dtype=np.float32)[:, None] * inv[None, :]
    ang = np.concatenate([ang, ang], axis=-1)
    cos = np.cos(ang).astype(np.float32).T
    sin = np.sin(ang).astype(np.float32).T
    sign = np.where(np.arange(64) < 32, -1.0, 1.0).astype(np.float32)[:, None]
    sins = sin * sign
    rope = np.stack([np.concatenate([cos, cos], 0), np.concatenate([sins, sins], 0)], 0).astype(np.float32)
    return cst, np.ascontiguousarray(rope)


def _pack_inputs(inp):
    f = lambda a: np.ascontiguousarray(np.asarray(a, dtype=np.float32))
    cst, rope = _constants()
    pp = np.zeros((128, 144), np.float32)
    pp[:, 0:124] = f(inp["l0_conv_w"]).T.reshape(4, 128, 31).transpose(1, 0, 2).reshape(128, 124)
    v4 = lambda a: f(a).reshape(4, 128).T
    pp[:, 124:128] = v4(inp["l0_conv_b"])
    pp[:, 128:132] = v4(inp["l0_conv_ln_g"])
    pp[:, 132:136] = v4(inp["l0_conv_ln_b"])
    pp[:, 136] = f(inp["l0_subln_g"])
    pp[:, 137:141] = v4(inp["l1_pool_scale"])
    rowp = np.concatenate([f(inp["l0_lambda_q1"]), f(inp["l0_lambda_q2"]), f(inp["l0_lambda_k1"]), f(inp["l0_lambda_k2"]),
                           f(inp["router_bias"])]).astype(np.float32)
    lnp = np.stack([f(inp["l0_ln_mix_g"]), f(inp["l0_ln_mix_b"]), f(inp["l0_ln_ffn_g"]), f(inp["l0_ln_ffn_b"]),
                    f(inp["l1_ln_mix_g"]), f(inp["l1_ln_mix_b"]), f(inp["l1_ln_ffn_g"]), f(inp["l1_ln_ffn_b"])], 0)
    shared = {
        "router_w": f(inp["router_w"]),
        "w_in0": f(inp["l0_in_proj"]), "w_in1": f(inp["l1_in_proj"]),
        "w_out0": f(inp["l0_out_proj"]), "w_out1": f(inp["l1_out_proj"]),
        "w_gate0": f(inp["l0_w_gate"]), "w_gate1": f(inp["l1_w_gate"]),
        "w_up0": f(inp["l0_w_up"]), "w_up1": f(inp["l1_w_up"]),
        "w_down0": f(inp["l0_w_down"]), "w_down1": f(inp["l1_w_down"]),
        "pool_w": f(inp["l1_pool_w"]),
        "pp": pp, "rowp": rowp, "lnp": np.ascontiguousarray(lnp), "cst": cst, "rope": rope,
    }
    x = f(inp["x"])
    return [dict(shared, x=np.ascontiguousarray(x[c])) for c in range(x.shape[0])]


def kernel(**inputs):
    in_maps = _pack_inputs(inputs)
    nc = Prog().build()
    res = run_bass_kernel_spmd(nc, in_maps, core_ids=list(range(8)))
    return np.stack([np.asarray(r["out"], dtype=np.float32) for r in res.results], axis=0)
```

```python
import contextlib
import math
import numpy as np
import concourse.bass as bass
import concourse.mybir as mybir
from concourse.alu_op_type import AluOpType as ALU
from concourse.bass_utils import run_bass_kernel_spmd

F32 = mybir.dt.float32
BF16 = mybir.dt.bfloat16
AF = mybir.ActivationFunctionType
AX = mybir.AxisListType

S = 2048
D = 1024
NT = 16
NE = 16
ALPHA = 4.0 ** 0.25
LN_EPS = 1e-5
RMS_EPS = 1e-5
LAM_INIT0 = 0.8 - 0.6 * math.exp(0.0)
POOL_WINDOWS = (2, 4, 8, 16)
NFILL = 3


import os as _os
TRACE = bool(_os.environ.get("KTRACE"))


class Sem:
    def __init__(self, h):
        self.h = h
        self.cnt = 0


class Buf:
    __slots__ = ("last_w", "readers", "name", "excl")

    def __init__(self, name="", excl=False):
        self.last_w = None
        self.readers = []
        self.name = name
        self.excl = excl


class Eng:
    def __init__(self, name, obj, sem, same_wait=True):
        self.name = name
        self.obj = obj
        self.sem = sem
        self.waited = {}
        self.same_wait = same_wait


class FW:
    def __init__(self, nc, stack, n_dma_sems=32):
        self.nc = nc
        def mk(n):
            s_ = Sem(stack.enter_context(nc.semaphore(n)))
            s_.name = n
            return s_
        self.pe = Eng("pe", nc.tensor, mk("s_pe"), same_wait=False)
        self.act = Eng("act", nc.scalar, mk("s_act"))
        self.dve = Eng("dve", nc.vector, mk("s_dve"))
        self.pool = Eng("pool", nc.gpsimd, mk("s_pool"))
        self.sp = Eng("sp", nc.sync, mk("s_sp"))
        self.engs = [self.pe, self.act, self.dve, self.pool, self.sp]
        self.dma_sems = [mk(f"s_dma{i}") for i in range(n_dma_sems)]
        self.dma_i = 0
        self.dma_qi = [0, 0]
        self.n_instr = 0
        self.pe_pending = False

    def _wait(self, eng, tok):
        sem, val = tok
        if sem is eng.sem and not eng.same_wait:
            return
        if eng.waited.get(id(sem), 0) >= val:
            return
        eng.obj.wait_ge(sem.h, val)
        eng.waited[id(sem)] = val
        if TRACE:
            print("   wait", eng.name, "on", getattr(sem, "name", "?"), val)

    def _deps(self, eng, reads, writes):
        for b in reads:
            if b.last_w is not None:
                self._wait(eng, b.last_w)
            if b.excl:
                for t in b.readers:
                    if t[0] is not eng.sem:
                        self._wait(eng, t)
        for b in writes:
            if b.last_w is not None:
                self._wait(eng, b.last_w)
            for t in b.readers:
                self._wait(eng, t)

    @staticmethod
    def _commit(tok, reads, writes):
        for b in reads:
            if len(b.readers) > 24:
                best = {}
                for s_, v_ in b.readers:
                    if best.get(id(s_), (None, -1))[1] < v_:
                        best[id(s_)] = (s_, v_)
                b.readers = list(best.values())
            b.readers.append(tok)
        for b in writes:
            b.last_w = tok
            b.readers = []

    def op(self, eng, fn, reads=(), writes=(), inc=True):
        if TRACE:
            print("op", eng.name, "cnt", eng.sem.cnt, "reads", [b.name for b in reads], "writes", [b.name for b in writes], "inc", inc)
        self._deps(eng, reads, writes)
        ins = fn()
        self.n_instr += 1
        if inc:
            eng.sem.cnt += 1
            ins.then_inc(eng.sem.h, 1)
            tok = (eng.sem, eng.sem.cnt)
            if eng is self.pe:
                self.pe_pending = False
        else:
            assert eng is self.pe
            tok = (eng.sem, eng.sem.cnt + 1)
            self.pe_pending = True
        self._commit(tok, reads, writes)
        return ins

    def dma(self, q, out, in_, reads=(), writes=(), **kw):
        half = len(self.dma_sems) // 2
        k = 0 if q is self.sp else 1
        sem = self.dma_sems[k * half + self.dma_qi[k] % half]
        self.dma_qi[k] += 1
        self.dma_i += 1
        if sem.cnt:
            self._wait(q, (sem, sem.cnt))
        self._deps(q, reads, writes)
        ins = q.obj.dma_start(out=out, in_=in_, **kw)
        sem.cnt += 16
        ins.then_inc(sem.h, 16)
        self.n_instr += 1
        tok = (sem, sem.cnt)
        self._commit(tok, reads, writes)
        return tok

    def barrier(self):
        assert not self.pe_pending
        for e in self.engs:
            for o in self.engs:
                if o is not e and o.sem.cnt:
                    self._wait(e, (o.sem, o.sem.cnt))
            for s in self.dma_sems:
                if s.cnt:
                    self._wait(e, (s, s.cnt))


class Ring:
    def __init__(self, aps, name="r"):
        self.items = [(ap, Buf(f"{name}{i}")) for i, ap in enumerate(aps)]
        self.i = 0

    def next(self):
        it = self.items[self.i % len(self.items)]
        self.i += 1
        return it


class Prog:
    def __init__(self, stop=None, taps=()):
        self.stop = stop
        self.taps = set(taps)
        self.dbg_specs = {}

    def build(self):
        nc = bass.Bass("TRN2", target_bir_lowering=False)
        self.nc = nc
        di = lambda n, s: nc.dram_tensor(n, list(s), F32, kind="ExternalInput").ap()
        self.x_d = di("x", [S, D])
        self.router_w = di("router_w", [D, NE])
        self.win = [di("w_in0", [D, 2560]), di("w_in1", [D, 2048])]
        self.wout = [di("w_out0", [D, D]), di("w_out1", [D, D])]
        self.wg = [di("w_gate0", [NE, D, 512]), di("w_gate1", [NE, D, 512])]
        self.wu = [di("w_up0", [NE, D, 512]), di("w_up1", [NE, D, 512])]
        self.wd = [di("w_down0", [NE, 512, D]), di("w_down1", [NE, 512, D])]
        self.pool_w = di("pool_w", [4, 128, 128])
        self.pp_d = di("pp", [128, 144])
        self.rowp_d = di("rowp", [272])
        self.lnp_d = di("lnp", [8, D])
        self.cst_d = di("cst", [128, 128 * 5 + 64])
        self.rope_d = di("rope", [2, 128, S])
        self.out_d = nc.dram_tensor("out", [S, D], F32, kind="ExternalOutput").ap()
        self.spill_d = nc.dram_tensor("xspill", [S, D], F32, kind="Internal").ap()
        with contextlib.ExitStack() as st:
            self.st = st
            self.fw = FW(nc, st)
            self._alloc()
            self._run()
        return nc

    def _alloc(self):
        nc, st = self.nc, self.st
        sb = lambda n, s, d: st.enter_context(nc.sbuf_tensor("sb_" + n, s, d))
        self.XT = sb("xt", [128, 8 * S], BF16)[:].rearrange("p (c t) -> p c t", c=8)
        self.XTb = [Buf(f"xt{t}") for t in range(NT)]
        bigf = sb("big", [128, 16640], F32)[:]
        self.X = bigf[:, 0:16384].rearrange("p (t d) -> p t d", t=NT)
        self.Xb = [Buf(f"x{t}") for t in range(NT)]
        bigb = bigf.bitcast(BF16)
        self.QK = bigb[:, 0:16384].rearrange("p (c t) -> p c t", c=8)
        self.QKb = [[Buf(f"qk{c}_{tb}") for tb in range(4)] for c in range(8)]
        self.V = bigb[:, 16384:24576].rearrange("p (t f) -> p t f", t=NT)
        self.Vb = [Buf(f"v{t}") for t in range(NT)]
        self.AU = bigb[:, 24576:32896].rearrange("p (c t) -> p c t", c=4)
        self.AUb = [Buf(f"au{c}") for c in range(4)]
        wb = sb("w", [128, 24576], BF16)[:]
        self.Wblk = [wb[:, i * 4096:(i + 1) * 4096] for i in range(6)]
        self.Wb = [Buf(f"w{i}") for i in range(6)]
        scr = sb("scr", [128, 4096], F32)[:]
        self.SCR = [scr[:, 0:2048], scr[:, 2048:4096]]
        self.SCRb = [Buf("scrA"), Buf("scrB")]
        tf = sb("tmpf", [128, 8 * 512], F32)[:]
        self.tmpf = Ring([tf[:, i * 512:(i + 1) * 512] for i in range(8)], "tf")
        tb_ = sb("tmpb", [128, 10 * 512], BF16)[:]
        self.tmpb = Ring([tb_[:, i * 512:(i + 1) * 512] for i in range(6)], "tb")
        self.tmpb2 = Ring([tb_[:, i * 512:(i + 1) * 512] for i in range(6, 10)], "tb2")
        self.tf_full, self.tb_full = tf, tb_
        self.ht = sb("ht", [128, 2 * 2048], BF16)[:]
        self.htr = Ring([self.ht[:, i * 2048:(i + 1) * 2048].rearrange("p (c t) -> p c t", c=4) for i in range(2)], "ht")
        self.cstf = sb("cstf", [128, 128 * 5 + 64], F32)[:]
        self.cstb = sb("cstb", [128, 128 * 7], BF16)[:]
        self.Bc = Buf("const")
        self.pp = sb("pp", [128, 144], F32)[:]
        self.rowp = sb("rowp", [128, 272], F32)[:]
        self.small = sb("small", [128, 64], F32)[:]
        self.Bsmall = Buf("small")
        self.comb = sb("comb", [128, NT * NE], F32)[:]
        self.Bcomb = Buf("comb")
        self.zeros_b = sb("zeros", [128, 128], BF16)[:]
        self.rt_halves = [scr[0:16, 1024:2048], scr[0:16, 3072:4096]]
        self.lgt = scr[:, 1024:1024 + NT * NE]
        self.Brt = Buf("rt")
        stt = sb("stats", [128, 4 * 16], F32)[:]
        self.statr = Ring([stt[:, i * 16:(i + 1) * 16] for i in range(4)], "stat")
        self.wr = sb("wr", [128, 8 * NE], F32)[:].rearrange("p (c e) -> p c e", c=8)
        self.pwb = sb("pwb", [128, 4 * 128], BF16)[:].rearrange("p (g d) -> p g d", g=4)
        self.PS = []
        self.PSb = []
        self.PSpair = []
        for k in range(4):
            pair = st.enter_context(nc.psum_tensor(f"pp{k}", [128, 1024], F32))[:]
            self.PSpair.append(pair)
            for h in range(2):
                self.PS.append(pair[:, h * 512:(h + 1) * 512])
                self.PSb.append(Buf(f"ps{2 * k + h}", excl=True))

    def psring(self, idxs):
        r = Ring([self.PS[i] for i in idxs])
        r.items = [(self.PS[i], self.PSb[i]) for i in idxs]
        return r

    def mm(self, out, lhsT, rhs, start, stop, reads, writes, inc=None):
        nc = self.nc
        inc = stop if inc is None else inc
        self.fw.op(self.fw.pe, lambda: nc.tensor.matmul(out, lhsT, rhs, start=start, stop=stop), reads, writes, inc=inc)

    def tr(self, out, in_, ident, reads, writes, inc=True):
        nc = self.nc
        self.fw.op(self.fw.pe, lambda: nc.tensor.transpose(out, in_, ident), reads, writes, inc=inc)

    def act(self, out, in_, func, reads, writes, **kw):
        nc = self.nc
        self.fw.op(self.fw.act, lambda: nc.scalar.activation(out, in_, func, **kw), reads, writes)

    def dve(self, fn, reads, writes):
        self.fw.op(self.fw.dve, fn, reads, writes)

    def pool(self, fn, reads, writes):
        self.fw.op(self.fw.pool, fn, reads, writes)

    def tap(self, name, ap, bufs, dtype=F32):
        if name not in self.taps:
            return
        nc = self.nc
        shape = list(ap.shape)
        d = nc.dram_tensor("dbg_" + name, shape, dtype, kind="ExternalOutput").ap()
        self.dbg_specs[name] = (shape, dtype)
        self.fw.barrier()
        self.fw.dma(self.fw.sp, d, ap, reads=bufs)
        self.fw.barrier()

    def _run(self):
        self._consts()
        for L in range(2):
            if L == 0:
                self._xt_from_dram()
            else:
                self._xt_from_x(router=False, spill=True)
                self.fw.barrier()
            if self.stop == f"l{L}_xt":
                return self._finish()
            self._in_proj(L)
            if self.stop in (f"l{L}_inproj", "l0_glu", "l0_qk"):
                return self._finish()
            self._load_wout(L)
            if L == 0:
                if self.stop == "l0_conv":
                    self._conv_branch()
                    return self._finish()
                self._diff_attn(self._conv_pe())
                self._conv_ln()
            else:
                self._pool_mix()
                self._sb_attn()
            if self.stop == f"l{L}_mix":
                return self._finish()
            self.fw.barrier()
            W0 = self._mid(L)
            self._routing()
            if self.stop == f"l{L}_route":
                return self._finish()
            self._experts(L, W0)
            self._final_ln(L)
            if self.stop == f"l{L}_moe":
                return self._finish()
        self._finish()

    def _finish(self):
        self.fw.barrier()
        allb = self.XTb + self.Xb + self.Vb + self.AUb + [b for r in self.QKb for b in r] + [self.Bcomb, self.Brt, self.Bsmall]
        self.tap("XT", self.XT, allb, BF16)
        self.tap("QK", self.QK, allb, BF16)
        self.tap("V", self.V, allb, BF16)
        self.tap("AU", self.AU, allb, BF16)
        self.tap("X", self.X, allb, F32)
        self.tap("comb", self.comb, allb, F32)
        self.tap("small", self.small, allb, F32)
        self.fw.barrier()

    def _consts(self):
        nc, fw = self.nc, self.fw
        fw.dma(fw.sp, self.cstf, self.cst_d[:, :], writes=[self.Bc])
        fw.dma(fw.sp, self.pp, self.pp_d[:, :], writes=[self.Bc])
        fw.dma(fw.sp, self.rowp, self.rowp_d.partition_broadcast(128), writes=[self.Bc])
        fw.dma(fw.sp, self.wr, self.router_w.rearrange("(c p) e -> p c e", p=128), writes=[self.Bc])
        fw.dma(fw.pool, self.pwb, self.pool_w.rearrange("g c d -> c g d"), writes=[self.Bc])
        self.dve(lambda: nc.vector.tensor_copy(self.cstb[:, 0:640], self.cstf[:, 0:640]), [self.Bc], [self.Bc])
        self.dve(lambda: nc.vector.memset(self.cstb[:, 640:768], 1.0), [], [self.Bc])
        self.dve(lambda: nc.vector.tensor_scalar(self.cstb[:, 768:896], self.cstf[:, 128:256], -8.0, None, ALU.mult), [self.Bc], [self.Bc])
        self.ident_f = self.cstf[:, 0:128]
        self.ident_b = self.cstb[:, 0:128]
        self.tri_b = self.cstb[:, 128:256]
        self.mle_b = self.cstb[:, 256:384]
        self.mlt_b = self.cstb[:, 384:512]
        self.pm_b = self.cstb[:, 512:640]
        self.ones_b = self.cstb[:, 640:768]
        self.ntri_b = self.cstb[:, 768:896]
        self.invc = self.cstf[:, 640:704].rearrange("p (g t) -> p g t", g=4)
        self.cw = self.pp[:, 0:124].rearrange("p (c w) -> p c w", c=4)
        self.conv_b = self.pp[:, 124:128]
        self.cln_g = self.pp[:, 128:132]
        self.cln_b = self.pp[:, 132:136]
        self.subln = self.pp[:, 136:137]
        self.pscale = self.pp[:, 137:141]
        sm = self.small
        lam4 = self.rowp[:, 0:256].rearrange("p (a d) -> p a d", a=4)
        tmp, tmpb_ = self.tmpf.next()
        self.dve(lambda: nc.vector.tensor_tensor(tmp[:, 0:128].rearrange("p (a d) -> p a d", a=2), lam4[:, 0:2, :], lam4[:, 2:4, :], ALU.mult), [self.Bc], [tmpb_])
        self.dve(lambda: nc.vector.tensor_reduce(sm[:, 2:4], tmp[:, 0:128].rearrange("p (a d) -> p a d", a=2), AX.X, ALU.add), [tmpb_], [self.Bsmall])
        self.act(sm[:, 4:6], sm[:, 2:4], AF.Exp, [self.Bsmall], [self.Bsmall])
        self.dve(lambda: nc.vector.tensor_tensor(sm[:, 6:7], sm[:, 5:6], sm[:, 4:5], ALU.subtract), [self.Bsmall], [self.Bsmall])
        self.dve(lambda: nc.vector.tensor_scalar(sm[:, 0:1], sm[:, 6:7], -LAM_INIT0, None, ALU.add), [self.Bsmall], [self.Bsmall])
        self.dve(lambda: nc.vector.tensor_scalar(sm[:, 1:2], self.subln, 1.0 - LAM_INIT0, None, ALU.mult), [self.Bc], [self.Bsmall])
        self.neglam = sm[:, 0:1]
        self.gsc = sm[:, 1:2]
        self.rbias = self.rowp[:, 256:272]

    def _xt_from_dram(self):
        nc, fw = self.nc, self.fw
        psr = self.psring([0, 1, 2, 3])
        k = 0
        for tt in range(NT):
            half = tt % 2
            for hh in range(2):
                pass
            xt_ap = self.SCR[half][:, 0:1024]
            fw.dma(fw.sp, xt_ap, self.x_d[tt * 128:(tt + 1) * 128, :], writes=[self.SCRb[half]])
            for dg in range(2):
                ps, psb = psr.next()
                for j in range(4):
                    dc = dg * 4 + j
                    self.tr(ps[:, j * 128:(j + 1) * 128], xt_ap[:, dc * 128:(dc + 1) * 128], self.ident_f,
                            [self.SCRb[half], self.Bc], [psb], inc=(j == 3))
                dst = self.XT[:, dg * 4:(dg + 1) * 4, tt * 128:(tt + 1) * 128]
                src = ps.rearrange("p (c t) -> p c t", c=4)
                if k % 2 == 0:
                    self.act(dst, src, AF.Copy, [psb], [self.XTb[tt]])
                else:
                    self.dve(lambda: nc.vector.tensor_copy(dst, src), [psb], [self.XTb[tt]])
                k += 1

    def _xt_from_x(self, router, spill, inv=1.0):
        nc, fw = self.nc, self.fw
        psr = self.psring([0, 1])
        lg_ps, lg_b = self.PS[2], self.PSb[2]
        if spill:
            for tt in range(NT):
                fw.dma(fw.sp, self.spill_d[tt * 128:(tt + 1) * 128, :], self.X[:, tt, :], reads=[self.Xb[tt]])
        for tb in range(4):
            for dc in range(8):
                ps, psb = psr.next()
                for j in range(4):
                    tt = tb * 4 + j
                    self.tr(ps[:, j * 128:(j + 1) * 128], self.X[:, tt, dc * 128:(dc + 1) * 128], self.ident_f,
                            [self.Xb[tt], self.Bc], [psb], inc=(j == 3))
                dst = self.XT[:, dc, tb * 512:(tb + 1) * 512]
                self.act(dst, ps, AF.Copy, [psb], self.XTb[tb * 4:tb * 4 + 4], scale=inv)
                if router:
                    xf, xfb = self.tmpf.next()
                    self.act(xf, ps, AF.Copy, [psb], [xfb], scale=inv)
                    self.mm(lg_ps[0:16, :], self.wr[:, dc, :], xf, dc == 0, dc == 7, [self.Bc, xfb], [lg_b])
            if router:
                self.dve(lambda: nc.vector.tensor_copy(self.rt_halves[tb // 2][:, (tb % 2) * 512:(tb % 2 + 1) * 512], lg_ps[0:16, :]), [lg_b], [self.Brt])

    @staticmethod
    def _win_slot(L):
        return {0: 0, 1: 1, 2: 2, 3: 3, 4: 4} if L == 0 else {3: 0, 0: 1, 1: 2, 2: 3}

    def _load_w_in(self, L, g, slot):
        fw = self.fw
        src = self.win[L].rearrange("(c p) n -> p c n", p=128)[:, :, g * 512:(g + 1) * 512]
        dst = self.Wblk[slot].rearrange("p (c n) -> p c n", c=8)
        fw.dma(fw.pool, dst, src, writes=[self.Wb[slot]])
        return dst

    def _proj_fm(self, W, wbuf, c, tb, ps, psb):
        for dc in range(8):
            self.mm(ps, W[:, dc, c * 128:(c + 1) * 128], self.XT[:, dc, tb * 512:(tb + 1) * 512], dc == 0, dc == 7,
                    [wbuf] + self.XTb[tb * 4:tb * 4 + 4], [psb])

    def _proj_v(self, W, wbuf):
        nc = self.nc
        psr = self.psring([0, 1, 2, 3])
        for tt in range(NT):
            ps, psb = psr.next()
            for dc in range(8):
                self.mm(ps, self.XT[:, dc, tt * 128:(tt + 1) * 128], W[:, dc, :], dc == 0, dc == 7, [wbuf, self.XTb[tt]], [psb])
            if tt % 2 == 0:
                self.act(self.V[:, tt, :], ps, AF.Copy, [psb], [self.Vb[tt]])
            else:
                self.dve(lambda: nc.vector.tensor_copy(self.V[:, tt, :], ps), [psb], [self.Vb[tt]])

    def _in_proj(self, L):
        nc, fw = self.nc, self.fw
        ng = 5 if L == 0 else 4
        pref = getattr(self, "pref_win", {}) if L == 1 else {}
        slot_of = self._win_slot(L)
        Ws = [pref[g] if g in pref else self._load_w_in(L, g, slot_of[g]) for g in range(ng)]
        Wbs = [self.Wb[slot_of[g]] for g in range(ng)]
        psr = self.psring([0, 1, 2, 3])
        if L == 0:
            fw.dma(fw.sp, self.SCR[0], self.rope_d[0], writes=[self.SCRb[0]])
            fw.dma(fw.sp, self.SCR[1], self.rope_d[1], writes=[self.SCRb[1]])
            for c in range(4):
                self.pool(lambda: nc.gpsimd.memset(self.AU[:, c, 0:32], 0.0), [], [self.AUb[c]])
            for c in range(4):
                for tb in range(4):
                    pv, pvb = psr.next()
                    pg, pgb = psr.next()
                    self._proj_fm(Ws[0], self.Wb[0], c, tb, pv, pvb)
                    self._proj_fm(Ws[1], self.Wb[1], c, tb, pg, pgb)
                    sg, sgb = self.tmpf.next()
                    self.act(sg, pg, AF.Sigmoid, [pgb], [sgb])
                    dst = self.AU[:, c, 32 + tb * 512:32 + (tb + 1) * 512]
                    self.dve(lambda: nc.vector.tensor_tensor(dst, pv, sg, ALU.mult), [pvb, sgb], [self.AUb[c]])
            if self.stop == "l0_glu":
                return
            self._conv_diag()
            pmr = self.psring([4, 5])
            pending = None

            import os
            KD = int(os.environ.get("KDBG", "0"))

            def rope_tail(item):
                c8, tb, p1, p1b, qb, qbb = item
                if KD == 1:
                    return
                if KD == 2:
                    p2, p2b = pmr.next()
                    self.mm(p2, self.pm_b, qb, True, True, [self.Bc, qbb], [p2b])
                    return
                if KD == 3:
                    t1, t1b = self.tmpf.next()
                    sl = slice(tb * 512, (tb + 1) * 512)
                    self.dve(lambda: nc.vector.tensor_tensor(t1, p1, self.SCR[0][:, sl], ALU.mult), [p1b, self.SCRb[0]], [t1b])
                    return
                if KD == 6:
                    t1, t1b = self.tmpf.next()
                    self.dve(lambda: nc.vector.tensor_tensor(t1, self.cstf[:, 0:512], self.cstf[:, 0:512], ALU.mult), [self.Bc], [t1b])
                    return
                if KD == 7:
                    t1, t1b = self.tmpf.next()
                    self.dve(lambda: nc.vector.tensor_copy(t1, p1), [p1b], [t1b])
                    return
                if KD == 8:
                    t1, t1b = self.tmpf.next()
                    self.dve(lambda: nc.vector.tensor_tensor(qb, p1, self.cstf[:, 0:512], ALU.mult), [p1b, self.Bc], [qbb])
                    return
                if KD == 4:
                    t1, t1b = self.tmpf.next()
                    self.dve(lambda: nc.vector.tensor_tensor(t1, p1, self.cstf[:, 0:512], ALU.mult), [p1b, self.Bc], [t1b])
                    return
                if KD == 5:
                    t1, t1b = self.tmpf.next()
                    sl = slice(tb * 512, (tb + 1) * 512)
                    self.dve(lambda: nc.vector.tensor_tensor(t1, p1, self.SCR[0][:, sl], ALU.mult), [p1b, self.SCRb[0]], [t1b])
                    if c8 == 0 and tb == 1:
                        raise StopIteration
                    return
                p2, p2b = pmr.next()
                self.mm(p2, self.pm_b, qb, True, True, [self.Bc, qbb], [p2b])
                t1, t1b = self.tmpf.next()
                t2, t2b = self.tmpf.next()
                sl = slice(tb * 512, (tb + 1) * 512)
                self.dve(lambda: nc.vector.tensor_tensor(t1, p1, self.SCR[0][:, sl], ALU.mult), [p1b, self.SCRb[0]], [t1b])
                self.dve(lambda: nc.vector.tensor_tensor(t2, p2, self.SCR[1][:, sl], ALU.mult), [p2b, self.SCRb[1]], [t2b])
                self.dve(lambda: nc.vector.tensor_tensor(self.QK[:, c8, sl], t1, t2, ALU.add), [t1b, t2b], [self.QKb[c8][tb]])

            try:
                for g in (2, 3):
                    for c in range(4):
                        for tb in range(4):
                            p1, p1b = psr.next()
                            self._proj_fm(Ws[g], self.Wb[g], c, tb, p1, p1b)
                            qb, qbb = self.tmpb.next()
                            self.act(qb, p1, AF.Copy, [p1b], [qbb])
                            if pending is not None:
                                rope_tail(pending)
                            pending = ((g - 2) * 4 + c, tb, p1, p1b, qb, qbb)
                rope_tail(pending)
            except StopIteration:
                pass
            if self.stop == "l0_qk":
                return
            self._proj_v(Ws[4], self.Wb[4])
        else:
            for c in range(4):
                self.pool(lambda: nc.gpsimd.memset(self.AU[:, c, 0:16], 0.0), [], [self.AUb[c]])
            for c in range(4):
                for tb in range(4):
                    p1, p1b = psr.next()
                    self._proj_fm(Ws[3], Wbs[3], c, tb, p1, p1b)
                    self.act(self.AU[:, c, 16 + tb * 512:16 + (tb + 1) * 512], p1, AF.Copy, [p1b], [self.AUb[c]])
            self._pool_prefix()
            for g in (0, 1):
                for c in range(4):
                    for tb in range(4):
                        p1, p1b = psr.next()
                        self._proj_fm(Ws[g], Wbs[g], c, tb, p1, p1b)
                        self.act(self.QK[:, g * 4 + c, tb * 512:(tb + 1) * 512], p1, AF.Copy, [p1b], [self.QKb[g * 4 + c][tb]])
            self._proj_v(Ws[2], Wbs[2])

    def _conv_diag(self):
        nc = self.nc
        homes = [(self.Wblk[0], [self.Wb[0]]), (self.Wblk[1], [self.Wb[1]]), (self.Wblk[5], [self.Wb[5]]),
                 (self.ht, [self.htr.items[0][1], self.htr.items[1][1]])]
        self.Dg = []
        for c in range(4):
            home, bufs = homes[c]
            D3 = home[:, 0:31 * 128].rearrange("p (w j) -> p w j", w=31)
            self.dve(lambda: nc.vector.tensor_tensor(D3, self.ident_b.unsqueeze(1).broadcast_to([128, 31, 128]),
                                                     self.cw[:, c, :].unsqueeze(2).broadcast_to([128, 31, 128]), ALU.mult),
                     [self.Bc], bufs)
            self.Dg.append((D3, bufs))

    def _conv_pe(self):
        MIX = self.XT
        ps, psb = self.PS[3], self.PSb[3]
        for c in range(4):
            D3, dbufs = self.Dg[c]
            for tb in range(4):
                for w in range(31):
                    o = 2 + w + tb * 512
                    self.mm(ps, D3[:, w, :], self.AU[:, c, o:o + 512], w == 0, w == 30, dbufs + [self.AUb[c]], [psb])
                    yield
                self.act(MIX[:, c, tb * 512:(tb + 1) * 512], ps, AF.Identity, [psb, self.Bc], self.XTb[tb * 4:tb * 4 + 4],
                         bias=self.conv_b[:, c:c + 1])
                yield

    def _conv_ln(self):
        nc = self.nc
        MIX = self.XT
        sls = [slice(tb * 512, (tb + 1) * 512) for tb in range(4)]
        for tb in range(4):
            sl = sls[tb]
            xb4 = self.XTb[tb * 4:tb * 4 + 4]
            sum_ps, sum_b = self.PS[tb], self.PSb[tb]
            sq_ps, sq_b = self.PS[4 + tb], self.PSb[4 + tb]
            for c in range(4):
                sq, sqb = self.tmpb.next()
                self.act(sq, MIX[:, c, sl], AF.Square, xb4, [sqb])
                self.mm(sum_ps, self.ones_b, MIX[:, c, sl], c == 0, c == 3, [self.Bc] + xb4, [sum_b])
                self.mm(sq_ps, self.ones_b, sq, c == 0, c == 3, [self.Bc, sqb], [sq_b])
        for tb in range(4):
            sl = sls[tb]
            mean, var = self.SCR[0][:, sl], self.SCR[1][:, sl]
            self.act(mean, self.PS[tb], AF.Copy, [self.PSb[tb]], [self.SCRb[0]], scale=1.0 / 512)
            nmsq, nmsqb = self.tmpf.next()
            self.dve(lambda: nc.vector.scalar_tensor_tensor(nmsq, mean, -1.0, mean, ALU.mult, ALU.mult), [self.SCRb[0]], [nmsqb])
            self.dve(lambda: nc.vector.scalar_tensor_tensor(var, self.PS[4 + tb], 1.0 / 512, nmsq, ALU.mult, ALU.add), [self.PSb[4 + tb], nmsqb], [self.SCRb[1]])
            self.dve(lambda: nc.vector.tensor_scalar(var, var, LN_EPS, None, ALU.add), [self.SCRb[1]], [self.SCRb[1]])
            self.act(var, var, AF.Ln, [self.SCRb[1]], [self.SCRb[1]])
            self.act(var, var, AF.Exp, [self.SCRb[1]], [self.SCRb[1]], scale=-0.5)
        for tb in range(4):
            sl = sls[tb]
            xb4 = self.XTb[tb * 4:tb * 4 + 4]
            mean, var = self.SCR[0][:, sl], self.SCR[1][:, sl]
            for c in range(4):
                d, db = self.tmpf.next()
                self.dve(lambda: nc.vector.tensor_tensor(d, MIX[:, c, sl], mean, ALU.subtract), xb4 + [self.SCRb[0]], [db])
                self.dve(lambda: nc.vector.tensor_tensor(d, d, var, ALU.mult), [db, self.SCRb[1]], [db])
                self.act(MIX[:, c, sl], d, AF.Silu, [db, self.Bc], xb4, scale=self.cln_g[:, c:c + 1], bias=self.cln_b[:, c:c + 1])

    def _conv_branch(self):
        for _ in self._conv_pe():
            pass
        self._conv_ln()

    def _diff_attn(self, side=None):
        nc = self.nc
        MIX = self.XT
        QK, V = self.QK, self.V
        scr = self.psring([0, 1, 2])
        O = [(self.PS[4], self.PSb[4]), (self.PS[6], self.PSb[6])]

        def pull(n):
            if side is not None:
                for _ in range(n):
                    next(side, None)
        Dn = [(self.PS[5], self.PSb[5]), (self.PS[7], self.PSb[7])]
        deferred = []
        for h in range(4):
            for qb in range(4):
                nkt = 4 * (qb + 1)
                q0 = qb * 512
                steps = [(kt, j) for kt in range(nkt) for j in range(2)]

                def stage_a(kt, j):
                    i = kt - 4 * qb
                    c0 = 128 * i if i > 0 else 0
                    sp_, spb = scr.next()
                    pr = slice(64 * j, 64 * j + 64)
                    self.mm(sp_[:, c0:512], QK[pr, 4 + h, kt * 128:(kt + 1) * 128], QK[pr, h, q0 + c0:q0 + 512], True, True,
                            [self.QKb[4 + h][kt // 4], self.QKb[h][qb]], [spb])
                    pt, ptb = self.tmpb.next()
                    self.act(pt[:, c0:512], sp_[:, c0:512], AF.Exp, [spb], [ptb], scale=0.125)
                    if i >= 0:
                        self.pool(lambda: nc.gpsimd.tensor_tensor(pt[:, c0:c0 + 128], pt[:, c0:c0 + 128], self.mle_b, ALU.mult), [ptb, self.Bc], [ptb])
                    return (kt, j, c0, pt, ptb)

                def stage_b(kt, j, c0, pt, ptb):
                    self.mm(O[j][0][:, c0:512], V[:, kt, h * 128:(h + 1) * 128], pt[:, c0:512], kt == 0, kt == nkt - 1,
                            [self.Vb[kt], ptb], [O[j][1]])
                    self.mm(Dn[j][0][:, c0:512], self.ones_b, pt[:, c0:512], kt == 0, kt == nkt - 1, [self.Bc, ptb], [Dn[j][1]])

                pend = []
                for si, (kt, j) in enumerate(steps):
                    pend.append(stage_a(kt, j))
                    if len(pend) > 2:
                        stage_b(*pend.pop(0))
                    pull(1)
                    while deferred and deferred[0][0] <= si:
                        deferred.pop(0)[1]()
                while pend:
                    stage_b(*pend.pop(0))
                while deferred:
                    deferred.pop(0)[1]()
                os_ = []
                for j in range(2):
                    r, rb = self.tmpf.next()
                    self.act(r, Dn[j][0], AF.Ln, [Dn[j][1]], [rb])
                    self.act(r, r, AF.Exp, [rb], [rb], scale=-1.0)
                    o, ob = self.tmpf.next()
                    self.dve(lambda: nc.vector.tensor_tensor(o, O[j][0], r, ALU.mult), [O[j][1], rb], [ob])
                    os_.append((o, ob))
                (o1, o1b), (o2, o2b) = os_
                self.dve(lambda: nc.vector.scalar_tensor_tensor(o1, o2, self.neglam, o1, ALU.mult, ALU.add), [o2b, o1b, self.Bsmall], [o1b])
                st = {}

                def fin_a(o1=o1, o1b=o1b, st=st):
                    st["osq"], st["osqb"] = self.tmpb.next()
                    self.act(st["osq"], o1, AF.Square, [o1b], [st["osqb"]])

                def fin_b(st=st):
                    ss, ssb = scr.next()
                    self.mm(ss, self.ones_b, st["osq"], True, True, [self.Bc, st["osqb"]], [ssb])
                    st["rs"], st["rsb"] = self.tmpf.next()
                    rs = st["rs"]
                    self.dve(lambda: nc.vector.tensor_scalar(rs, ss, 1.0 / 128, RMS_EPS, ALU.mult, ALU.add), [ssb], [st["rsb"]])

                def fin_c(o1=o1, o1b=o1b, st=st, h=h, q0=q0, qb=qb):
                    rs, rsb = st["rs"], st["rsb"]
                    self.act(rs, rs, AF.Ln, [rsb], [rsb])
                    self.act(rs, rs, AF.Exp, [rsb], [rsb], scale=-0.5)
                    self.dve(lambda: nc.vector.scalar_tensor_tensor(MIX[:, 4 + h, q0:q0 + 512], o1, self.gsc, rs, ALU.mult, ALU.mult),
                             [o1b, rsb, self.Bsmall], self.XTb[qb * 4:qb * 4 + 4])

                deferred = [(2, fin_a), (4, fin_b), (6, fin_c)]
                pull(12)
        while deferred:
            deferred.pop(0)[1]()
        if side is not None:
            for _ in side:
                pass

    def _pool_prefix(self):
        nc = self.nc
        for g in range(4):
            U = self.AU[:, g, :]
            bufs = [(self.SCR[0], self.SCRb[0]), (self.SCR[1], self.SCRb[1])]
            cur, curb = bufs[0]
            self.dve(lambda: nc.vector.tensor_tensor(cur, U[:, 16:16 + S], U[:, 15:15 + S], ALU.add), [self.AUb[g]], [curb])
            k = 0
            sh = 2
            while sh < POOL_WINDOWS[g]:
                nxt, nxtb = bufs[(k + 1) % 2]
                self.dve(lambda: nc.vector.tensor_tensor(nxt[:, sh:S], cur[:, sh:S], cur[:, 0:S - sh], ALU.add), [curb], [nxtb])
                self.pool(lambda: nc.gpsimd.tensor_copy(nxt[:, 0:sh], cur[:, 0:sh]), [curb], [nxtb])
                cur, curb = nxt, nxtb
                k += 1
                sh *= 2
            win = POOL_WINDOWS[g]
            t, tb_ = self.tmpf.next()
            self.dve(lambda: nc.vector.tensor_tensor(t[:, 0:16], cur[:, 0:16], self.invc[:, g, :], ALU.mult), [curb, self.Bc], [tb_])
            self.dve(lambda: nc.vector.tensor_tensor(t[:, 0:16], t[:, 0:16], U[:, 16:32], ALU.subtract), [tb_, self.AUb[g]], [tb_])
            for tb in range(4):
                sl = slice(tb * 512, (tb + 1) * 512)
                usl = U[:, 16 + tb * 512:16 + (tb + 1) * 512]
                self.dve(lambda: nc.vector.scalar_tensor_tensor(usl, cur[:, sl], 1.0 / win, usl, ALU.mult, ALU.subtract),
                         [curb, self.AUb[g]], [self.AUb[g]])
            self.dve(lambda: nc.vector.tensor_copy(U[:, 16:32], t[:, 0:16]), [tb_], [self.AUb[g]])

    def _pool_mix(self):
        nc = self.nc
        MIX = self.XT
        pr = self.psring([4, 5, 6, 7])
        for g in range(4):
            for tb in range(4):
                sl = slice(tb * 512, (tb + 1) * 512)
                pm_ps, pm_b = pr.next()
                self.mm(pm_ps, self.pwb[:, g, :], self.AU[:, g, 16 + tb * 512:16 + (tb + 1) * 512], True, True, [self.Bc, self.AUb[g]], [pm_b])
                self.dve(lambda: nc.vector.tensor_scalar(MIX[:, 4 + g, sl], pm_ps, self.pscale[:, g:g + 1], None, ALU.mult),
                         [pm_b, self.Bc], self.XTb[tb * 4:tb * 4 + 4])

    def _sb_attn(self):
        nc = self.nc
        MIX = self.XT
        QK, V = self.QK, self.V
        self.fw.barrier()
        tf, tb_ = self.tf_full, self.tb_full
        fpr = Ring([tf[:, i * 1024:(i + 1) * 1024] for i in range(4)], "tfp")
        spr = Ring([tb_[:, i * 1024:(i + 1) * 1024] for i in range(3)], "tbp")
        apr = Ring([tb_[:, 3072 + i * 1024:3072 + (i + 1) * 1024] for i in range(2)], "tap")
        rpr = Ring([self.SCR[i // 2][:, (i % 2) * 1024:(i % 2 + 1) * 1024] for i in range(4)], "trp")
        zpairs = [(self.PSpair[0], [self.PSb[0], self.PSb[1]]), (self.PSpair[1], [self.PSb[2], self.PSb[3]])]
        zi = [0]
        Rp, Rb = self.PSpair[2], [self.PSb[4], self.PSb[5]]
        Or = self.psring([6])
        zeros = self.zeros_b
        self.pool(lambda: nc.gpsimd.memset(zeros, 0.0), [], [self.Bsmall])
        w = lambda ap, c0: ap.rearrange("p (h q) -> p h q", h=2)[:, :, c0:512]
        hv = lambda ap, hh: ap[:, hh * 512:(hh + 1) * 512]
        for c in range(4):
            for qb in range(4):
                q0 = qb * 512
                nkt = 4 * (qb + 1)
                o_ps, o_b = Or.next()
                for hh in range(2):
                    self.mm(hv(Rp, hh), zeros, QK[:, c, q0:q0 + 512], True, True, [self.Bsmall, self.QKb[c][qb]], [Rb[hh]], inc=True)
                kts = list(range(nkt - 1, -1, -1))

                def a_pe(idx, kt):
                    i = kt - 4 * qb
                    c0 = 128 * i if i > 0 else 0
                    zp, zb = zpairs[zi[0] % 2]
                    zi[0] += 1
                    for hh in range(2):
                        pr = slice(64 * hh, 64 * hh + 64)
                        self.mm(hv(zp, hh)[:, c0:512], QK[pr, 4 + c, kt * 128:(kt + 1) * 128], QK[pr, c, q0 + c0:q0 + 512], True, True,
                                [self.QKb[4 + c][kt // 4], self.QKb[c][qb]], [zb[hh]])
                    return dict(idx=idx, kt=kt, i=i, c0=c0, zp=zp, zb=zb)

                def a_act(st):
                    c0 = st["c0"]
                    e, eb = fpr.next()
                    self.act(w(e, c0), w(st["zp"], c0), AF.Exp, st["zb"], [eb], scale=0.125)
                    sp_, spb = spr.next()
                    self.act(w(sp_, c0), w(e, c0), AF.Ln, [eb], [spb], bias=1.0)
                    if st["i"] >= 0:
                        for hh in range(2):
                            blk = hv(sp_, hh)[:, c0:c0 + 128]
                            self.pool(lambda: nc.gpsimd.tensor_tensor(blk, blk, self.mlt_b, ALU.mult), [spb, self.Bc], [spb])
                    st["sp"], st["spb"] = sp_, spb

                def b_copy(st):
                    c0 = st["c0"]
                    rsb_, rsbb = rpr.next()
                    self.dve(lambda: nc.vector.tensor_copy(w(rsb_, c0), w(Rp, c0)), Rb, [rsbb])
                    st["rsb"], st["rsbb"] = rsb_, rsbb

                def b_pe(st):
                    c0 = st["c0"]
                    for hh in range(2):
                        sph = hv(st["sp"], hh)[:, c0:512]
                        self.mm(hv(st["zp"], hh)[:, c0:512], self.ntri_b, sph, False, True, [self.Bc, st["spb"]], [st["zb"][hh]], inc=True)
                        self.mm(hv(Rp, hh)[:, c0:512], self.ones_b, sph, False, True, [self.Bc, st["spb"]], [Rb[hh]], inc=True)

                def b_dve(st):
                    c0 = st["c0"]
                    t, tbf = fpr.next()
                    self.dve(lambda: nc.vector.scalar_tensor_tensor(w(t, c0), w(st["zp"], c0), 0.125, w(st["rsb"], c0), ALU.mult, ALU.subtract),
                             st["zb"] + [st["rsbb"]], [tbf])
                    st["t"], st["tbf"] = t, tbf

                def b_act(st):
                    c0 = st["c0"]
                    a, ab = apr.next()
                    self.act(w(a, c0), w(st["t"], c0), AF.Exp, [st["tbf"]], [ab])
                    if st["i"] >= 0:
                        for hh in range(2):
                            blk = hv(a, hh)[:, c0:c0 + 128]
                            self.pool(lambda: nc.gpsimd.tensor_tensor(blk, blk, self.mlt_b, ALU.mult), [ab, self.Bc], [ab])
                    return (st["idx"], st["kt"], c0, a, ab)

                def stage_c(idx, kt, c0, a, ab):
                    for hh in range(2):
                        pr = slice(64 * hh, 64 * hh + 64)
                        h = 2 * c + hh
                        self.mm(o_ps[pr, c0:512], V[:, kt, h * 64:(h + 1) * 64], hv(a, hh)[:, c0:512], idx == 0, idx == nkt - 1,
                                [self.Vb[kt], ab], [o_b], inc=True)

                pa = None
                pb = None
                pc = None
                for idx in range(nkt + 3):
                    cur = a_pe(idx, kts[idx]) if idx < nkt else None
                    if cur is not None:
                        for _ in range(NFILL):
                            self.mm(self.PS[7], self.ones_b, QK[:, c, q0:q0 + 512], True, True, [self.Bc, self.QKb[c][qb]], [self.PSb[7]], inc=False)
                    if pa is not None:
                        b_copy(pa)
                        b_pe(pa)
                        b_dve(pa)
                    if pc is not None:
                        stage_c(*pc)
                    if cur is not None:
                        a_act(cur)
                    nc_ = b_act(pb) if pb is not None else None
                    pa, pb, pc = cur, pa, nc_
                self.act(MIX[:, c, q0:q0 + 512], o_ps, AF.Copy, [o_b], self.XTb[qb * 4:qb * 4 + 4])

    def _load_ln(self, idx, scale=None):
        nc, fw = self.nc, self.fw
        G = self.SCR[0][:, 0:1024]
        B_ = self.SCR[1][:, 0:1024]
        fw.dma(fw.sp, G, self.lnp_d[2 * idx].partition_broadcast(128), writes=[self.SCRb[0]])
        fw.dma(fw.sp, B_, self.lnp_d[2 * idx + 1].partition_broadcast(128), writes=[self.SCRb[1]])
        if scale is not None:
            self.act(G, G, AF.Copy, [self.SCRb[0]], [self.SCRb[0]], scale=scale)
            self.act(B_, B_, AF.Copy, [self.SCRb[1]], [self.SCRb[1]], scale=scale)
        return G, B_

    def _ln_stats(self, tt):
        nc = self.nc
        xs = self.X[:, tt, :]
        st_, stb = self.statr.next()
        xb = [self.Xb[tt]]
        self.dve(lambda: nc.vector.bn_stats(st_[:, 0:6], xs[:, 0:512]), xb, [stb])
        self.dve(lambda: nc.vector.bn_stats(st_[:, 6:12], xs[:, 512:1024]), xb, [stb])
        self.dve(lambda: nc.vector.bn_aggr(st_[:, 12:14], st_[:, 0:12]), [stb], [stb])
        self.dve(lambda: nc.vector.tensor_scalar(st_[:, 14:15], st_[:, 13:14], LN_EPS, None, ALU.add), [stb], [stb])
        self.act(st_[:, 14:15], st_[:, 14:15], AF.Ln, [stb], [stb])
        self.act(st_[:, 14:15], st_[:, 14:15], AF.Exp, [stb], [stb], scale=-0.5)
        return (tt, st_, stb)

    def _ln_apply(self, tt, st_, stb, G, B_):
        nc = self.nc
        xs = self.X[:, tt, :]
        xb = [self.Xb[tt]]
        self.dve(lambda: nc.vector.scalar_tensor_tensor(xs, xs, st_[:, 12:13], G, ALU.subtract, ALU.mult), xb + [stb, self.SCRb[0]], xb)
        self.dve(lambda: nc.vector.scalar_tensor_tensor(xs, xs, st_[:, 14:15], B_, ALU.mult, ALU.add), xb + [stb, self.SCRb[1]], xb)

    def _ln_tile(self, tt, G, B_):
        self._ln_apply(*self._ln_stats(tt), G, B_)

    def _load_wout(self, L):
        fw = self.fw
        for i in range(2):
            src = self.wout[L].rearrange("(c p) n -> p c n", p=128)[:, i * 4:(i + 1) * 4, :]
            dst = self.Wblk[3 + i].rearrange("p (c n) -> p c n", c=4)
            fw.dma(fw.pool, dst, src, writes=[self.Wb[3 + i]])

    def _mid(self, L):
        nc, fw = self.nc, self.fw
        MIX = self.XT
        inv = 1.0 / ALPHA
        Wo = [self.Wblk[3].rearrange("p (c n) -> p c n", c=4), self.Wblk[4].rearrange("p (c n) -> p c n", c=4)]
        W0 = self._expert_load(L, 0)
        G, B_ = self._load_ln(2 * L, scale=ALPHA)
        xsrc = self.x_d if L == 0 else self.spill_d
        for tt in range(NT):
            fw.dma(fw.sp, self.X[:, tt, :], xsrc[tt * 128:(tt + 1) * 128, :], writes=[self.Xb[tt]])
        psr = self.psring([5, 6])
        tpr = self.psring([0, 1])
        RB = [(self.PS[i], self.PSb[i]) for i in (2, 3, 4, 7)]

        def op_tile(tt):
            for half in range(2):
                ps, psb = psr.next()
                for fc in range(8):
                    self.mm(ps, MIX[:, fc, tt * 128:(tt + 1) * 128], Wo[fc // 4][:, fc % 4, half * 512:(half + 1) * 512], fc == 0, fc == 7,
                            [self.XTb[tt], self.Wb[3 + fc // 4]], [psb])
                xs = self.X[:, tt, half * 512:(half + 1) * 512]
                self.dve(lambda: nc.vector.scalar_tensor_tensor(xs, xs, ALPHA, ps, ALU.mult, ALU.add), [self.Xb[tt], psb], [self.Xb[tt]])
            st = self._ln_stats(tt)
            ln_flush()
            ln_pend.append(st)

        ln_pend = []

        def ln_flush():
            while ln_pend:
                self._ln_apply(*ln_pend.pop(0), G, B_)

        def xt_T(tb, dc):
            ps, psb = tpr.next()
            for j in range(4):
                tt = tb * 4 + j
                self.tr(ps[:, j * 128:(j + 1) * 128], self.X[:, tt, dc * 128:(dc + 1) * 128], self.ident_f,
                        [self.Xb[tt], self.Bc], [psb], inc=(j == 3))
            xf, xfb = self.tmpf.next()
            self.act(xf, ps, AF.Copy, [psb], [xfb], scale=inv)
            self.act(self.XT[:, dc, tb * 512:(tb + 1) * 512], ps, AF.Copy, [psb], self.XTb[tb * 4:tb * 4 + 4], scale=inv)
            return (tb, dc, xf, xfb)

        def xt_R(tb, dc, xf, xfb):
            for j in range(4):
                self.mm(RB[j][0][:, 0:NE], xf[:, j * 128:(j + 1) * 128], self.wr[:, dc, :], dc == 0, dc == 7, [self.Bc, xfb], [RB[j][1]])
            if dc == 7:
                for j in range(4):
                    tt = tb * 4 + j
                    self.act(self.lgt[:, tt * NE:(tt + 1) * NE], RB[j][0][:, 0:NE], AF.Copy, [RB[j][1]], [self.Brt])

        pend = []

        def xt_step(tb, dc):
            pend.append(xt_T(tb, dc))
            if len(pend) > 1:
                xt_R(*pend.pop(0))

        for tt in range(4):
            op_tile(tt)
        for tb in range(4):
            for j in range(4):
                if tb + 1 < 4:
                    op_tile((tb + 1) * 4 + j)
                else:
                    ln_flush()
                xt_step(tb, 2 * j)
                xt_step(tb, 2 * j + 1)
        while pend:
            xt_R(*pend.pop(0))
        return W0

    def _routing(self):
        nc = self.nc
        def wt():
            t, tb_ = self.tmpf.next()
            return t[:, 0:256], tb_
        aff, affb = wt()
        self.act(aff, self.lgt, AF.Sigmoid, [self.Brt], [affb])
        sel, selb = wt()
        v3 = lambda a: a.rearrange("p (t e) -> p t e", t=NT)
        v4 = lambda a: a.rearrange("p (t g j) -> p t g j", t=NT, g=4)
        g3 = lambda a: a.rearrange("p (t g) -> p t g", t=NT)
        self.dve(lambda: nc.vector.tensor_tensor(v3(sel), v3(aff), self.rbias.unsqueeze(1).broadcast_to([128, NT, NE]), ALU.add), [affb, self.Bc], [selb])
        sm = self.small
        m1 = sm[:, 8:8 + 64] if False else None
        m1t, m1b = self.tmpf.next()
        m1 = m1t[:, 0:64]
        m2 = m1t[:, 64:128]
        gs = m1t[:, 128:192]
        gmax = m1t[:, 192:208]
        gmask = m1t[:, 208:272]
        gsum = m1t[:, 272:288]
        self.dve(lambda: nc.vector.tensor_reduce(m1, sel.rearrange("p (tg j) -> p tg j", j=4), AX.X, ALU.max), [selb], [m1b])
        eq, eqb = wt()
        m1bc = m1.unsqueeze(2).broadcast_to([128, 64, 4])
        self.dve(lambda: nc.vector.tensor_tensor(eq.rearrange("p (tg j) -> p tg j", j=4), sel.rearrange("p (tg j) -> p tg j", j=4), m1bc, ALU.is_equal), [selb, m1b], [eqb])
        self.dve(lambda: nc.vector.scalar_tensor_tensor(eq, eq, -1.0e9, sel, ALU.mult, ALU.add), [eqb, selb], [eqb])
        self.dve(lambda: nc.vector.tensor_reduce(m2, eq.rearrange("p (tg j) -> p tg j", j=4), AX.X, ALU.max), [eqb], [m1b])
        self.dve(lambda: nc.vector.tensor_tensor(gs, m1, m2, ALU.add), [m1b], [m1b])
        self.dve(lambda: nc.vector.tensor_reduce(gmax, g3(gs), AX.X, ALU.max), [m1b], [m1b])
        self.dve(lambda: nc.vector.tensor_tensor(g3(gmask), g3(gs), gmax.unsqueeze(2).broadcast_to([128, NT, 4]), ALU.is_equal), [m1b], [m1b])
        ge, geb = wt()
        m2bc = m2.unsqueeze(2).broadcast_to([128, 64, 4])
        gmbc = gmask.unsqueeze(2).broadcast_to([128, 64, 4])
        j4 = lambda a: a.rearrange("p (tg j) -> p tg j", j=4)
        self.dve(lambda: nc.vector.tensor_tensor(j4(ge), j4(sel), m2bc, ALU.is_ge), [selb, m1b], [geb])
        self.dve(lambda: nc.vector.tensor_tensor(j4(ge), j4(ge), gmbc, ALU.mult), [geb, m1b], [geb])
        self.dve(lambda: nc.vector.tensor_tensor(ge, ge, aff, ALU.mult), [geb, affb], [geb])
        self.dve(lambda: nc.vector.tensor_reduce(gsum, v3(ge), AX.X, ALU.add), [geb], [m1b])
        self.dve(lambda: nc.vector.reciprocal(gsum, gsum), [m1b], [m1b])
        self.dve(lambda: nc.vector.tensor_tensor(v3(self.comb), v3(ge), gsum.unsqueeze(2).broadcast_to([128, NT, NE]), ALU.mult), [geb, m1b], [self.Bcomb])

    def _expert_load(self, L, e):
        fw = self.fw
        s = e % 2
        Wg = self.Wblk[3 * s].rearrange("p (c n) -> p c n", c=8)
        Wu = self.Wblk[3 * s + 1].rearrange("p (c n) -> p c n", c=8)
        Wd = self.Wblk[3 * s + 2].rearrange("p (c n) -> p c n", c=4)
        fw.dma(fw.pool, Wg, self.wg[L][e].rearrange("(c p) n -> p c n", p=128), writes=[self.Wb[3 * s]])
        fw.dma(fw.pool, Wu, self.wu[L][e].rearrange("(c p) n -> p c n", p=128), writes=[self.Wb[3 * s + 1]])
        fw.dma(fw.pool, Wd, self.wd[L][e].rearrange("(c p) n -> p c n", p=128), writes=[self.Wb[3 * s + 2]])
        return (Wg, Wu, Wd, s)

    def _experts(self, L, W0):
        nc, fw = self.nc, self.fw
        gr = self.psring([0, 1])
        ur = self.psring([2, 3])
        yr = self.psring([4, 5, 6, 7])
        comb3 = self.comb.rearrange("p (t e) -> p t e", t=NT)

        def load(e):
            s = e % 2
            Wg = self.Wblk[3 * s].rearrange("p (c n) -> p c n", c=8)
            Wu = self.Wblk[3 * s + 1].rearrange("p (c n) -> p c n", c=8)
            Wd = self.Wblk[3 * s + 2].rearrange("p (c n) -> p c n", c=4)
            fw.dma(fw.pool, Wg, self.wg[L][e].rearrange("(c p) n -> p c n", p=128), writes=[self.Wb[3 * s]])
            fw.dma(fw.pool, Wu, self.wu[L][e].rearrange("(c p) n -> p c n", p=128), writes=[self.Wb[3 * s + 1]])
            fw.dma(fw.pool, Wd, self.wd[L][e].rearrange("(c p) n -> p c n", p=128), writes=[self.Wb[3 * s + 2]])
            return (Wg, Wu, Wd, s)

        G_, B_ = self._load_ln(2 * L + 1)

        lnp = []

        def ln_fin():
            while lnp:
                st = lnp.pop(0)
                self._ln_apply(*st, G_, B_)
                if L == 1:
                    tt = st[0]
                    fw.dma(fw.sp, self.out_d[tt * 128:(tt + 1) * 128, :], self.X[:, tt, :], reads=[self.Xb[tt]])

        def ln_out(tt):
            st = self._ln_stats(tt)
            ln_fin()
            lnp.append(st)

        def stage_gu(e, tb, W, ln_tiles=()):
            Wg, Wu, Wd, s = W
            H, Hb = self.htr.next()
            xb4 = self.XTb[tb * 4:tb * 4 + 4]
            for fc in range(4):
                g, gb = gr.next()
                u, ub = ur.next()
                for dc in range(8):
                    self.mm(g, Wg[:, dc, fc * 128:(fc + 1) * 128], self.XT[:, dc, tb * 512:(tb + 1) * 512], dc == 0, dc == 7, [self.Wb[3 * s]] + xb4, [gb])
                for dc in range(8):
                    self.mm(u, Wu[:, dc, fc * 128:(fc + 1) * 128], self.XT[:, dc, tb * 512:(tb + 1) * 512], dc == 0, dc == 7, [self.Wb[3 * s + 1]] + xb4, [ub])
                sg, sgb = self.tmpf.next()
                self.act(sg, g, AF.Silu, [gb], [sgb])
                self.dve(lambda: nc.vector.tensor_tensor(H[:, fc, :], u, sg, ALU.mult), [ub, sgb], [Hb])
                if fc < len(ln_tiles):
                    ln_out(ln_tiles[fc])
            return (e, tb, W, H, Hb)

        def stage_dn(e, tb, W, H, Hb):
            Wg, Wu, Wd, s = W
            for t4 in range(4):
                tt = tb * 4 + t4
                for half in range(2):
                    y, yb = yr.next()
                    for fc in range(4):
                        self.mm(y, H[:, fc, t4 * 128:(t4 + 1) * 128], Wd[:, fc, half * 512:(half + 1) * 512], fc == 0, fc == 3, [Hb, self.Wb[3 * s + 2]], [yb])
                    xs = self.X[:, tt, half * 512:(half + 1) * 512]
                    self.dve(lambda: nc.vector.scalar_tensor_tensor(xs, y, comb3[:, tt, e:e + 1], xs, ALU.mult, ALU.add), [yb, self.Bcomb, self.Xb[tt]], [self.Xb[tt]])

        Wn = W0
        prev = None
        for e in range(NE):
            W = Wn
            for tb in range(4):
                if e == NE - 1:
                    stage_dn(*prev)
                    done = list(range((tb - 1) * 4, tb * 4)) if tb > 0 else []
                    cur = stage_gu(e, tb, W, done)
                else:
                    cur = stage_gu(e, tb, W)
                    if prev is not None:
                        stage_dn(*prev)
                prev = cur
                if tb == 0 and e + 1 < NE:
                    Wn = self._expert_load(L, e + 1)
                if tb == 0 and e + 1 == NE and L == 0:
                    self.pref_win = {g: self._load_w_in(1, g, self._win_slot(1)[g]) for g in (3, 0, 1)}
        stage_dn(*prev)
        for tt in range(12, 16):
            ln_out(tt)
        ln_fin()

    def _final_ln(self, L):
        pass


def _constants():
    j = np.arange(128)[:, None]
    k = np.arange(128)[None, :]
    ident = (j == k).astype(np.float32)
    tri = (j >= k).astype(np.float32)
    mle = (j <= k).astype(np.float32)
    mlt = (j < k).astype(np.float32)
    m = np.arange(128)
    perm = np.where((m % 64) < 32, m + 32, m - 32)
    pm = np.zeros((128, 128), np.float32)
    pm[perm, m] = 1.0
    invc = np.zeros((128, 4, 16), np.float32)
    for g, w in enumerate(POOL_WINDOWS):
        invc[:, g, :] = 1.0 / np.minimum(np.arange(16) + 1, w)
    cst = np.concatenate([ident, tri, mle, mlt, pm, invc.reshape(128, 64)], axis=1).astype(np.float32)
    inv = (10000.0 ** (-np.arange(0, 64, 2, dtype=np.float32) / 64)).astype(np.float32)
    ang = np.arange(S, dtype=np.float32)[:, None] * inv[None, :]
    ang = np.concatenate([ang, ang], axis=-1)
    cos = np.cos(ang).astype(np.float32).T
    sin = np.sin(ang).astype(np.float32).T
    sign = np.where(np.arange(64) < 32, -1.0, 1.0).astype(np.float32)[:, None]
    sins = sin * sign
    rope = np.stack([np.concatenate([cos, cos], 0), np.concatenate([sins, sins], 0)], 0).astype(np.float32)
    return cst, np.ascontiguousarray(rope)


def _pack_inputs(inp):
    f = lambda a: np.ascontiguousarray(np.asarray(a, dtype=np.float32))
    cst, rope = _constants()
    pp = np.zeros((128, 144), np.float32)
    pp[:, 0:124] = f(inp["l0_conv_w"]).T.reshape(4, 128, 31).transpose(1, 0, 2).reshape(128, 124)
    v4 = lambda a: f(a).reshape(4, 128).T
    pp[:, 124:128] = v4(inp["l0_conv_b"])
    pp[:, 128:132] = v4(inp["l0_conv_ln_g"])
    pp[:, 132:136] = v4(inp["l0_conv_ln_b"])
    pp[:, 136] = f(inp["l0_subln_g"])
    pp[:, 137:141] = v4(inp["l1_pool_scale"])
    rowp = np.concatenate([f(inp["l0_lambda_q1"]), f(inp["l0_lambda_q2"]), f(inp["l0_lambda_k1"]), f(inp["l0_lambda_k2"]),
                           f(inp["router_bias"])]).astype(np.float32)
    lnp = np.stack([f(inp["l0_ln_mix_g"]), f(inp["l0_ln_mix_b"]), f(inp["l0_ln_ffn_g"]), f(inp["l0_ln_ffn_b"]),
                    f(inp["l1_ln_mix_g"]), f(inp["l1_ln_mix_b"]), f(inp["l1_ln_ffn_g"]), f(inp["l1_ln_ffn_b"])], 0)
    shared = {
        "router_w": f(inp["router_w"]),
        "w_in0": f(inp["l0_in_proj"]), "w_in1": f(inp["l1_in_proj"]),
        "w_out0": f(inp["l0_out_proj"]), "w_out1": f(inp["l1_out_proj"]),
        "w_gate0": f(inp["l0_w_gate"]), "w_gate1": f(inp["l1_w_gate"]),
        "w_up0": f(inp["l0_w_up"]), "w_up1": f(inp["l1_w_up"]),
        "w_down0": f(inp["l0_w_down"]), "w_down1": f(inp["l1_w_down"]),
        "pool_w": f(inp["l1_pool_w"]),
        "pp": pp, "rowp": rowp, "lnp": np.ascontiguousarray(lnp), "cst": cst, "rope": rope,
    }
    x = f(inp["x"])
    return [dict(shared, x=np.ascontiguousarray(x[c])) for c in range(x.shape[0])]


def kernel(**inputs):
    in_maps = _pack_inputs(inputs)
    nc = Prog().build()
    res = run_bass_kernel_spmd(nc, in_maps, core_ids=list(range(8)))
    return np.stack([np.asarray(r["out"], dtype=np.float32) for r in res.results], axis=0)
```

```python
import contextlib
import math
import numpy as np
import concourse.bass as bass
import concourse.mybir as mybir
from concourse.alu_op_type import AluOpType as ALU
from concourse.bass_utils import run_bass_kernel_spmd

F32 = mybir.dt.float32
BF16 = mybir.dt.bfloat16
AF = mybir.ActivationFunctionType
AX = mybir.AxisListType

S = 2048
D = 1024
NT = 16
NE = 16
ALPHA = 4.0 ** 0.25
LN_EPS = 1e-5
RMS_EPS = 1e-5
LAM_INIT0 = 0.8 - 0.6 * math.exp(0.0)
POOL_WINDOWS = (2, 4, 8, 16)
NFILL = 3


import os as _os
TRACE = bool(_os.environ.get("KTRACE"))


class Sem:
    def __init__(self, h):
        self.h = h
        self.cnt = 0


class Buf:
    __slots__ = ("last_w", "readers", "name", "excl")

    def __init__(self, name="", excl=False):
        self.last_w = None
        self.readers = []
        self.name = name
        self.excl = excl


class Eng:
    def __init__(self, name, obj, sem, same_wait=True):
        self.name = name
        self.obj = obj
        self.sem = sem
        self.waited = {}
        self.same_wait = same_wait


class FW:
    def __init__(self, nc, stack, n_dma_sems=32):
        self.nc = nc
        def mk(n):
            s_ = Sem(stack.enter_context(nc.semaphore(n)))
            s_.name = n
            return s_
        self.pe = Eng("pe", nc.tensor, mk("s_pe"), same_wait=False)
        self.act = Eng("act", nc.scalar, mk("s_act"))
        self.dve = Eng("dve", nc.vector, mk("s_dve"))
        self.pool = Eng("pool", nc.gpsimd, mk("s_pool"))
        self.sp = Eng("sp", nc.sync, mk("s_sp"))
        self.engs = [self.pe, self.act, self.dve, self.pool, self.sp]
        self.dma_sems = [mk(f"s_dma{i}") for i in range(n_dma_sems)]
        self.dma_i = 0
        self.dma_qi = [0, 0]
        self.n_instr = 0
        self.pe_pending = False

    def _wait(self, eng, tok):
        sem, val = tok
        if sem is eng.sem and not eng.same_wait:
            return
        if eng.waited.get(id(sem), 0) >= val:
            return
        eng.obj.wait_ge(sem.h, val)
        eng.waited[id(sem)] = val
        if TRACE:
            print("   wait", eng.name, "on", getattr(sem, "name", "?"), val)

    def _deps(self, eng, reads, writes):
        for b in reads:
            if b.last_w is not None:
                self._wait(eng, b.last_w)
            if b.excl:
                for t in b.readers:
                    if t[0] is not eng.sem:
                        self._wait(eng, t)
        for b in writes:
            if b.last_w is not None:
                self._wait(eng, b.last_w)
            for t in b.readers:
                self._wait(eng, t)

    @staticmethod
    def _commit(tok, reads, writes):
        for b in reads:
            if len(b.readers) > 24:
                best = {}
                for s_, v_ in b.readers:
                    if best.get(id(s_), (None, -1))[1] < v_:
                        best[id(s_)] = (s_, v_)
                b.readers = list(best.values())
            b.readers.append(tok)
        for b in writes:
            b.last_w = tok
            b.readers = []

    def op(self, eng, fn, reads=(), writes=(), inc=True):
        if TRACE:
            print("op", eng.name, "cnt", eng.sem.cnt, "reads", [b.name for b in reads], "writes", [b.name for b in writes], "inc", inc)
        self._deps(eng, reads, writes)
        ins = fn()
        self.n_instr += 1
        if inc:
            eng.sem.cnt += 1
            ins.then_inc(eng.sem.h, 1)
            tok = (eng.sem, eng.sem.cnt)
            if eng is self.pe:
                self.pe_pending = False
        else:
            assert eng is self.pe
            tok = (eng.sem, eng.sem.cnt + 1)
            self.pe_pending = True
        self._commit(tok, reads, writes)
        return ins

    def dma(self, q, out, in_, reads=(), writes=(), **kw):
        half = len(self.dma_sems) // 2
        k = 0 if q is self.sp else 1
        sem = self.dma_sems[k * half + self.dma_qi[k] % half]
        self.dma_qi[k] += 1
        self.dma_i += 1
        if sem.cnt:
            self._wait(q, (sem, sem.cnt))
        self._deps(q, reads, writes)
        ins = q.obj.dma_start(out=out, in_=in_, **kw)
        sem.cnt += 16
        ins.then_inc(sem.h, 16)
        self.n_instr += 1
        tok = (sem, sem.cnt)
        self._commit(tok, reads, writes)
        return tok

    def barrier(self):
        assert not self.pe_pending
        for e in self.engs:
            for o in self.engs:
                if o is not e and o.sem.cnt:
                    self._wait(e, (o.sem, o.sem.cnt))
            for s in self.dma_sems:
                if s.cnt:
                    self._wait(e, (s, s.cnt))


class Ring:
    def __init__(self, aps, name="r"):
        self.items = [(ap, Buf(f"{name}{i}")) for i, ap in enumerate(aps)]
        self.i = 0

    def next(self):
        it = self.items[self.i % len(self.items)]
        self.i += 1
        return it


class Prog:
    def __init__(self, stop=None, taps=()):
        self.stop = stop
        self.taps = set(taps)
        self.dbg_specs = {}

    def build(self):
        nc = bass.Bass("TRN2", target_bir_lowering=False)
        self.nc = nc
        di = lambda n, s: nc.dram_tensor(n, list(s), F32, kind="ExternalInput").ap()
        self.x_d = di("x", [S, D])
        self.router_w = di("router_w", [D, NE])
        self.win = [di("w_in0", [D, 2560]), di("w_in1", [D, 2048])]
        self.wout = [di("w_out0", [D, D]), di("w_out1", [D, D])]
        self.wg = [di("w_gate0", [NE, D, 512]), di("w_gate1", [NE, D, 512])]
        self.wu = [di("w_up0", [NE, D, 512]), di("w_up1", [NE, D, 512])]
        self.wd = [di("w_down0", [NE, 512, D]), di("w_down1", [NE, 512, D])]
        self.pool_w = di("pool_w", [4, 128, 128])
        self.pp_d = di("pp", [128, 144])
        self.rowp_d = di("rowp", [272])
        self.lnp_d = di("lnp", [8, D])
        self.cst_d = di("cst", [128, 128 * 5 + 64])
        self.rope_d = di("rope", [2, 128, S])
        self.out_d = nc.dram_tensor("out", [S, D], F32, kind="ExternalOutput").ap()
        self.spill_d = nc.dram_tensor("xspill", [S, D], F32, kind="Internal").ap()
        with contextlib.ExitStack() as st:
            self.st = st
            self.fw = FW(nc, st)
            self._alloc()
            self._run()
        return nc

    def _alloc(self):
        nc, st = self.nc, self.st
        sb = lambda n, s, d: st.enter_context(nc.sbuf_tensor("sb_" + n, s, d))
        self.XT = sb("xt", [128, 8 * S], BF16)[:].rearrange("p (c t) -> p c t", c=8)
        self.XTb = [Buf(f"xt{t}") for t in range(NT)]
        bigf = sb("big", [128, 16640], F32)[:]
        self.X = bigf[:, 0:16384].rearrange("p (t d) -> p t d", t=NT)
        self.Xb = [Buf(f"x{t}") for t in range(NT)]
        bigb = bigf.bitcast(BF16)
        self.QK = bigb[:, 0:16384].rearrange("p (c t) -> p c t", c=8)
        self.QKb = [[Buf(f"qk{c}_{tb}") for tb in range(4)] for c in range(8)]
        self.V = bigb[:, 16384:24576].rearrange("p (t f) -> p t f", t=NT)
        self.Vb = [Buf(f"v{t}") for t in range(NT)]
        self.AU = bigb[:, 24576:32896].rearrange("p (c t) -> p c t", c=4)
        self.AUb = [Buf(f"au{c}") for c in range(4)]
        wb = sb("w", [128, 24576], BF16)[:]
        self.Wblk = [wb[:, i * 4096:(i + 1) * 4096] for i in range(6)]
        self.Wb = [Buf(f"w{i}") for i in range(6)]
        scr = sb("scr", [128, 4096], F32)[:]
        self.SCR = [scr[:, 0:2048], scr[:, 2048:4096]]
        self.SCRb = [Buf("scrA"), Buf("scrB")]
        tf = sb("tmpf", [128, 8 * 512], F32)[:]
        self.tmpf = Ring([tf[:, i * 512:(i + 1) * 512] for i in range(8)], "tf")
        tb_ = sb("tmpb", [128, 10 * 512], BF16)[:]
        self.tmpb = Ring([tb_[:, i * 512:(i + 1) * 512] for i in range(6)], "tb")
        self.tmpb2 = Ring([tb_[:, i * 512:(i + 1) * 512] for i in range(6, 10)], "tb2")
        self.tf_full, self.tb_full = tf, tb_
        self.ht = sb("ht", [128, 2 * 2048], BF16)[:]
        self.htr = Ring([self.ht[:, i * 2048:(i + 1) * 2048].rearrange("p (c t) -> p c t", c=4) for i in range(2)], "ht")
        self.cstf = sb("cstf", [128, 128 * 5 + 64], F32)[:]
        self.cstb = sb("cstb", [128, 128 * 7], BF16)[:]
        self.Bc = Buf("const")
        self.pp = sb("pp", [128, 144], F32)[:]
        self.rowp = sb("rowp", [128, 272], F32)[:]
        self.small = sb("small", [128, 64], F32)[:]
        self.Bsmall = Buf("small")
        self.comb = sb("comb", [128, NT * NE], F32)[:]
        self.Bcomb = Buf("comb")
        self.zeros_b = sb("zeros", [128, 128], BF16)[:]
        self.rt_halves = [scr[0:16, 1024:2048], scr[0:16, 3072:4096]]
        self.lgt = scr[:, 1024:1024 + NT * NE]
        self.Brt = Buf("rt")
        stt = sb("stats", [128, 4 * 16], F32)[:]
        self.statr = Ring([stt[:, i * 16:(i + 1) * 16] for i in range(4)], "stat")
        self.wr = sb("wr", [128, 8 * NE], F32)[:].rearrange("p (c e) -> p c e", c=8)
        self.pwb = sb("pwb", [128, 4 * 128], BF16)[:].rearrange("p (g d) -> p g d", g=4)
        self.PS = []
        self.PSb = []
        self.PSpair = []
        for k in range(4):
            pair = st.enter_context(nc.psum_tensor(f"pp{k}", [128, 1024], F32))[:]
            self.PSpair.append(pair)
            for h in range(2):
                self.PS.append(pair[:, h * 512:(h + 1) * 512])
                self.PSb.append(Buf(f"ps{2 * k + h}", excl=True))

    def psring(self, idxs):
        r = Ring([self.PS[i] for i in idxs])
        r.items = [(self.PS[i], self.PSb[i]) for i in idxs]
        return r

    def mm(self, out, lhsT, rhs, start, stop, reads, writes, inc=None):
        nc = self.nc
        inc = stop if inc is None else inc
        self.fw.op(self.fw.pe, lambda: nc.tensor.matmul(out, lhsT, rhs, start=start, stop=stop), reads, writes, inc=inc)

    def tr(self, out, in_, ident, reads, writes, inc=True):
        nc = self.nc
        self.fw.op(self.fw.pe, lambda: nc.tensor.transpose(out, in_, ident), reads, writes, inc=inc)

    def act(self, out, in_, func, reads, writes, **kw):
        nc = self.nc
        self.fw.op(self.fw.act, lambda: nc.scalar.activation(out, in_, func, **kw), reads, writes)

    def dve(self, fn, reads, writes):
        self.fw.op(self.fw.dve, fn, reads, writes)

    def pool(self, fn, reads, writes):
        self.fw.op(self.fw.pool, fn, reads, writes)

    def tap(self, name, ap, bufs, dtype=F32):
        if name not in self.taps:
            return
        nc = self.nc
        shape = list(ap.shape)
        d = nc.dram_tensor("dbg_" + name, shape, dtype, kind="ExternalOutput").ap()
        self.dbg_specs[name] = (shape, dtype)
        self.fw.barrier()
        self.fw.dma(self.fw.sp, d, ap, reads=bufs)
        self.fw.barrier()

    def _run(self):
        self._consts()
        for L in range(2):
            if L == 0:
                self._xt_from_dram()
            else:
                self._xt_from_x(router=False, spill=True)
                self.fw.barrier()
            if self.stop == f"l{L}_xt":
                return self._finish()
            self._in_proj(L)
            if self.stop in (f"l{L}_inproj", "l0_glu", "l0_qk"):
                return self._finish()
            self._load_wout(L)
            if L == 0:
                if self.stop == "l0_conv":
                    self._conv_branch()
                    return self._finish()
                self._diff_attn(self._conv_pe())
                self._conv_ln()
            else:
                self._pool_mix()
                self._sb_attn()
            if self.stop == f"l{L}_mix":
                return self._finish()
            self.fw.barrier()
            W0 = self._mid(L)
            self._routing()
            if self.stop == f"l{L}_route":
                return self._finish()
            self._experts(L, W0)
            self._final_ln(L)
            if self.stop == f"l{L}_moe":
                return self._finish()
        self._finish()

    def _finish(self):
        self.fw.barrier()
        allb = self.XTb + self.Xb + self.Vb + self.AUb + [b for r in self.QKb for b in r] + [self.Bcomb, self.Brt, self.Bsmall]
        self.tap("XT", self.XT, allb, BF16)
        self.tap("QK", self.QK, allb, BF16)
        self.tap("V", self.V, allb, BF16)
        self.tap("AU", self.AU, allb, BF16)
        self.tap("X", self.X, allb, F32)
        self.tap("comb", self.comb, allb, F32)
        self.tap("small", self.small, allb, F32)
        self.fw.barrier()

    def _consts(self):
        nc, fw = self.nc, self.fw
        fw.dma(fw.sp, self.cstf, self.cst_d[:, :], writes=[self.Bc])
        fw.dma(fw.sp, self.pp, self.pp_d[:, :], writes=[self.Bc])
        fw.dma(fw.sp, self.rowp, self.rowp_d.partition_broadcast(128), writes=[self.Bc])
        fw.dma(fw.sp, self.wr, self.router_w.rearrange("(c p) e -> p c e", p=128), writes=[self.Bc])
        fw.dma(fw.pool, self.pwb, self.pool_w.rearrange("g c d -> c g d"), writes=[self.Bc])
        self.dve(lambda: nc.vector.tensor_copy(self.cstb[:, 0:640], self.cstf[:, 0:640]), [self.Bc], [self.Bc])
        self.dve(lambda: nc.vector.memset(self.cstb[:, 640:768], 1.0), [], [self.Bc])
        self.dve(lambda: nc.vector.tensor_scalar(self.cstb[:, 768:896], self.cstf[:, 128:256], -8.0, None, ALU.mult), [self.Bc], [self.Bc])
        self.ident_f = self.cstf[:, 0:128]
        self.ident_b = self.cstb[:, 0:128]
        self.tri_b = self.cstb[:, 128:256]
        self.mle_b = self.cstb[:, 256:384]
        self.mlt_b = self.cstb[:, 384:512]
        self.pm_b = self.cstb[:, 512:640]
        self.ones_b = self.cstb[:, 640:768]
        self.ntri_b = self.cstb[:, 768:896]
        self.invc = self.cstf[:, 640:704].rearrange("p (g t) -> p g t", g=4)
        self.cw = self.pp[:, 0:124].rearrange("p (c w) -> p c w", c=4)
        self.conv_b = self.pp[:, 124:128]
        self.cln_g = self.pp[:, 128:132]
        self.cln_b = self.pp[:, 132:136]
        self.subln = self.pp[:, 136:137]
        self.pscale = self.pp[:, 137:141]
        sm = self.small
        lam4 = self.rowp[:, 0:256].rearrange("p (a d) -> p a d", a=4)
        tmp, tmpb_ = self.tmpf.next()
        self.dve(lambda: nc.vector.tensor_tensor(tmp[:, 0:128].rearrange("p (a d) -> p a d", a=2), lam4[:, 0:2, :], lam4[:, 2:4, :], ALU.mult), [self.Bc], [tmpb_])
        self.dve(lambda: nc.vector.tensor_reduce(sm[:, 2:4], tmp[:, 0:128].rearrange("p (a d) -> p a d", a=2), AX.X, ALU.add), [tmpb_], [self.Bsmall])
        self.act(sm[:, 4:6], sm[:, 2:4], AF.Exp, [self.Bsmall], [self.Bsmall])
        self.dve(lambda: nc.vector.tensor_tensor(sm[:, 6:7], sm[:, 5:6], sm[:, 4:5], ALU.subtract), [self.Bsmall], [self.Bsmall])
        self.dve(lambda: nc.vector.tensor_scalar(sm[:, 0:1], sm[:, 6:7], -LAM_INIT0, None, ALU.add), [self.Bsmall], [self.Bsmall])
        self.dve(lambda: nc.vector.tensor_scalar(sm[:, 1:2], self.subln, 1.0 - LAM_INIT0, None, ALU.mult), [self.Bc], [self.Bsmall])
        self.neglam = sm[:, 0:1]
        self.gsc = sm[:, 1:2]
        self.rbias = self.rowp[:, 256:272]

    def _xt_from_dram(self):
        nc, fw = self.nc, self.fw
        psr = self.psring([0, 1, 2, 3])
        k = 0
        for tt in range(NT):
            half = tt % 2
            for hh in range(2):
                pass
            xt_ap = self.SCR[half][:, 0:1024]
            fw.dma(fw.sp, xt_ap, self.x_d[tt * 128:(tt + 1) * 128, :], writes=[self.SCRb[half]])
            for dg in range(2):
                ps, psb = psr.next()
                for j in range(4):
                    dc = dg * 4 + j
                    self.tr(ps[:, j * 128:(j + 1) * 128], xt_ap[:, dc * 128:(dc + 1) * 128], self.ident_f,
                            [self.SCRb[half], self.Bc], [psb], inc=(j == 3))
                dst = self.XT[:, dg * 4:(dg + 1) * 4, tt * 128:(tt + 1) * 128]
                src = ps.rearrange("p (c t) -> p c t", c=4)
                if k % 2 == 0:
                    self.act(dst, src, AF.Copy, [psb], [self.XTb[tt]])
                else:
                    self.dve(lambda: nc.vector.tensor_copy(dst, src), [psb], [self.XTb[tt]])
                k += 1

    def _xt_from_x(self, router, spill, inv=1.0):
        nc, fw = self.nc, self.fw
        psr = self.psring([0, 1])
        lg_ps, lg_b = self.PS[2], self.PSb[2]
        if spill:
            for tt in range(NT):
                fw.dma(fw.sp, self.spill_d[tt * 128:(tt + 1) * 128, :], self.X[:, tt, :], reads=[self.Xb[tt]])
        for tb in range(4):
            for dc in range(8):
                ps, psb = psr.next()
                for j in range(4):
                    tt = tb * 4 + j
                    self.tr(ps[:, j * 128:(j + 1) * 128], self.X[:, tt, dc * 128:(dc + 1) * 128], self.ident_f,
                            [self.Xb[tt], self.Bc], [psb], inc=(j == 3))
                dst = self.XT[:, dc, tb * 512:(tb + 1) * 512]
                self.act(dst, ps, AF.Copy, [psb], self.XTb[tb * 4:tb * 4 + 4], scale=inv)
                if router:
                    xf, xfb = self.tmpf.next()
                    self.act(xf, ps, AF.Copy, [psb], [xfb], scale=inv)
                    self.mm(lg_ps[0:16, :], self.wr[:, dc, :], xf, dc == 0, dc == 7, [self.Bc, xfb], [lg_b])
            if router:
                self.dve(lambda: nc.vector.tensor_copy(self.rt_halves[tb // 2][:, (tb % 2) * 512:(tb % 2 + 1) * 512], lg_ps[0:16, :]), [lg_b], [self.Brt])

    @staticmethod
    def _win_slot(L):
        return {0: 0, 1: 1, 2: 2, 3: 3, 4: 4} if L == 0 else {3: 0, 0: 1, 1: 2, 2: 3}

    def _load_w_in(self, L, g, slot):
        fw = self.fw
        src = self.win[L].rearrange("(c p) n -> p c n", p=128)[:, :, g * 512:(g + 1) * 512]
        dst = self.Wblk[slot].rearrange("p (c n) -> p c n", c=8)
        fw.dma(fw.pool, dst, src, writes=[self.Wb[slot]])
        return dst

    def _proj_fm(self, W, wbuf, c, tb, ps, psb):
        for dc in range(8):
            self.mm(ps, W[:, dc, c * 128:(c + 1) * 128], self.XT[:, dc, tb * 512:(tb + 1) * 512], dc == 0, dc == 7,
                    [wbuf] + self.XTb[tb * 4:tb * 4 + 4], [psb])

    def _proj_v(self, W, wbuf):
        nc = self.nc
        psr = self.psring([0, 1, 2, 3])
        for tt in range(NT):
            ps, psb = psr.next()
            for dc in range(8):
                self.mm(ps, self.XT[:, dc, tt * 128:(tt + 1) * 128], W[:, dc, :], dc == 0, dc == 7, [wbuf, self.XTb[tt]], [psb])
            if tt % 2 == 0:
                self.act(self.V[:, tt, :], ps, AF.Copy, [psb], [self.Vb[tt]])
            else:
                self.dve(lambda: nc.vector.tensor_copy(self.V[:, tt, :], ps), [psb], [self.Vb[tt]])

    def _in_proj(self, L):
        nc, fw = self.nc, self.fw
        ng = 5 if L == 0 else 4
        pref = getattr(self, "pref_win", {}) if L == 1 else {}
        slot_of = self._win_slot(L)
        Ws = [pref[g] if g in pref else self._load_w_in(L, g, slot_of[g]) for g in range(ng)]
        Wbs = [self.Wb[slot_of[g]] for g in range(ng)]
        psr = self.psring([0, 1, 2, 3])
        if L == 0:
            fw.dma(fw.sp, self.SCR[0], self.rope_d[0], writes=[self.SCRb[0]])
            fw.dma(fw.sp, self.SCR[1], self.rope_d[1], writes=[self.SCRb[1]])
            for c in range(4):
                self.pool(lambda: nc.gpsimd.memset(self.AU[:, c, 0:32], 0.0), [], [self.AUb[c]])
            for c in range(4):
                for tb in range(4):
                    pv, pvb = psr.next()
                    pg, pgb = psr.next()
                    self._proj_fm(Ws[0], self.Wb[0], c, tb, pv, pvb)
                    self._proj_fm(Ws[1], self.Wb[1], c, tb, pg, pgb)
                    sg, sgb = self.tmpf.next()
                    self.act(sg, pg, AF.Sigmoid, [pgb], [sgb])
                    dst = self.AU[:, c, 32 + tb * 512:32 + (tb + 1) * 512]
                    self.dve(lambda: nc.vector.tensor_tensor(dst, pv, sg, ALU.mult), [pvb, sgb], [self.AUb[c]])
            if self.stop == "l0_glu":
                return
            self._conv_diag()
            pmr = self.psring([4, 5])
            pending = None

            import os
            KD = int(os.environ.get("KDBG", "0"))

            def rope_tail(item):
                c8, tb, p1, p1b, qb, qbb = item
                if KD == 1:
                    return
                if KD == 2:
                    p2, p2b = pmr.next()
                    self.mm(p2, self.pm_b, qb, True, True, [self.Bc, qbb], [p2b])
                    return
                if KD == 3:
                    t1, t1b = self.tmpf.next()
                    sl = slice(tb * 512, (tb + 1) * 512)
                    self.dve(lambda: nc.vector.tensor_tensor(t1, p1, self.SCR[0][:, sl], ALU.mult), [p1b, self.SCRb[0]], [t1b])
                    return
                if KD == 6:
                    t1, t1b = self.tmpf.next()
                    self.dve(lambda: nc.vector.tensor_tensor(t1, self.cstf[:, 0:512], self.cstf[:, 0:512], ALU.mult), [self.Bc], [t1b])
                    return
                if KD == 7:
                    t1, t1b = self.tmpf.next()
                    self.dve(lambda: nc.vector.tensor_copy(t1, p1), [p1b], [t1b])
                    return
                if KD == 8:
                    t1, t1b = self.tmpf.next()
                    self.dve(lambda: nc.vector.tensor_tensor(qb, p1, self.cstf[:, 0:512], ALU.mult), [p1b, self.Bc], [qbb])
                    return
                if KD == 4:
                    t1, t1b = self.tmpf.next()
                    self.dve(lambda: nc.vector.tensor_tensor(t1, p1, self.cstf[:, 0:512], ALU.mult), [p1b, self.Bc], [t1b])
                    return
                if KD == 5:
                    t1, t1b = self.tmpf.next()
                    sl = slice(tb * 512, (tb + 1) * 512)
                    self.dve(lambda: nc.vector.tensor_tensor(t1, p1, self.SCR[0][:, sl], ALU.mult), [p1b, self.SCRb[0]], [t1b])
                    if c8 == 0 and tb == 1:
                        raise StopIteration
                    return
                p2, p2b = pmr.next()
                self.mm(p2, self.pm_b, qb, True, True, [self.Bc, qbb], [p2b])
                t1, t1b = self.tmpf.next()
                t2, t2b = self.tmpf.next()
                sl = slice(tb * 512, (tb + 1) * 512)
                self.dve(lambda: nc.vector.tensor_tensor(t1, p1, self.SCR[0][:, sl], ALU.mult), [p1b, self.SCRb[0]], [t1b])
                self.dve(lambda: nc.vector.tensor_tensor(t2, p2, self.SCR[1][:, sl], ALU.mult), [p2b, self.SCRb[1]], [t2b])
                self.dve(lambda: nc.vector.tensor_tensor(self.QK[:, c8, sl], t1, t2, ALU.add), [t1b, t2b], [self.QKb[c8][tb]])

            try:
                for g in (2, 3):
                    for c in range(4):
                        for tb in range(4):
                            p1, p1b = psr.next()
                            self._proj_fm(Ws[g], self.Wb[g], c, tb, p1, p1b)
                            qb, qbb = self.tmpb.next()
                            self.act(qb, p1, AF.Copy, [p1b], [qbb])
                            if pending is not None:
                                rope_tail(pending)
                            pending = ((g - 2) * 4 + c, tb, p1, p1b, qb, qbb)
                rope_tail(pending)
            except StopIteration:
                pass
            if self.stop == "l0_qk":
                return
            self._proj_v(Ws[4], self.Wb[4])
        else:
            for c in range(4):
                self.pool(lambda: nc.gpsimd.memset(self.AU[:, c, 0:16], 0.0), [], [self.AUb[c]])
            for c in range(4):
                for tb in range(4):
                    p1, p1b = psr.next()
                    self._proj_fm(Ws[3], Wbs[3], c, tb, p1, p1b)
                    self.act(self.AU[:, c, 16 + tb * 512:16 + (tb + 1) * 512], p1, AF.Copy, [p1b], [self.AUb[c]])
            self._pool_prefix()
            for g in (0, 1):
                for c in range(4):
                    for tb in range(4):
                        p1, p1b = psr.next()
                        self._proj_fm(Ws[g], Wbs[g], c, tb, p1, p1b)
                        self.act(self.QK[:, g * 4 + c, tb * 512:(tb + 1) * 512], p1, AF.Copy, [p1b], [self.QKb[g * 4 + c][tb]])
            self._proj_v(Ws[2], Wbs[2])

    def _conv_diag(self):
        nc = self.nc
        homes = [(self.Wblk[0], [self.Wb[0]]), (self.Wblk[1], [self.Wb[1]]), (self.Wblk[5], [self.Wb[5]]),
                 (self.ht, [self.htr.items[0][1], self.htr.items[1][1]])]
        self.Dg = []
        for c in range(4):
            home, bufs = homes[c]
            D3 = home[:, 0:31 * 128].rearrange("p (w j) -> p w j", w=31)
            self.dve(lambda: nc.vector.tensor_tensor(D3, self.ident_b.unsqueeze(1).broadcast_to([128, 31, 128]),
                                                     self.cw[:, c, :].unsqueeze(2).broadcast_to([128, 31, 128]), ALU.mult),
                     [self.Bc], bufs)
            self.Dg.append((D3, bufs))

    def _conv_pe(self):
        MIX = self.XT
        ps, psb = self.PS[3], self.PSb[3]
        for c in range(4):
            D3, dbufs = self.Dg[c]
            for tb in range(4):
                for w in range(31):
                    o = 2 + w + tb * 512
                    self.mm(ps, D3[:, w, :], self.AU[:, c, o:o + 512], w == 0, w == 30, dbufs + [self.AUb[c]], [psb])
                    yield
                self.act(MIX[:, c, tb * 512:(tb + 1) * 512], ps, AF.Identity, [psb, self.Bc], self.XTb[tb * 4:tb * 4 + 4],
                         bias=self.conv_b[:, c:c + 1])
                yield

    def _conv_ln(self):
        nc = self.nc
        MIX = self.XT
        sls = [slice(tb * 512, (tb + 1) * 512) for tb in range(4)]
        for tb in range(4):
            sl = sls[tb]
            xb4 = self.XTb[tb * 4:tb * 4 + 4]
            sum_ps, sum_b = self.PS[tb], self.PSb[tb]
            sq_ps, sq_b = self.PS[4 + tb], self.PSb[4 + tb]
            for c in range(4):
                sq, sqb = self.tmpb.next()
                self.act(sq, MIX[:, c, sl], AF.Square, xb4, [sqb])
                self.mm(sum_ps, self.ones_b, MIX[:, c, sl], c == 0, c == 3, [self.Bc] + xb4, [sum_b])
                self.mm(sq_ps, self.ones_b, sq, c == 0, c == 3, [self.Bc, sqb], [sq_b])
        for tb in range(4):
            sl = sls[tb]
            mean, var = self.SCR[0][:, sl], self.SCR[1][:, sl]
            self.act(mean, self.PS[tb], AF.Copy, [self.PSb[tb]], [self.SCRb[0]], scale=1.0 / 512)
            nmsq, nmsqb = self.tmpf.next()
            self.dve(lambda: nc.vector.scalar_tensor_tensor(nmsq, mean, -1.0, mean, ALU.mult, ALU.mult), [self.SCRb[0]], [nmsqb])
            self.dve(lambda: nc.vector.scalar_tensor_tensor(var, self.PS[4 + tb], 1.0 / 512, nmsq, ALU.mult, ALU.add), [self.PSb[4 + tb], nmsqb], [self.SCRb[1]])
            self.dve(lambda: nc.vector.tensor_scalar(var, var, LN_EPS, None, ALU.add), [self.SCRb[1]], [self.SCRb[1]])
            self.act(var, var, AF.Ln, [self.SCRb[1]], [self.SCRb[1]])
            self.act(var, var, AF.Exp, [self.SCRb[1]], [self.SCRb[1]], scale=-0.5)
        for tb in range(4):
            sl = sls[tb]
            xb4 = self.XTb[tb * 4:tb * 4 + 4]
            mean, var = self.SCR[0][:, sl], self.SCR[1][:, sl]
            for c in range(4):
                d, db = self.tmpf.next()
                self.dve(lambda: nc.vector.tensor_tensor(d, MIX[:, c, sl], mean, ALU.subtract), xb4 + [self.SCRb[0]], [db])
                self.dve(lambda: nc.vector.tensor_tensor(d, d, var, ALU.mult), [db, self.SCRb[1]], [db])
                self.act(MIX[:, c, sl], d, AF.Silu, [db, self.Bc], xb4, scale=self.cln_g[:, c:c + 1], bias=self.cln_b[:, c:c + 1])

    def _conv_branch(self):
        for _ in self._conv_pe():
            pass
        self._conv_ln()

    def _diff_attn(self, side=None):
        nc = self.nc
        MIX = self.XT
        QK, V = self.QK, self.V
        scr = self.psring([0, 1, 2])
        O = [(self.PS[4], self.PSb[4]), (self.PS[6], self.PSb[6])]

        def pull(n):
            if side is not None:
                for _ in range(n):
                    next(side, None)
        Dn = [(self.PS[5], self.PSb[5]), (self.PS[7], self.PSb[7])]
        deferred = []
        for h in range(4):
            for qb in range(4):
                nkt = 4 * (qb + 1)
                q0 = qb * 512
                steps = [(kt, j) for kt in range(nkt) for j in range(2)]

                def stage_a(kt, j):
                    i = kt - 4 * qb
                    c0 = 128 * i if i > 0 else 0
                    sp_, spb = scr.next()
                    pr = slice(64 * j, 64 * j + 64)
                    self.mm(sp_[:, c0:512], QK[pr, 4 + h, kt * 128:(kt + 1) * 128], QK[pr, h, q0 + c0:q0 + 512], True, True,
                            [self.QKb[4 + h][kt // 4], self.QKb[h][qb]], [spb])
                    pt, ptb = self.tmpb.next()
                    self.act(pt[:, c0:512], sp_[:, c0:512], AF.Exp, [spb], [ptb], scale=0.125)
                    if i >= 0:
                        self.pool(lambda: nc.gpsimd.tensor_tensor(pt[:, c0:c0 + 128], pt[:, c0:c0 + 128], self.mle_b, ALU.mult), [ptb, self.Bc], [ptb])
                    return (kt, j, c0, pt, ptb)

                def stage_b(kt, j, c0, pt, ptb):
                    self.mm(O[j][0][:, c0:512], V[:, kt, h * 128:(h + 1) * 128], pt[:, c0:512], kt == 0, kt == nkt - 1,
                            [self.Vb[kt], ptb], [O[j][1]])
                    self.mm(Dn[j][0][:, c0:512], self.ones_b, pt[:, c0:512], kt == 0, kt == nkt - 1, [self.Bc, ptb], [Dn[j][1]])

                pend = []
                for si, (kt, j) in enumerate(steps):
                    pend.append(stage_a(kt, j))
                    if len(pend) > 2:
                        stage_b(*pend.pop(0))
                    pull(1)
                    while deferred and deferred[0][0] <= si:
                        deferred.pop(0)[1]()
                while pend:
                    stage_b(*pend.pop(0))
                while deferred:
                    deferred.pop(0)[1]()
                os_ = []
                for j in range(2):
                    r, rb = self.tmpf.next()
                    self.act(r, Dn[j][0], AF.Ln, [Dn[j][1]], [rb])
                    self.act(r, r, AF.Exp, [rb], [rb], scale=-1.0)
                    o, ob = self.tmpf.next()
                    self.dve(lambda: nc.vector.tensor_tensor(o, O[j][0], r, ALU.mult), [O[j][1], rb], [ob])
                    os_.append((o, ob))
                (o1, o1b), (o2, o2b) = os_
                self.dve(lambda: nc.vector.scalar_tensor_tensor(o1, o2, self.neglam, o1, ALU.mult, ALU.add), [o2b, o1b, self.Bsmall], [o1b])
                st = {}

                def fin_a(o1=o1, o1b=o1b, st=st):
                    st["osq"], st["osqb"] = self.tmpb.next()
                    self.act(st["osq"], o1, AF.Square, [o1b], [st["osqb"]])

                def fin_b(st=st):
                    ss, ssb = scr.next()
                    self.mm(ss, self.ones_b, st["osq"], True, True, [self.Bc, st["osqb"]], [ssb])
                    st["rs"], st["rsb"] = self.tmpf.next()
                    rs = st["rs"]
                    self.dve(lambda: nc.vector.tensor_scalar(rs, ss, 1.0 / 128, RMS_EPS, ALU.mult, ALU.add), [ssb], [st["rsb"]])

                def fin_c(o1=o1, o1b=o1b, st=st, h=h, q0=q0, qb=qb):
                    rs, rsb = st["rs"], st["rsb"]
                    self.act(rs, rs, AF.Ln, [rsb], [rsb])
                    self.act(rs, rs, AF.Exp, [rsb], [rsb], scale=-0.5)
                    self.dve(lambda: nc.vector.scalar_tensor_tensor(MIX[:, 4 + h, q0:q0 + 512], o1, self.gsc, rs, ALU.mult, ALU.mult),
                             [o1b, rsb, self.Bsmall], self.XTb[qb * 4:qb * 4 + 4])

                deferred = [(2, fin_a), (4, fin_b), (6, fin_c)]
                pull(12)
        while deferred:
            deferred.pop(0)[1]()
        if side is not None:
            for _ in side:
                pass

    def _pool_prefix(self):
        nc = self.nc
        for g in range(4):
            U = self.AU[:, g, :]
            bufs = [(self.SCR[0], self.SCRb[0]), (self.SCR[1], self.SCRb[1])]
            cur, curb = bufs[0]
            self.dve(lambda: nc.vector.tensor_tensor(cur, U[:, 16:16 + S], U[:, 15:15 + S], ALU.add), [self.AUb[g]], [curb])
            k = 0
            sh = 2
            while sh < POOL_WINDOWS[g]:
                nxt, nxtb = bufs[(k + 1) % 2]
                self.dve(lambda: nc.vector.tensor_tensor(nxt[:, sh:S], cur[:, sh:S], cur[:, 0:S - sh], ALU.add), [curb], [nxtb])
                self.pool(lambda: nc.gpsimd.tensor_copy(nxt[:, 0:sh], cur[:, 0:sh]), [curb], [nxtb])
                cur, curb = nxt, nxtb
                k += 1
                sh *= 2
            win = POOL_WINDOWS[g]
            t, tb_ = self.tmpf.next()
            self.dve(lambda: nc.vector.tensor_tensor(t[:, 0:16], cur[:, 0:16], self.invc[:, g, :], ALU.mult), [curb, self.Bc], [tb_])
            self.dve(lambda: nc.vector.tensor_tensor(t[:, 0:16], t[:, 0:16], U[:, 16:32], ALU.subtract), [tb_, self.AUb[g]], [tb_])
            for tb in range(4):
                sl = slice(tb * 512, (tb + 1) * 512)
                usl = U[:, 16 + tb * 512:16 + (tb + 1) * 512]
                self.dve(lambda: nc.vector.scalar_tensor_tensor(usl, cur[:, sl], 1.0 / win, usl, ALU.mult, ALU.subtract),
                         [curb, self.AUb[g]], [self.AUb[g]])
            self.dve(lambda: nc.vector.tensor_copy(U[:, 16:32], t[:, 0:16]), [tb_], [self.AUb[g]])

    def _pool_mix(self):
        nc = self.nc
        MIX = self.XT
        pr = self.psring([4, 5, 6, 7])
        for g in range(4):
            for tb in range(4):
                sl = slice(tb * 512, (tb + 1) * 512)
                pm_ps, pm_b = pr.next()
                self.mm(pm_ps, self.pwb[:, g, :], self.AU[:, g, 16 + tb * 512:16 + (tb + 1) * 512], True, True, [self.Bc, self.AUb[g]], [pm_b])
                self.dve(lambda: nc.vector.tensor_scalar(MIX[:, 4 + g, sl], pm_ps, self.pscale[:, g:g + 1], None, ALU.mult),
                         [pm_b, self.Bc], self.XTb[tb * 4:tb * 4 + 4])

    def _sb_attn(self):
        nc = self.nc
        MIX = self.XT
        QK, V = self.QK, self.V
        self.fw.barrier()
        tf, tb_ = self.tf_full, self.tb_full
        fpr = Ring([tf[:, i * 1024:(i + 1) * 1024] for i in range(4)], "tfp")
        spr = Ring([tb_[:, i * 1024:(i + 1) * 1024] for i in range(3)], "tbp")
        apr = Ring([tb_[:, 3072 + i * 1024:3072 + (i + 1) * 1024] for i in range(2)], "tap")
        rpr = Ring([self.SCR[i // 2][:, (i % 2) * 1024:(i % 2 + 1) * 1024] for i in range(4)], "trp")
        zpairs = [(self.PSpair[0], [self.PSb[0], self.PSb[1]]), (self.PSpair[1], [self.PSb[2], self.PSb[3]])]
        zi = [0]
        Rp, Rb = self.PSpair[2], [self.PSb[4], self.PSb[5]]
        Or = self.psring([6])
        zeros = self.zeros_b
        self.pool(lambda: nc.gpsimd.memset(zeros, 0.0), [], [self.Bsmall])
        w = lambda ap, c0: ap.rearrange("p (h q) -> p h q", h=2)[:, :, c0:512]
        hv = lambda ap, hh: ap[:, hh * 512:(hh + 1) * 512]
        for c in range(4):
            for qb in range(4):
                q0 = qb * 512
                nkt = 4 * (qb + 1)
                o_ps, o_b = Or.next()
                for hh in range(2):
                    self.mm(hv(Rp, hh), zeros, QK[:, c, q0:q0 + 512], True, True, [self.Bsmall, self.QKb[c][qb]], [Rb[hh]], inc=True)
                kts = list(range(nkt - 1, -1, -1))

                def a_pe(idx, kt):
                    i = kt - 4 * qb
                    c0 = 128 * i if i > 0 else 0
                    zp, zb = zpairs[zi[0] % 2]
                    zi[0] += 1
                    for hh in range(2):
                        pr = slice(64 * hh, 64 * hh + 64)
                        self.mm(hv(zp, hh)[:, c0:512], QK[pr, 4 + c, kt * 128:(kt + 1) * 128], QK[pr, c, q0 + c0:q0 + 512], True, True,
                                [self.QKb[4 + c][kt // 4], self.QKb[c][qb]], [zb[hh]])
                    return dict(idx=idx, kt=kt, i=i, c0=c0, zp=zp, zb=zb)

                def a_act(st):
                    c0 = st["c0"]
                    e, eb = fpr.next()
                    self.act(w(e, c0), w(st["zp"], c0), AF.Exp, st["zb"], [eb], scale=0.125)
                    sp_, spb = spr.next()
                    self.act(w(sp_, c0), w(e, c0), AF.Ln, [eb], [spb], bias=1.0)
                    if st["i"] >= 0:
                        for hh in range(2):
                            blk = hv(sp_, hh)[:, c0:c0 + 128]
                            self.pool(lambda: nc.gpsimd.tensor_tensor(blk, blk, self.mlt_b, ALU.mult), [spb, self.Bc], [spb])
                    st["sp"], st["spb"] = sp_, spb

                def b_copy(st):
                    c0 = st["c0"]
                    rsb_, rsbb = rpr.next()
                    self.dve(lambda: nc.vector.tensor_copy(w(rsb_, c0), w(Rp, c0)), Rb, [rsbb])
                    st["rsb"], st["rsbb"] = rsb_, rsbb

                def b_pe(st):
                    c0 = st["c0"]
                    for hh in range(2):
                        sph = hv(st["sp"], hh)[:, c0:512]
                        self.mm(hv(st["zp"], hh)[:, c0:512], self.ntri_b, sph, False, True, [self.Bc, st["spb"]], [st["zb"][hh]], inc=True)
                    for hh in range(2):
                        sph = hv(st["sp"], hh)[:, c0:512]
                        self.mm(hv(Rp, hh)[:, c0:512], self.ones_b, sph, False, True, [self.Bc, st["spb"]], [Rb[hh]], inc=True)

                def b_dve(st):
                    c0 = st["c0"]
                    t, tbf = fpr.next()
                    self.dve(lambda: nc.vector.scalar_tensor_tensor(w(t, c0), w(st["zp"], c0), 0.125, w(st["rsb"], c0), ALU.mult, ALU.subtract),
                             st["zb"] + [st["rsbb"]], [tbf])
                    st["t"], st["tbf"] = t, tbf

                def b_act(st):
                    c0 = st["c0"]
                    a, ab = apr.next()
                    self.act(w(a, c0), w(st["t"], c0), AF.Exp, [st["tbf"]], [ab])
                    if st["i"] >= 0:
                        for hh in range(2):
                            blk = hv(a, hh)[:, c0:c0 + 128]
                            self.pool(lambda: nc.gpsimd.tensor_tensor(blk, blk, self.mlt_b, ALU.mult), [ab, self.Bc], [ab])
                    return (st["idx"], st["kt"], c0, a, ab)

                def stage_c(idx, kt, c0, a, ab):
                    for hh in range(2):
                        pr = slice(64 * hh, 64 * hh + 64)
                        h = 2 * c + hh
                        self.mm(o_ps[pr, c0:512], V[:, kt, h * 64:(h + 1) * 64], hv(a, hh)[:, c0:512], idx == 0, idx == nkt - 1,
                                [self.Vb[kt], ab], [o_b], inc=True)

                pa = None
                pb = None
                pc = None
                for idx in range(nkt + 3):
                    cur = a_pe(idx, kts[idx]) if idx < nkt else None
                    if cur is not None:
                        for _ in range(NFILL):
                            self.mm(self.PS[7], self.ones_b, QK[:, c, q0:q0 + 512], True, True, [self.Bc, self.QKb[c][qb]], [self.PSb[7]], inc=False)
                    if pa is not None:
                        b_pe(pa)
                        b_dve(pa)
                    if cur is not None:
                        b_copy(cur)
                    if pc is not None:
                        stage_c(*pc)
                    if cur is not None:
                        a_act(cur)
                    nc_ = b_act(pb) if pb is not None else None
                    pa, pb, pc = cur, pa, nc_
                self.act(MIX[:, c, q0:q0 + 512], o_ps, AF.Copy, [o_b], self.XTb[qb * 4:qb * 4 + 4])

    def _load_ln(self, idx, scale=None):
        nc, fw = self.nc, self.fw
        G = self.SCR[0][:, 0:1024]
        B_ = self.SCR[1][:, 0:1024]
        fw.dma(fw.sp, G, self.lnp_d[2 * idx].partition_broadcast(128), writes=[self.SCRb[0]])
        fw.dma(fw.sp, B_, self.lnp_d[2 * idx + 1].partition_broadcast(128), writes=[self.SCRb[1]])
        if scale is not None:
            self.act(G, G, AF.Copy, [self.SCRb[0]], [self.SCRb[0]], scale=scale)
            self.act(B_, B_, AF.Copy, [self.SCRb[1]], [self.SCRb[1]], scale=scale)
        return G, B_

    def _ln_stats(self, tt):
        nc = self.nc
        xs = self.X[:, tt, :]
        st_, stb = self.statr.next()
        xb = [self.Xb[tt]]
        self.dve(lambda: nc.vector.bn_stats(st_[:, 0:6], xs[:, 0:512]), xb, [stb])
        self.dve(lambda: nc.vector.bn_stats(st_[:, 6:12], xs[:, 512:1024]), xb, [stb])
        self.dve(lambda: nc.vector.bn_aggr(st_[:, 12:14], st_[:, 0:12]), [stb], [stb])
        self.dve(lambda: nc.vector.tensor_scalar(st_[:, 14:15], st_[:, 13:14], LN_EPS, None, ALU.add), [stb], [stb])
        self.act(st_[:, 14:15], st_[:, 14:15], AF.Ln, [stb], [stb])
        self.act(st_[:, 14:15], st_[:, 14:15], AF.Exp, [stb], [stb], scale=-0.5)
        return (tt, st_, stb)

    def _ln_apply(self, tt, st_, stb, G, B_):
        nc = self.nc
        xs = self.X[:, tt, :]
        xb = [self.Xb[tt]]
        self.dve(lambda: nc.vector.scalar_tensor_tensor(xs, xs, st_[:, 12:13], G, ALU.subtract, ALU.mult), xb + [stb, self.SCRb[0]], xb)
        self.dve(lambda: nc.vector.scalar_tensor_tensor(xs, xs, st_[:, 14:15], B_, ALU.mult, ALU.add), xb + [stb, self.SCRb[1]], xb)

    def _ln_tile(self, tt, G, B_):
        self._ln_apply(*self._ln_stats(tt), G, B_)

    def _load_wout(self, L):
        fw = self.fw
        for i in range(2):
            src = self.wout[L].rearrange("(c p) n -> p c n", p=128)[:, i * 4:(i + 1) * 4, :]
            dst = self.Wblk[3 + i].rearrange("p (c n) -> p c n", c=4)
            fw.dma(fw.pool, dst, src, writes=[self.Wb[3 + i]])

    def _mid(self, L):
        nc, fw = self.nc, self.fw
        MIX = self.XT
        inv = 1.0 / ALPHA
        Wo = [self.Wblk[3].rearrange("p (c n) -> p c n", c=4), self.Wblk[4].rearrange("p (c n) -> p c n", c=4)]
        W0 = self._expert_load(L, 0)
        G, B_ = self._load_ln(2 * L, scale=ALPHA)
        xsrc = self.x_d if L == 0 else self.spill_d
        for tt in range(NT):
            fw.dma(fw.sp, self.X[:, tt, :], xsrc[tt * 128:(tt + 1) * 128, :], writes=[self.Xb[tt]])
        psr = self.psring([5, 6])
        tpr = self.psring([0, 1])
        RB = [(self.PS[i], self.PSb[i]) for i in (2, 3, 4, 7)]

        def op_tile(tt):
            for half in range(2):
                ps, psb = psr.next()
                for fc in range(8):
                    self.mm(ps, MIX[:, fc, tt * 128:(tt + 1) * 128], Wo[fc // 4][:, fc % 4, half * 512:(half + 1) * 512], fc == 0, fc == 7,
                            [self.XTb[tt], self.Wb[3 + fc // 4]], [psb])
                xs = self.X[:, tt, half * 512:(half + 1) * 512]
                self.dve(lambda: nc.vector.scalar_tensor_tensor(xs, xs, ALPHA, ps, ALU.mult, ALU.add), [self.Xb[tt], psb], [self.Xb[tt]])
            st = self._ln_stats(tt)
            ln_flush()
            ln_pend.append(st)

        ln_pend = []

        def ln_flush():
            while ln_pend:
                self._ln_apply(*ln_pend.pop(0), G, B_)

        def xt_T(tb, dc):
            ps, psb = tpr.next()
            for j in range(4):
                tt = tb * 4 + j
                self.tr(ps[:, j * 128:(j + 1) * 128], self.X[:, tt, dc * 128:(dc + 1) * 128], self.ident_f,
                        [self.Xb[tt], self.Bc], [psb], inc=(j == 3))
            xf, xfb = self.tmpf.next()
            self.act(xf, ps, AF.Copy, [psb], [xfb], scale=inv)
            self.act(self.XT[:, dc, tb * 512:(tb + 1) * 512], ps, AF.Copy, [psb], self.XTb[tb * 4:tb * 4 + 4], scale=inv)
            return (tb, dc, xf, xfb)

        def xt_R(tb, dc, xf, xfb):
            for j in range(4):
                self.mm(RB[j][0][:, 0:NE], xf[:, j * 128:(j + 1) * 128], self.wr[:, dc, :], dc == 0, dc == 7, [self.Bc, xfb], [RB[j][1]])
            if dc == 7:
                for j in range(4):
                    tt = tb * 4 + j
                    self.act(self.lgt[:, tt * NE:(tt + 1) * NE], RB[j][0][:, 0:NE], AF.Copy, [RB[j][1]], [self.Brt])

        pend = []

        def xt_step(tb, dc):
            pend.append(xt_T(tb, dc))
            if len(pend) > 1:
                xt_R(*pend.pop(0))

        for tt in range(4):
            op_tile(tt)
        for tb in range(4):
            for j in range(4):
                if tb + 1 < 4:
                    op_tile((tb + 1) * 4 + j)
                else:
                    ln_flush()
                xt_step(tb, 2 * j)
                xt_step(tb, 2 * j + 1)
        while pend:
            xt_R(*pend.pop(0))
        return W0

    def _routing(self):
        nc = self.nc
        def wt():
            t, tb_ = self.tmpf.next()
            return t[:, 0:256], tb_
        aff, affb = wt()
        self.act(aff, self.lgt, AF.Sigmoid, [self.Brt], [affb])
        sel, selb = wt()
        v3 = lambda a: a.rearrange("p (t e) -> p t e", t=NT)
        v4 = lambda a: a.rearrange("p (t g j) -> p t g j", t=NT, g=4)
        g3 = lambda a: a.rearrange("p (t g) -> p t g", t=NT)
        self.dve(lambda: nc.vector.tensor_tensor(v3(sel), v3(aff), self.rbias.unsqueeze(1).broadcast_to([128, NT, NE]), ALU.add), [affb, self.Bc], [selb])
        sm = self.small
        m1 = sm[:, 8:8 + 64] if False else None
        m1t, m1b = self.tmpf.next()
        m1 = m1t[:, 0:64]
        m2 = m1t[:, 64:128]
        gs = m1t[:, 128:192]
        gmax = m1t[:, 192:208]
        gmask = m1t[:, 208:272]
        gsum = m1t[:, 272:288]
        self.dve(lambda: nc.vector.tensor_reduce(m1, sel.rearrange("p (tg j) -> p tg j", j=4), AX.X, ALU.max), [selb], [m1b])
        eq, eqb = wt()
        m1bc = m1.unsqueeze(2).broadcast_to([128, 64, 4])
        self.dve(lambda: nc.vector.tensor_tensor(eq.rearrange("p (tg j) -> p tg j", j=4), sel.rearrange("p (tg j) -> p tg j", j=4), m1bc, ALU.is_equal), [selb, m1b], [eqb])
        self.dve(lambda: nc.vector.scalar_tensor_tensor(eq, eq, -1.0e9, sel, ALU.mult, ALU.add), [eqb, selb], [eqb])
        self.dve(lambda: nc.vector.tensor_reduce(m2, eq.rearrange("p (tg j) -> p tg j", j=4), AX.X, ALU.max), [eqb], [m1b])
        self.dve(lambda: nc.vector.tensor_tensor(gs, m1, m2, ALU.add), [m1b], [m1b])
        self.dve(lambda: nc.vector.tensor_reduce(gmax, g3(gs), AX.X, ALU.max), [m1b], [m1b])
        self.dve(lambda: nc.vector.tensor_tensor(g3(gmask), g3(gs), gmax.unsqueeze(2).broadcast_to([128, NT, 4]), ALU.is_equal), [m1b], [m1b])
        ge, geb = wt()
        m2bc = m2.unsqueeze(2).broadcast_to([128, 64, 4])
        gmbc = gmask.unsqueeze(2).broadcast_to([128, 64, 4])
        j4 = lambda a: a.rearrange("p (tg j) -> p tg j", j=4)
        self.dve(lambda: nc.vector.tensor_tensor(j4(ge), j4(sel), m2bc, ALU.is_ge), [selb, m1b], [geb])
        self.dve(lambda: nc.vector.tensor_tensor(j4(ge), j4(ge), gmbc, ALU.mult), [geb, m1b], [geb])
        self.dve(lambda: nc.vector.tensor_tensor(ge, ge, aff, ALU.mult), [geb, affb], [geb])
        self.dve(lambda: nc.vector.tensor_reduce(gsum, v3(ge), AX.X, ALU.add), [geb], [m1b])
        self.dve(lambda: nc.vector.reciprocal(gsum, gsum), [m1b], [m1b])
        self.dve(lambda: nc.vector.tensor_tensor(v3(self.comb), v3(ge), gsum.unsqueeze(2).broadcast_to([128, NT, NE]), ALU.mult), [geb, m1b], [self.Bcomb])

    def _expert_load(self, L, e):
        fw = self.fw
        s = e % 2
        Wg = self.Wblk[3 * s].rearrange("p (c n) -> p c n", c=8)
        Wu = self.Wblk[3 * s + 1].rearrange("p (c n) -> p c n", c=8)
        Wd = self.Wblk[3 * s + 2].rearrange("p (c n) -> p c n", c=4)
        fw.dma(fw.pool, Wg, self.wg[L][e].rearrange("(c p) n -> p c n", p=128), writes=[self.Wb[3 * s]])
        fw.dma(fw.pool, Wu, self.wu[L][e].rearrange("(c p) n -> p c n", p=128), writes=[self.Wb[3 * s + 1]])
        fw.dma(fw.pool, Wd, self.wd[L][e].rearrange("(c p) n -> p c n", p=128), writes=[self.Wb[3 * s + 2]])
        return (Wg, Wu, Wd, s)

    def _experts(self, L, W0):
        nc, fw = self.nc, self.fw
        gr = self.psring([0, 1])
        ur = self.psring([2, 3])
        yr = self.psring([4, 5, 6, 7])
        comb3 = self.comb.rearrange("p (t e) -> p t e", t=NT)

        def load(e):
            s = e % 2
            Wg = self.Wblk[3 * s].rearrange("p (c n) -> p c n", c=8)
            Wu = self.Wblk[3 * s + 1].rearrange("p (c n) -> p c n", c=8)
            Wd = self.Wblk[3 * s + 2].rearrange("p (c n) -> p c n", c=4)
            fw.dma(fw.pool, Wg, self.wg[L][e].rearrange("(c p) n -> p c n", p=128), writes=[self.Wb[3 * s]])
            fw.dma(fw.pool, Wu, self.wu[L][e].rearrange("(c p) n -> p c n", p=128), writes=[self.Wb[3 * s + 1]])
            fw.dma(fw.pool, Wd, self.wd[L][e].rearrange("(c p) n -> p c n", p=128), writes=[self.Wb[3 * s + 2]])
            return (Wg, Wu, Wd, s)

        G_, B_ = self._load_ln(2 * L + 1)

        lnp = []

        def ln_fin():
            while lnp:
                st = lnp.pop(0)
                self._ln_apply(*st, G_, B_)
                if L == 1:
                    tt = st[0]
                    fw.dma(fw.sp, self.out_d[tt * 128:(tt + 1) * 128, :], self.X[:, tt, :], reads=[self.Xb[tt]])

        def ln_out(tt):
            st = self._ln_stats(tt)
            ln_fin()
            lnp.append(st)

        def stage_gu(e, tb, W, ln_tiles=()):
            Wg, Wu, Wd, s = W
            H, Hb = self.htr.next()
            xb4 = self.XTb[tb * 4:tb * 4 + 4]
            for fc in range(4):
                g, gb = gr.next()
                u, ub = ur.next()
                for dc in range(8):
                    self.mm(g, Wg[:, dc, fc * 128:(fc + 1) * 128], self.XT[:, dc, tb * 512:(tb + 1) * 512], dc == 0, dc == 7, [self.Wb[3 * s]] + xb4, [gb])
                for dc in range(8):
                    self.mm(u, Wu[:, dc, fc * 128:(fc + 1) * 128], self.XT[:, dc, tb * 512:(tb + 1) * 512], dc == 0, dc == 7, [self.Wb[3 * s + 1]] + xb4, [ub])
                sg, sgb = self.tmpf.next()
                self.act(sg, g, AF.Silu, [gb], [sgb])
                self.dve(lambda: nc.vector.tensor_tensor(H[:, fc, :], u, sg, ALU.mult), [ub, sgb], [Hb])
                if fc < len(ln_tiles):
                    ln_out(ln_tiles[fc])
            return (e, tb, W, H, Hb)

        def stage_dn(e, tb, W, H, Hb):
            Wg, Wu, Wd, s = W
            for t4 in range(4):
                tt = tb * 4 + t4
                for half in range(2):
                    y, yb = yr.next()
                    for fc in range(4):
                        self.mm(y, H[:, fc, t4 * 128:(t4 + 1) * 128], Wd[:, fc, half * 512:(half + 1) * 512], fc == 0, fc == 3, [Hb, self.Wb[3 * s + 2]], [yb])
                    xs = self.X[:, tt, half * 512:(half + 1) * 512]
                    self.dve(lambda: nc.vector.scalar_tensor_tensor(xs, y, comb3[:, tt, e:e + 1], xs, ALU.mult, ALU.add), [yb, self.Bcomb, self.Xb[tt]], [self.Xb[tt]])

        Wn = W0
        prev = None
        for e in range(NE):
            W = Wn
            for tb in range(4):
                if e == NE - 1:
                    stage_dn(*prev)
                    done = list(range((tb - 1) * 4, tb * 4)) if tb > 0 else []
                    cur = stage_gu(e, tb, W, done)
                else:
                    cur = stage_gu(e, tb, W)
                    if prev is not None:
                        stage_dn(*prev)
                prev = cur
                if tb == 0 and e + 1 < NE:
                    Wn = self._expert_load(L, e + 1)
                if tb == 0 and e + 1 == NE and L == 0:
                    self.pref_win = {g: self._load_w_in(1, g, self._win_slot(1)[g]) for g in (3, 0, 1)}
        stage_dn(*prev)
        for tt in range(12, 16):
            ln_out(tt)
        ln_fin()

    def _final_ln(self, L):
        pass


def _constants():
    j = np.arange(128)[:, None]
    k = np.arange(128)[None, :]
    ident = (j == k).astype(np.float32)
    tri = (j >= k).astype(np.float32)
    mle = (j <= k).astype(np.float32)
    mlt = (j < k).astype(np.float32)
    m = np.arange(128)
    perm = np.where((m % 64) < 32, m + 32, m - 32)
    pm = np.zeros((128, 128), np.float32)
    pm[perm, m] = 1.0
    invc = np.zeros((128, 4, 16), np.float32)
    for g, w in enumerate(POOL_WINDOWS):
        invc[:, g, :] = 1.0 / np.minimum(np.arange(16) + 1, w)
    cst = np.concatenate([ident, tri, mle, mlt, pm, invc.reshape(128, 64)], axis=1).astype(np.float32)
    inv = (10000.0 ** (-np.arange(0, 64, 2, dtype=np.float32) / 64)).astype(np.float32)
    ang = np.arange(S, dtype=np.float32)[:, None] * inv[None, :]
    ang = np.concatenate([ang, ang], axis=-1)
    cos = np.cos(ang).astype(np.float32).T
    sin = np.sin(ang).astype(np.float32).T
    sign = np.where(np.arange(64) < 32, -1.0, 1.0).astype(np.float32)[:, None]
    sins = sin * sign
    rope = np.stack([np.concatenate([cos, cos], 0), np.concatenate([sins, sins], 0)], 0).astype(np.float32)
    return cst, np.ascontiguousarray(rope)


def _pack_inputs(inp):
    f = lambda a: np.ascontiguousarray(np.asarray(a, dtype=np.float32))
    cst, rope = _constants()
    pp = np.zeros((128, 144), np.float32)
    pp[:, 0:124] = f(inp["l0_conv_w"]).T.reshape(4, 128, 31).transpose(1, 0, 2).reshape(128, 124)
    v4 = lambda a: f(a).reshape(4, 128).T
    pp[:, 124:128] = v4(inp["l0_conv_b"])
    pp[:, 128:132] = v4(inp["l0_conv_ln_g"])
    pp[:, 132:136] = v4(inp["l0_conv_ln_b"])
    pp[:, 136] = f(inp["l0_subln_g"])
    pp[:, 137:141] = v4(inp["l1_pool_scale"])
    rowp = np.concatenate([f(inp["l0_lambda_q1"]), f(inp["l0_lambda_q2"]), f(inp["l0_lambda_k1"]), f(inp["l0_lambda_k2"]),
                           f(inp["router_bias"])]).astype(np.float32)
    lnp = np.stack([f(inp["l0_ln_mix_g"]), f(inp["l0_ln_mix_b"]), f(inp["l0_ln_ffn_g"]), f(inp["l0_ln_ffn_b"]),
                    f(inp["l1_ln_mix_g"]), f(inp["l1_ln_mix_b"]), f(inp["l1_ln_ffn_g"]), f(inp["l1_ln_ffn_b"])], 0)
    shared = {
        "router_w": f(inp["router_w"]),
        "w_in0": f(inp["l0_in_proj"]), "w_in1": f(inp["l1_in_proj"]),
        "w_out0": f(inp["l0_out_proj"]), "w_out1": f(inp["l1_out_proj"]),
        "w_gate0": f(inp["l0_w_gate"]), "w_gate1": f(inp["l1_w_gate"]),
        "w_up0": f(inp["l0_w_up"]), "w_up1": f(inp["l1_w_up"]),
        "w_down0": f(inp["l0_w_down"]), "w_down1": f(inp["l1_w_down"]),
        "pool_w": f(inp["l1_pool_w"]),
        "pp": pp, "rowp": rowp, "lnp": np.ascontiguousarray(lnp), "cst": cst, "rope": rope,
    }
    x = f(inp["x"])
    return [dict(shared, x=np.ascontiguousarray(x[c])) for c in range(x.shape[0])]


def kernel(**inputs):
    in_maps = _pack_inputs(inputs)
    nc = Prog().build()
    res = run_bass_kernel_spmd(nc, in_maps, core_ids=list(range(8)))
    return np.stack([np.asarray(r["out"], dtype=np.float32) for r in res.results], axis=0)
```

```python
import contextlib
import math
import numpy as np
import concourse.bass as bass
import concourse.mybir as mybir
from concourse.alu_op_type import AluOpType as ALU
from concourse.bass_utils import run_bass_kernel_spmd

F32 = mybir.dt.float32
BF16 = mybir.dt.bfloat16
AF = mybir.ActivationFunctionType
AX = mybir.AxisListType

S = 2048
D = 1024
NT = 16
NE = 16
ALPHA = 4.0 ** 0.25
LN_EPS = 1e-5
RMS_EPS = 1e-5
LAM_INIT0 = 0.8 - 0.6 * math.exp(0.0)
POOL_WINDOWS = (2, 4, 8, 16)
NFILL = 3


import os as _os
TRACE = bool(_os.environ.get("KTRACE"))


class Sem:
    def __init__(self, h):
        self.h = h
        self.cnt = 0


class Buf:
    __slots__ = ("last_w", "readers", "name", "excl")

    def __init__(self, name="", excl=False):
        self.last_w = None
        self.readers = []
        self.name = name
        self.excl = excl


class Eng:
    def __init__(self, name, obj, sem, same_wait=True):
        self.name = name
        self.obj = obj
        self.sem = sem
        self.waited = {}
        self.same_wait = same_wait


class FW:
    def __init__(self, nc, stack, n_dma_sems=32):
        self.nc = nc
        def mk(n):
            s_ = Sem(stack.enter_context(nc.semaphore(n)))
            s_.name = n
            return s_
        self.pe = Eng("pe", nc.tensor, mk("s_pe"), same_wait=False)
        self.act = Eng("act", nc.scalar, mk("s_act"))
        self.dve = Eng("dve", nc.vector, mk("s_dve"))
        self.pool = Eng("pool", nc.gpsimd, mk("s_pool"))
        self.sp = Eng("sp", nc.sync, mk("s_sp"))
        self.engs = [self.pe, self.act, self.dve, self.pool, self.sp]
        self.dma_sems = [mk(f"s_dma{i}") for i in range(n_dma_sems)]
        self.dma_i = 0
        self.dma_qi = [0, 0]
        self.n_instr = 0
        self.pe_pending = False

    def _wait(self, eng, tok):
        sem, val = tok
        if sem is eng.sem and not eng.same_wait:
            return
        if eng.waited.get(id(sem), 0) >= val:
            return
        eng.obj.wait_ge(sem.h, val)
        eng.waited[id(sem)] = val
        if TRACE:
            print("   wait", eng.name, "on", getattr(sem, "name", "?"), val)

    def _deps(self, eng, reads, writes):
        for b in reads:
            if b.last_w is not None:
                self._wait(eng, b.last_w)
            if b.excl:
                for t in b.readers:
                    if t[0] is not eng.sem:
                        self._wait(eng, t)
        for b in writes:
            if b.last_w is not None:
                self._wait(eng, b.last_w)
            for t in b.readers:
                self._wait(eng, t)

    @staticmethod
    def _commit(tok, reads, writes):
        for b in reads:
            if len(b.readers) > 24:
                best = {}
                for s_, v_ in b.readers:
                    if best.get(id(s_), (None, -1))[1] < v_:
                        best[id(s_)] = (s_, v_)
                b.readers = list(best.values())
            b.readers.append(tok)
        for b in writes:
            b.last_w = tok
            b.readers = []

    def op(self, eng, fn, reads=(), writes=(), inc=True):
        if TRACE:
            print("op", eng.name, "cnt", eng.sem.cnt, "reads", [b.name for b in reads], "writes", [b.name for b in writes], "inc", inc)
        self._deps(eng, reads, writes)
        ins = fn()
        self.n_instr += 1
        if inc:
            eng.sem.cnt += 1
            ins.then_inc(eng.sem.h, 1)
            tok = (eng.sem, eng.sem.cnt)
            if eng is self.pe:
                self.pe_pending = False
        else:
            assert eng is self.pe
            tok = (eng.sem, eng.sem.cnt + 1)
            self.pe_pending = True
        self._commit(tok, reads, writes)
        return ins

    def dma(self, q, out, in_, reads=(), writes=(), **kw):
        half = len(self.dma_sems) // 2
        k = 0 if q is self.sp else 1
        sem = self.dma_sems[k * half + self.dma_qi[k] % half]
        self.dma_qi[k] += 1
        self.dma_i += 1
        if sem.cnt:
            self._wait(q, (sem, sem.cnt))
        self._deps(q, reads, writes)
        ins = q.obj.dma_start(out=out, in_=in_, **kw)
        sem.cnt += 16
        ins.then_inc(sem.h, 16)
        self.n_instr += 1
        tok = (sem, sem.cnt)
        self._commit(tok, reads, writes)
        return tok

    def barrier(self):
        assert not self.pe_pending
        for e in self.engs:
            for o in self.engs:
                if o is not e and o.sem.cnt:
                    self._wait(e, (o.sem, o.sem.cnt))
            for s in self.dma_sems:
                if s.cnt:
                    self._wait(e, (s, s.cnt))


class Ring:
    def __init__(self, aps, name="r"):
        self.items = [(ap, Buf(f"{name}{i}")) for i, ap in enumerate(aps)]
        self.i = 0

    def next(self):
        it = self.items[self.i % len(self.items)]
        self.i += 1
        return it


class Prog:
    def __init__(self, stop=None, taps=()):
        self.stop = stop
        self.taps = set(taps)
        self.dbg_specs = {}

    def build(self):
        nc = bass.Bass("TRN2", target_bir_lowering=False)
        self.nc = nc
        di = lambda n, s: nc.dram_tensor(n, list(s), F32, kind="ExternalInput").ap()
        self.x_d = di("x", [S, D])
        self.router_w = di("router_w", [D, NE])
        self.win = [di("w_in0", [D, 2560]), di("w_in1", [D, 2048])]
        self.wout = [di("w_out0", [D, D]), di("w_out1", [D, D])]
        self.wg = [di("w_gate0", [NE, D, 512]), di("w_gate1", [NE, D, 512])]
        self.wu = [di("w_up0", [NE, D, 512]), di("w_up1", [NE, D, 512])]
        self.wd = [di("w_down0", [NE, 512, D]), di("w_down1", [NE, 512, D])]
        self.pool_w = di("pool_w", [4, 128, 128])
        self.pp_d = di("pp", [128, 144])
        self.rowp_d = di("rowp", [272])
        self.lnp_d = di("lnp", [8, D])
        self.cst_d = di("cst", [128, 128 * 5 + 64])
        self.rope_d = di("rope", [2, 128, S])
        self.out_d = nc.dram_tensor("out", [S, D], F32, kind="ExternalOutput").ap()
        self.spill_d = nc.dram_tensor("xspill", [S, D], F32, kind="Internal").ap()
        with contextlib.ExitStack() as st:
            self.st = st
            self.fw = FW(nc, st)
            self._alloc()
            self._run()
        return nc

    def _alloc(self):
        nc, st = self.nc, self.st
        sb = lambda n, s, d: st.enter_context(nc.sbuf_tensor("sb_" + n, s, d))
        self.XT = sb("xt", [128, 8 * S], BF16)[:].rearrange("p (c t) -> p c t", c=8)
        self.XTb = [Buf(f"xt{t}") for t in range(NT)]
        bigf = sb("big", [128, 16640], F32)[:]
        self.X = bigf[:, 0:16384].rearrange("p (t d) -> p t d", t=NT)
        self.Xb = [Buf(f"x{t}") for t in range(NT)]
        bigb = bigf.bitcast(BF16)
        self.QK = bigb[:, 0:16384].rearrange("p (c t) -> p c t", c=8)
        self.QKb = [[Buf(f"qk{c}_{tb}") for tb in range(4)] for c in range(8)]
        self.V = bigb[:, 16384:24576].rearrange("p (t f) -> p t f", t=NT)
        self.Vb = [Buf(f"v{t}") for t in range(NT)]
        self.AU = bigb[:, 24576:32896].rearrange("p (c t) -> p c t", c=4)
        self.AUb = [Buf(f"au{c}") for c in range(4)]
        wb = sb("w", [128, 24576], BF16)[:]
        self.Wblk = [wb[:, i * 4096:(i + 1) * 4096] for i in range(6)]
        self.Wb = [Buf(f"w{i}") for i in range(6)]
        scr = sb("scr", [128, 4096], F32)[:]
        self.SCR = [scr[:, 0:2048], scr[:, 2048:4096]]
        self.SCRb = [Buf("scrA"), Buf("scrB")]
        tf = sb("tmpf", [128, 8 * 512], F32)[:]
        self.tmpf = Ring([tf[:, i * 512:(i + 1) * 512] for i in range(8)], "tf")
        tb_ = sb("tmpb", [128, 10 * 512], BF16)[:]
        self.tmpb = Ring([tb_[:, i * 512:(i + 1) * 512] for i in range(6)], "tb")
        self.tmpb2 = Ring([tb_[:, i * 512:(i + 1) * 512] for i in range(6, 10)], "tb2")
        self.tf_full, self.tb_full = tf, tb_
        self.ht = sb("ht", [128, 2 * 2048], BF16)[:]
        self.htr = Ring([self.ht[:, i * 2048:(i + 1) * 2048].rearrange("p (c t) -> p c t", c=4) for i in range(2)], "ht")
        self.cstf = sb("cstf", [128, 128 * 5 + 64], F32)[:]
        self.cstb = sb("cstb", [128, 128 * 7], BF16)[:]
        self.Bc = Buf("const")
        self.pp = sb("pp", [128, 144], F32)[:]
        self.rowp = sb("rowp", [128, 272], F32)[:]
        self.small = sb("small", [128, 64], F32)[:]
        self.Bsmall = Buf("small")
        self.comb = sb("comb", [128, NT * NE], F32)[:]
        self.Bcomb = Buf("comb")
        self.zeros_b = sb("zeros", [128, 128], BF16)[:]
        self.rt_halves = [scr[0:16, 1024:2048], scr[0:16, 3072:4096]]
        self.lgt = scr[:, 1024:1024 + NT * NE]
        self.Brt = Buf("rt")
        stt = sb("stats", [128, 4 * 16], F32)[:]
        self.statr = Ring([stt[:, i * 16:(i + 1) * 16] for i in range(4)], "stat")
        self.wr = sb("wr", [128, 8 * NE], F32)[:].rearrange("p (c e) -> p c e", c=8)
        self.pwb = sb("pwb", [128, 4 * 128], BF16)[:].rearrange("p (g d) -> p g d", g=4)
        self.PS = []
        self.PSb = []
        self.PSpair = []
        for k in range(4):
            pair = st.enter_context(nc.psum_tensor(f"pp{k}", [128, 1024], F32))[:]
            self.PSpair.append(pair)
            for h in range(2):
                self.PS.append(pair[:, h * 512:(h + 1) * 512])
                self.PSb.append(Buf(f"ps{2 * k + h}", excl=True))

    def psring(self, idxs):
        r = Ring([self.PS[i] for i in idxs])
        r.items = [(self.PS[i], self.PSb[i]) for i in idxs]
        return r

    def mm(self, out, lhsT, rhs, start, stop, reads, writes, inc=None):
        nc = self.nc
        inc = stop if inc is None else inc
        self.fw.op(self.fw.pe, lambda: nc.tensor.matmul(out, lhsT, rhs, start=start, stop=stop), reads, writes, inc=inc)

    def tr(self, out, in_, ident, reads, writes, inc=True):
        nc = self.nc
        self.fw.op(self.fw.pe, lambda: nc.tensor.transpose(out, in_, ident), reads, writes, inc=inc)

    def act(self, out, in_, func, reads, writes, **kw):
        nc = self.nc
        self.fw.op(self.fw.act, lambda: nc.scalar.activation(out, in_, func, **kw), reads, writes)

    def dve(self, fn, reads, writes):
        self.fw.op(self.fw.dve, fn, reads, writes)

    def pool(self, fn, reads, writes):
        self.fw.op(self.fw.pool, fn, reads, writes)

    def tap(self, name, ap, bufs, dtype=F32):
        if name not in self.taps:
            return
        nc = self.nc
        shape = list(ap.shape)
        d = nc.dram_tensor("dbg_" + name, shape, dtype, kind="ExternalOutput").ap()
        self.dbg_specs[name] = (shape, dtype)
        self.fw.barrier()
        self.fw.dma(self.fw.sp, d, ap, reads=bufs)
        self.fw.barrier()

    def _run(self):
        self._consts()
        for L in range(2):
            if L == 0:
                self._xt_from_dram()
            else:
                self._xt_from_x(router=False, spill=True)
                self.fw.barrier()
            if self.stop == f"l{L}_xt":
                return self._finish()
            self._in_proj(L)
            if self.stop in (f"l{L}_inproj", "l0_glu", "l0_qk"):
                return self._finish()
            self._load_wout(L)
            if L == 0:
                if self.stop == "l0_conv":
                    self._conv_branch()
                    return self._finish()
                self._diff_attn(self._conv_pe())
                self._conv_ln()
            else:
                self._pool_mix()
                self._sb_attn()
            if self.stop == f"l{L}_mix":
                return self._finish()
            self.fw.barrier()
            W0 = self._mid(L)
            self._routing()
            if self.stop == f"l{L}_route":
                return self._finish()
            self._experts(L, W0)
            self._final_ln(L)
            if self.stop == f"l{L}_moe":
                return self._finish()
        self._finish()

    def _finish(self):
        self.fw.barrier()
        allb = self.XTb + self.Xb + self.Vb + self.AUb + [b for r in self.QKb for b in r] + [self.Bcomb, self.Brt, self.Bsmall]
        self.tap("XT", self.XT, allb, BF16)
        self.tap("QK", self.QK, allb, BF16)
        self.tap("V", self.V, allb, BF16)
        self.tap("AU", self.AU, allb, BF16)
        self.tap("X", self.X, allb, F32)
        self.tap("comb", self.comb, allb, F32)
        self.tap("small", self.small, allb, F32)
        self.fw.barrier()

    def _consts(self):
        nc, fw = self.nc, self.fw
        fw.dma(fw.sp, self.cstf, self.cst_d[:, :], writes=[self.Bc])
        fw.dma(fw.sp, self.pp, self.pp_d[:, :], writes=[self.Bc])
        fw.dma(fw.sp, self.rowp, self.rowp_d.partition_broadcast(128), writes=[self.Bc])
        fw.dma(fw.sp, self.wr, self.router_w.rearrange("(c p) e -> p c e", p=128), writes=[self.Bc])
        fw.dma(fw.pool, self.pwb, self.pool_w.rearrange("g c d -> c g d"), writes=[self.Bc])
        self.dve(lambda: nc.vector.tensor_copy(self.cstb[:, 0:640], self.cstf[:, 0:640]), [self.Bc], [self.Bc])
        self.dve(lambda: nc.vector.memset(self.cstb[:, 640:768], 1.0), [], [self.Bc])
        self.dve(lambda: nc.vector.tensor_scalar(self.cstb[:, 768:896], self.cstf[:, 128:256], -8.0, None, ALU.mult), [self.Bc], [self.Bc])
        self.ident_f = self.cstf[:, 0:128]
        self.ident_b = self.cstb[:, 0:128]
        self.tri_b = self.cstb[:, 128:256]
        self.mle_b = self.cstb[:, 256:384]
        self.mlt_b = self.cstb[:, 384:512]
        self.pm_b = self.cstb[:, 512:640]
        self.ones_b = self.cstb[:, 640:768]
        self.ntri_b = self.cstb[:, 768:896]
        self.invc = self.cstf[:, 640:704].rearrange("p (g t) -> p g t", g=4)
        self.cw = self.pp[:, 0:124].rearrange("p (c w) -> p c w", c=4)
        self.conv_b = self.pp[:, 124:128]
        self.cln_g = self.pp[:, 128:132]
        self.cln_b = self.pp[:, 132:136]
        self.subln = self.pp[:, 136:137]
        self.pscale = self.pp[:, 137:141]
        sm = self.small
        lam4 = self.rowp[:, 0:256].rearrange("p (a d) -> p a d", a=4)
        tmp, tmpb_ = self.tmpf.next()
        self.dve(lambda: nc.vector.tensor_tensor(tmp[:, 0:128].rearrange("p (a d) -> p a d", a=2), lam4[:, 0:2, :], lam4[:, 2:4, :], ALU.mult), [self.Bc], [tmpb_])
        self.dve(lambda: nc.vector.tensor_reduce(sm[:, 2:4], tmp[:, 0:128].rearrange("p (a d) -> p a d", a=2), AX.X, ALU.add), [tmpb_], [self.Bsmall])
        self.act(sm[:, 4:6], sm[:, 2:4], AF.Exp, [self.Bsmall], [self.Bsmall])
        self.dve(lambda: nc.vector.tensor_tensor(sm[:, 6:7], sm[:, 5:6], sm[:, 4:5], ALU.subtract), [self.Bsmall], [self.Bsmall])
        self.dve(lambda: nc.vector.tensor_scalar(sm[:, 0:1], sm[:, 6:7], -LAM_INIT0, None, ALU.add), [self.Bsmall], [self.Bsmall])
        self.dve(lambda: nc.vector.tensor_scalar(sm[:, 1:2], self.subln, 1.0 - LAM_INIT0, None, ALU.mult), [self.Bc], [self.Bsmall])
        self.neglam = sm[:, 0:1]
        self.gsc = sm[:, 1:2]
        self.rbias = self.rowp[:, 256:272]

    def _xt_from_dram(self):
        nc, fw = self.nc, self.fw
        psr = self.psring([0, 1, 2, 3])
        k = 0
        for tt in range(NT):
            half = tt % 2
            for hh in range(2):
                pass
            xt_ap = self.SCR[half][:, 0:1024]
            fw.dma(fw.sp, xt_ap, self.x_d[tt * 128:(tt + 1) * 128, :], writes=[self.SCRb[half]])
            for dg in range(2):
                ps, psb = psr.next()
                for j in range(4):
                    dc = dg * 4 + j
                    self.tr(ps[:, j * 128:(j + 1) * 128], xt_ap[:, dc * 128:(dc + 1) * 128], self.ident_f,
                            [self.SCRb[half], self.Bc], [psb], inc=(j == 3))
                dst = self.XT[:, dg * 4:(dg + 1) * 4, tt * 128:(tt + 1) * 128]
                src = ps.rearrange("p (c t) -> p c t", c=4)
                if k % 2 == 0:
                    self.act(dst, src, AF.Copy, [psb], [self.XTb[tt]])
                else:
                    self.dve(lambda: nc.vector.tensor_copy(dst, src), [psb], [self.XTb[tt]])
                k += 1

    def _xt_from_x(self, router, spill, inv=1.0):
        nc, fw = self.nc, self.fw
        psr = self.psring([0, 1])
        lg_ps, lg_b = self.PS[2], self.PSb[2]
        if spill:
            for tt in range(NT):
                fw.dma(fw.sp, self.spill_d[tt * 128:(tt + 1) * 128, :], self.X[:, tt, :], reads=[self.Xb[tt]])
        for tb in range(4):
            for dc in range(8):
                ps, psb = psr.next()
                for j in range(4):
                    tt = tb * 4 + j
                    self.tr(ps[:, j * 128:(j + 1) * 128], self.X[:, tt, dc * 128:(dc + 1) * 128], self.ident_f,
                            [self.Xb[tt], self.Bc], [psb], inc=(j == 3))
                dst = self.XT[:, dc, tb * 512:(tb + 1) * 512]
                self.act(dst, ps, AF.Copy, [psb], self.XTb[tb * 4:tb * 4 + 4], scale=inv)
                if router:
                    xf, xfb = self.tmpf.next()
                    self.act(xf, ps, AF.Copy, [psb], [xfb], scale=inv)
                    self.mm(lg_ps[0:16, :], self.wr[:, dc, :], xf, dc == 0, dc == 7, [self.Bc, xfb], [lg_b])
            if router:
                self.dve(lambda: nc.vector.tensor_copy(self.rt_halves[tb // 2][:, (tb % 2) * 512:(tb % 2 + 1) * 512], lg_ps[0:16, :]), [lg_b], [self.Brt])

    @staticmethod
    def _win_slot(L):
        return {0: 0, 1: 1, 2: 2, 3: 3, 4: 4} if L == 0 else {3: 0, 0: 1, 1: 2, 2: 3}

    def _load_w_in(self, L, g, slot):
        fw = self.fw
        src = self.win[L].rearrange("(c p) n -> p c n", p=128)[:, :, g * 512:(g + 1) * 512]
        dst = self.Wblk[slot].rearrange("p (c n) -> p c n", c=8)
        fw.dma(fw.pool, dst, src, writes=[self.Wb[slot]])
        return dst

    def _proj_fm(self, W, wbuf, c, tb, ps, psb):
        for dc in range(8):
            self.mm(ps, W[:, dc, c * 128:(c + 1) * 128], self.XT[:, dc, tb * 512:(tb + 1) * 512], dc == 0, dc == 7,
                    [wbuf] + self.XTb[tb * 4:tb * 4 + 4], [psb])

    def _proj_v(self, W, wbuf):
        nc = self.nc
        psr = self.psring([0, 1, 2, 3])
        for tt in range(NT):
            ps, psb = psr.next()
            for dc in range(8):
                self.mm(ps, self.XT[:, dc, tt * 128:(tt + 1) * 128], W[:, dc, :], dc == 0, dc == 7, [wbuf, self.XTb[tt]], [psb])
            if tt % 2 == 0:
                self.act(self.V[:, tt, :], ps, AF.Copy, [psb], [self.Vb[tt]])
            else:
                self.dve(lambda: nc.vector.tensor_copy(self.V[:, tt, :], ps), [psb], [self.Vb[tt]])

    def _in_proj(self, L):
        nc, fw = self.nc, self.fw
        ng = 5 if L == 0 else 4
        pref = getattr(self, "pref_win", {}) if L == 1 else {}
        slot_of = self._win_slot(L)
        Ws = [pref[g] if g in pref else self._load_w_in(L, g, slot_of[g]) for g in range(ng)]
        Wbs = [self.Wb[slot_of[g]] for g in range(ng)]
        psr = self.psring([0, 1, 2, 3])
        if L == 0:
            fw.dma(fw.sp, self.SCR[0], self.rope_d[0], writes=[self.SCRb[0]])
            fw.dma(fw.sp, self.SCR[1], self.rope_d[1], writes=[self.SCRb[1]])
            for c in range(4):
                self.pool(lambda: nc.gpsimd.memset(self.AU[:, c, 0:32], 0.0), [], [self.AUb[c]])
            for c in range(4):
                for tb in range(4):
                    pv, pvb = psr.next()
                    pg, pgb = psr.next()
                    self._proj_fm(Ws[0], self.Wb[0], c, tb, pv, pvb)
                    self._proj_fm(Ws[1], self.Wb[1], c, tb, pg, pgb)
                    sg, sgb = self.tmpf.next()
                    self.act(sg, pg, AF.Sigmoid, [pgb], [sgb])
                    dst = self.AU[:, c, 32 + tb * 512:32 + (tb + 1) * 512]
                    self.dve(lambda: nc.vector.tensor_tensor(dst, pv, sg, ALU.mult), [pvb, sgb], [self.AUb[c]])
            if self.stop == "l0_glu":
                return
            self._conv_diag()
            pmr = self.psring([4, 5])
            pending = None

            import os
            KD = int(os.environ.get("KDBG", "0"))

            def rope_tail(item):
                c8, tb, p1, p1b, qb, qbb = item
                if KD == 1:
                    return
                if KD == 2:
                    p2, p2b = pmr.next()
                    self.mm(p2, self.pm_b, qb, True, True, [self.Bc, qbb], [p2b])
                    return
                if KD == 3:
                    t1, t1b = self.tmpf.next()
                    sl = slice(tb * 512, (tb + 1) * 512)
                    self.dve(lambda: nc.vector.tensor_tensor(t1, p1, self.SCR[0][:, sl], ALU.mult), [p1b, self.SCRb[0]], [t1b])
                    return
                if KD == 6:
                    t1, t1b = self.tmpf.next()
                    self.dve(lambda: nc.vector.tensor_tensor(t1, self.cstf[:, 0:512], self.cstf[:, 0:512], ALU.mult), [self.Bc], [t1b])
                    return
                if KD == 7:
                    t1, t1b = self.tmpf.next()
                    self.dve(lambda: nc.vector.tensor_copy(t1, p1), [p1b], [t1b])
                    return
                if KD == 8:
                    t1, t1b = self.tmpf.next()
                    self.dve(lambda: nc.vector.tensor_tensor(qb, p1, self.cstf[:, 0:512], ALU.mult), [p1b, self.Bc], [qbb])
                    return
                if KD == 4:
                    t1, t1b = self.tmpf.next()
                    self.dve(lambda: nc.vector.tensor_tensor(t1, p1, self.cstf[:, 0:512], ALU.mult), [p1b, self.Bc], [t1b])
                    return
                if KD == 5:
                    t1, t1b = self.tmpf.next()
                    sl = slice(tb * 512, (tb + 1) * 512)
                    self.dve(lambda: nc.vector.tensor_tensor(t1, p1, self.SCR[0][:, sl], ALU.mult), [p1b, self.SCRb[0]], [t1b])
                    if c8 == 0 and tb == 1:
                        raise StopIteration
                    return
                p2, p2b = pmr.next()
                self.mm(p2, self.pm_b, qb, True, True, [self.Bc, qbb], [p2b])
                t1, t1b = self.tmpf.next()
                t2, t2b = self.tmpf.next()
                sl = slice(tb * 512, (tb + 1) * 512)
                self.dve(lambda: nc.vector.tensor_tensor(t1, p1, self.SCR[0][:, sl], ALU.mult), [p1b, self.SCRb[0]], [t1b])
                self.dve(lambda: nc.vector.tensor_tensor(t2, p2, self.SCR[1][:, sl], ALU.mult), [p2b, self.SCRb[1]], [t2b])
                self.dve(lambda: nc.vector.tensor_tensor(self.QK[:, c8, sl], t1, t2, ALU.add), [t1b, t2b], [self.QKb[c8][tb]])

            try:
                for g in (2, 3):
                    for c in range(4):
                        for tb in range(4):
                            p1, p1b = psr.next()
                            self._proj_fm(Ws[g], self.Wb[g], c, tb, p1, p1b)
                            qb, qbb = self.tmpb.next()
                            self.act(qb, p1, AF.Copy, [p1b], [qbb])
                            if pending is not None:
                                rope_tail(pending)
                            pending = ((g - 2) * 4 + c, tb, p1, p1b, qb, qbb)
                rope_tail(pending)
            except StopIteration:
                pass
            if self.stop == "l0_qk":
                return
            self._proj_v(Ws[4], self.Wb[4])
        else:
            for c in range(4):
                self.pool(lambda: nc.gpsimd.memset(self.AU[:, c, 0:16], 0.0), [], [self.AUb[c]])
            for c in range(4):
                for tb in range(4):
                    p1, p1b = psr.next()
                    self._proj_fm(Ws[3], Wbs[3], c, tb, p1, p1b)
                    self.act(self.AU[:, c, 16 + tb * 512:16 + (tb + 1) * 512], p1, AF.Copy, [p1b], [self.AUb[c]])
            self._pool_prefix()
            for g in (0, 1):
                for c in range(4):
                    for tb in range(4):
                        p1, p1b = psr.next()
                        self._proj_fm(Ws[g], Wbs[g], c, tb, p1, p1b)
                        self.act(self.QK[:, g * 4 + c, tb * 512:(tb + 1) * 512], p1, AF.Copy, [p1b], [self.QKb[g * 4 + c][tb]])
            self._proj_v(Ws[2], Wbs[2])

    def _conv_diag(self):
        nc = self.nc
        homes = [(self.Wblk[0], [self.Wb[0]]), (self.Wblk[1], [self.Wb[1]]), (self.Wblk[5], [self.Wb[5]]),
                 (self.ht, [self.htr.items[0][1], self.htr.items[1][1]])]
        self.Dg = []
        for c in range(4):
            home, bufs = homes[c]
            D3 = home[:, 0:31 * 128].rearrange("p (w j) -> p w j", w=31)
            self.dve(lambda: nc.vector.tensor_tensor(D3, self.ident_b.unsqueeze(1).broadcast_to([128, 31, 128]),
                                                     self.cw[:, c, :].unsqueeze(2).broadcast_to([128, 31, 128]), ALU.mult),
                     [self.Bc], bufs)
            self.Dg.append((D3, bufs))

    def _conv_pe(self):
        MIX = self.XT
        ps, psb = self.PS[3], self.PSb[3]
        for c in range(4):
            D3, dbufs = self.Dg[c]
            for tb in range(4):
                for w in range(31):
                    o = 2 + w + tb * 512
                    self.mm(ps, D3[:, w, :], self.AU[:, c, o:o + 512], w == 0, w == 30, dbufs + [self.AUb[c]], [psb])
                    yield
                self.act(MIX[:, c, tb * 512:(tb + 1) * 512], ps, AF.Identity, [psb, self.Bc], self.XTb[tb * 4:tb * 4 + 4],
                         bias=self.conv_b[:, c:c + 1])
                yield

    def _conv_ln(self):
        nc = self.nc
        MIX = self.XT
        sls = [slice(tb * 512, (tb + 1) * 512) for tb in range(4)]
        for tb in range(4):
            sl = sls[tb]
            xb4 = self.XTb[tb * 4:tb * 4 + 4]
            sum_ps, sum_b = self.PS[tb], self.PSb[tb]
            sq_ps, sq_b = self.PS[4 + tb], self.PSb[4 + tb]
            for c in range(4):
                sq, sqb = self.tmpb.next()
                self.act(sq, MIX[:, c, sl], AF.Square, xb4, [sqb])
                self.mm(sum_ps, self.ones_b, MIX[:, c, sl], c == 0, c == 3, [self.Bc] + xb4, [sum_b])
                self.mm(sq_ps, self.ones_b, sq, c == 0, c == 3, [self.Bc, sqb], [sq_b])
        for tb in range(4):
            sl = sls[tb]
            mean, var = self.SCR[0][:, sl], self.SCR[1][:, sl]
            self.act(mean, self.PS[tb], AF.Copy, [self.PSb[tb]], [self.SCRb[0]], scale=1.0 / 512)
            nmsq, nmsqb = self.tmpf.next()
            self.dve(lambda: nc.vector.scalar_tensor_tensor(nmsq, mean, -1.0, mean, ALU.mult, ALU.mult), [self.SCRb[0]], [nmsqb])
            self.dve(lambda: nc.vector.scalar_tensor_tensor(var, self.PS[4 + tb], 1.0 / 512, nmsq, ALU.mult, ALU.add), [self.PSb[4 + tb], nmsqb], [self.SCRb[1]])
            self.dve(lambda: nc.vector.tensor_scalar(var, var, LN_EPS, None, ALU.add), [self.SCRb[1]], [self.SCRb[1]])
            self.act(var, var, AF.Ln, [self.SCRb[1]], [self.SCRb[1]])
            self.act(var, var, AF.Exp, [self.SCRb[1]], [self.SCRb[1]], scale=-0.5)
        for tb in range(4):
            sl = sls[tb]
            xb4 = self.XTb[tb * 4:tb * 4 + 4]
            mean, var = self.SCR[0][:, sl], self.SCR[1][:, sl]
            for c in range(4):
                d, db = self.tmpf.next()
                self.dve(lambda: nc.vector.tensor_tensor(d, MIX[:, c, sl], mean, ALU.subtract), xb4 + [self.SCRb[0]], [db])
                self.dve(lambda: nc.vector.tensor_tensor(d, d, var, ALU.mult), [db, self.SCRb[1]], [db])
                self.act(MIX[:, c, sl], d, AF.Silu, [db, self.Bc], xb4, scale=self.cln_g[:, c:c + 1], bias=self.cln_b[:, c:c + 1])

    def _conv_branch(self):
        for _ in self._conv_pe():
            pass
        self._conv_ln()

    def _diff_attn(self, side=None):
        nc = self.nc
        MIX = self.XT
        QK, V = self.QK, self.V
        scr = self.psring([0, 1, 2])
        O = [(self.PS[4], self.PSb[4]), (self.PS[6], self.PSb[6])]

        def pull(n):
            if side is not None:
                for _ in range(n):
                    next(side, None)
        Dn = [(self.PS[5], self.PSb[5]), (self.PS[7], self.PSb[7])]
        deferred = []
        for h in range(4):
            for qb in range(4):
                nkt = 4 * (qb + 1)
                q0 = qb * 512
                steps = [(kt, j) for kt in range(nkt) for j in range(2)]

                def stage_a(kt, j):
                    i = kt - 4 * qb
                    c0 = 128 * i if i > 0 else 0
                    sp_, spb = scr.next()
                    pr = slice(64 * j, 64 * j + 64)
                    self.mm(sp_[:, c0:512], QK[pr, 4 + h, kt * 128:(kt + 1) * 128], QK[pr, h, q0 + c0:q0 + 512], True, True,
                            [self.QKb[4 + h][kt // 4], self.QKb[h][qb]], [spb])
                    pt, ptb = self.tmpb.next()
                    self.act(pt[:, c0:512], sp_[:, c0:512], AF.Exp, [spb], [ptb], scale=0.125)
                    if i >= 0:
                        self.pool(lambda: nc.gpsimd.tensor_tensor(pt[:, c0:c0 + 128], pt[:, c0:c0 + 128], self.mle_b, ALU.mult), [ptb, self.Bc], [ptb])
                    return (kt, j, c0, pt, ptb)

                def stage_b(kt, j, c0, pt, ptb):
                    self.mm(O[j][0][:, c0:512], V[:, kt, h * 128:(h + 1) * 128], pt[:, c0:512], kt == 0, kt == nkt - 1,
                            [self.Vb[kt], ptb], [O[j][1]])
                    self.mm(Dn[j][0][:, c0:512], self.ones_b, pt[:, c0:512], kt == 0, kt == nkt - 1, [self.Bc, ptb], [Dn[j][1]])

                pend = []
                for si, (kt, j) in enumerate(steps):
                    pend.append(stage_a(kt, j))
                    if len(pend) > 2:
                        stage_b(*pend.pop(0))
                    pull(1)
                    while deferred and deferred[0][0] <= si:
                        deferred.pop(0)[1]()
                while pend:
                    stage_b(*pend.pop(0))
                while deferred:
                    deferred.pop(0)[1]()
                os_ = []
                for j in range(2):
                    r, rb = self.tmpf.next()
                    self.act(r, Dn[j][0], AF.Ln, [Dn[j][1]], [rb])
                    self.act(r, r, AF.Exp, [rb], [rb], scale=-1.0)
                    o, ob = self.tmpf.next()
                    self.dve(lambda: nc.vector.tensor_tensor(o, O[j][0], r, ALU.mult), [O[j][1], rb], [ob])
                    os_.append((o, ob))
                (o1, o1b), (o2, o2b) = os_
                self.dve(lambda: nc.vector.scalar_tensor_tensor(o1, o2, self.neglam, o1, ALU.mult, ALU.add), [o2b, o1b, self.Bsmall], [o1b])
                st = {}

                def fin_a(o1=o1, o1b=o1b, st=st):
                    st["osq"], st["osqb"] = self.tmpb.next()
                    self.act(st["osq"], o1, AF.Square, [o1b], [st["osqb"]])

                def fin_b(st=st):
                    ss, ssb = scr.next()
                    self.mm(ss, self.ones_b, st["osq"], True, True, [self.Bc, st["osqb"]], [ssb])
                    st["rs"], st["rsb"] = self.tmpf.next()
                    rs = st["rs"]
                    self.dve(lambda: nc.vector.tensor_scalar(rs, ss, 1.0 / 128, RMS_EPS, ALU.mult, ALU.add), [ssb], [st["rsb"]])

                def fin_c(o1=o1, o1b=o1b, st=st, h=h, q0=q0, qb=qb):
                    rs, rsb = st["rs"], st["rsb"]
                    self.act(rs, rs, AF.Ln, [rsb], [rsb])
                    self.act(rs, rs, AF.Exp, [rsb], [rsb], scale=-0.5)
                    self.dve(lambda: nc.vector.scalar_tensor_tensor(MIX[:, 4 + h, q0:q0 + 512], o1, self.gsc, rs, ALU.mult, ALU.mult),
                             [o1b, rsb, self.Bsmall], self.XTb[qb * 4:qb * 4 + 4])

                deferred = [(2, fin_a), (4, fin_b), (6, fin_c)]
                pull(12)
        while deferred:
            deferred.pop(0)[1]()
        if side is not None:
            for _ in side:
                pass

    def _pool_prefix(self):
        nc = self.nc
        for g in range(4):
            U = self.AU[:, g, :]
            bufs = [(self.SCR[0], self.SCRb[0]), (self.SCR[1], self.SCRb[1])]
            cur, curb = bufs[0]
            self.dve(lambda: nc.vector.tensor_tensor(cur, U[:, 16:16 + S], U[:, 15:15 + S], ALU.add), [self.AUb[g]], [curb])
            k = 0
            sh = 2
            while sh < POOL_WINDOWS[g]:
                nxt, nxtb = bufs[(k + 1) % 2]
                self.dve(lambda: nc.vector.tensor_tensor(nxt[:, sh:S], cur[:, sh:S], cur[:, 0:S - sh], ALU.add), [curb], [nxtb])
                self.pool(lambda: nc.gpsimd.tensor_copy(nxt[:, 0:sh], cur[:, 0:sh]), [curb], [nxtb])
                cur, curb = nxt, nxtb
                k += 1
                sh *= 2
            win = POOL_WINDOWS[g]
            t, tb_ = self.tmpf.next()
            self.dve(lambda: nc.vector.tensor_tensor(t[:, 0:16], cur[:, 0:16], self.invc[:, g, :], ALU.mult), [curb, self.Bc], [tb_])
            self.dve(lambda: nc.vector.tensor_tensor(t[:, 0:16], t[:, 0:16], U[:, 16:32], ALU.subtract), [tb_, self.AUb[g]], [tb_])
            for tb in range(4):
                sl = slice(tb * 512, (tb + 1) * 512)
                usl = U[:, 16 + tb * 512:16 + (tb + 1) * 512]
                self.dve(lambda: nc.vector.scalar_tensor_tensor(usl, cur[:, sl], 1.0 / win, usl, ALU.mult, ALU.subtract),
                         [curb, self.AUb[g]], [self.AUb[g]])
            self.dve(lambda: nc.vector.tensor_copy(U[:, 16:32], t[:, 0:16]), [tb_], [self.AUb[g]])

    def _pool_mix(self):
        nc = self.nc
        MIX = self.XT
        pr = self.psring([4, 5, 6, 7])
        for g in range(4):
            for tb in range(4):
                sl = slice(tb * 512, (tb + 1) * 512)
                pm_ps, pm_b = pr.next()
                self.mm(pm_ps, self.pwb[:, g, :], self.AU[:, g, 16 + tb * 512:16 + (tb + 1) * 512], True, True, [self.Bc, self.AUb[g]], [pm_b])
                self.dve(lambda: nc.vector.tensor_scalar(MIX[:, 4 + g, sl], pm_ps, self.pscale[:, g:g + 1], None, ALU.mult),
                         [pm_b, self.Bc], self.XTb[tb * 4:tb * 4 + 4])

    def _sb_attn(self):
        nc = self.nc
        MIX = self.XT
        QK, V = self.QK, self.V
        self.fw.barrier()
        tf, tb_ = self.tf_full, self.tb_full
        fpr = Ring([tf[:, i * 1024:(i + 1) * 1024] for i in range(4)], "tfp")
        spr = Ring([tb_[:, i * 1024:(i + 1) * 1024] for i in range(3)], "tbp")
        apr = Ring([tb_[:, 3072 + i * 1024:3072 + (i + 1) * 1024] for i in range(2)], "tap")
        rpr = Ring([self.SCR[i // 2][:, (i % 2) * 1024:(i % 2 + 1) * 1024] for i in range(4)], "trp")
        zpairs = [(self.PSpair[0], [self.PSb[0], self.PSb[1]]), (self.PSpair[1], [self.PSb[2], self.PSb[3]])]
        zi = [0]
        Rp, Rb = self.PSpair[2], [self.PSb[4], self.PSb[5]]
        Or = self.psring([6, 7])
        zeros = self.zeros_b
        self.pool(lambda: nc.gpsimd.memset(zeros, 0.0), [], [self.Bsmall])
        w = lambda ap, c0: ap.rearrange("p (h q) -> p h q", h=2)[:, :, c0:512]
        hv = lambda ap, hh: ap[:, hh * 512:(hh + 1) * 512]

        steps = []
        for c in range(4):
            for qb in range(4):
                nkt = 4 * (qb + 1)
                for idx, kt in enumerate(range(nkt - 1, -1, -1)):
                    steps.append((c, qb, nkt, idx, kt))

        def a_pe(c, qb, nkt, idx, kt, o_ps, o_b):
            q0 = qb * 512
            i = kt - 4 * qb
            c0 = 128 * i if i > 0 else 0
            zp, zb = zpairs[zi[0] % 2]
            zi[0] += 1
            for hh in range(2):
                pr = slice(64 * hh, 64 * hh + 64)
                self.mm(hv(zp, hh)[:, c0:512], QK[pr, 4 + c, kt * 128:(kt + 1) * 128], QK[pr, c, q0 + c0:q0 + 512], True, True,
                        [self.QKb[4 + c][kt // 4], self.QKb[c][qb]], [zb[hh]])
            return dict(c=c, qb=qb, q0=q0, nkt=nkt, idx=idx, kt=kt, i=i, c0=c0, zp=zp, zb=zb, o_ps=o_ps, o_b=o_b)

        def a_act(st):
            c0 = st["c0"]
            e, eb = fpr.next()
            self.act(w(e, c0), w(st["zp"], c0), AF.Exp, st["zb"], [eb], scale=0.125)
            sp_, spb = spr.next()
            self.act(w(sp_, c0), w(e, c0), AF.Ln, [eb], [spb], bias=1.0)
            if st["i"] >= 0:
                for hh in range(2):
                    blk = hv(sp_, hh)[:, c0:c0 + 128]
                    self.pool(lambda: nc.gpsimd.tensor_tensor(blk, blk, self.mlt_b, ALU.mult), [spb, self.Bc], [spb])
            st["sp"], st["spb"] = sp_, spb

        def r_init(st):
            for hh in range(2):
                self.mm(hv(Rp, hh), zeros, QK[:, st["c"], st["q0"]:st["q0"] + 512], True, True,
                        [self.Bsmall, self.QKb[st["c"]][st["qb"]]], [Rb[hh]], inc=True)

        def b_copy(st):
            c0 = st["c0"]
            rsb_, rsbb = rpr.next()
            self.dve(lambda: nc.vector.tensor_copy(w(rsb_, c0), w(Rp, c0)), Rb, [rsbb])
            st["rsb"], st["rsbb"] = rsb_, rsbb

        def b_pe(st):
            c0 = st["c0"]
            for hh in range(2):
                sph = hv(st["sp"], hh)[:, c0:512]
                self.mm(hv(st["zp"], hh)[:, c0:512], self.ntri_b, sph, False, True, [self.Bc, st["spb"]], [st["zb"][hh]], inc=True)
            if st["idx"] < st["nkt"] - 1:
                for hh in range(2):
                    sph = hv(st["sp"], hh)[:, c0:512]
                    self.mm(hv(Rp, hh)[:, c0:512], self.ones_b, sph, False, True, [self.Bc, st["spb"]], [Rb[hh]], inc=True)

        def b_dve(st):
            c0 = st["c0"]
            t, tbf = fpr.next()
            self.dve(lambda: nc.vector.scalar_tensor_tensor(w(t, c0), w(st["zp"], c0), 0.125, w(st["rsb"], c0), ALU.mult, ALU.subtract),
                     st["zb"] + [st["rsbb"]], [tbf])
            st["t"], st["tbf"] = t, tbf

        def b_act(st):
            c0 = st["c0"]
            a, ab = apr.next()
            self.act(w(a, c0), w(st["t"], c0), AF.Exp, [st["tbf"]], [ab])
            if st["i"] >= 0:
                for hh in range(2):
                    blk = hv(a, hh)[:, c0:c0 + 128]
                    self.pool(lambda: nc.gpsimd.tensor_tensor(blk, blk, self.mlt_b, ALU.mult), [ab, self.Bc], [ab])
            st["a"], st["ab"] = a, ab
            return st

        def stage_c(st):
            c0, kt, idx, nkt = st["c0"], st["kt"], st["idx"], st["nkt"]
            o_ps, o_b = st["o_ps"], st["o_b"]
            for hh in range(2):
                pr = slice(64 * hh, 64 * hh + 64)
                h = 2 * st["c"] + hh
                self.mm(o_ps[pr, c0:512], V[:, kt, h * 64:(h + 1) * 64], hv(st["a"], hh)[:, c0:512], idx == 0, idx == nkt - 1,
                        [self.Vb[kt], st["ab"]], [o_b], inc=True)
            if idx == nkt - 1:
                qb = st["qb"]
                self.act(MIX[:, st["c"], st["q0"]:st["q0"] + 512], o_ps, AF.Copy, [o_b], self.XTb[qb * 4:qb * 4 + 4])

        pa = None
        pb = None
        pc = None
        obank = None
        for n in range(len(steps) + 3):
            cur = None
            if n < len(steps):
                c, qb, nkt, idx, kt = steps[n]
                if idx == 0:
                    obank = Or.next()
                cur = a_pe(c, qb, nkt, idx, kt, obank[0], obank[1])
            if pa is not None:
                b_pe(pa)
                b_dve(pa)
            if cur is not None:
                if cur["idx"] == 0:
                    r_init(cur)
                b_copy(cur)
            if pc is not None:
                stage_c(pc)
            if cur is not None:
                a_act(cur)
            nc_ = b_act(pb) if pb is not None else None
            pa, pb, pc = cur, pa, nc_

    def _load_ln(self, idx, scale=None):
        nc, fw = self.nc, self.fw
        G = self.SCR[0][:, 0:1024]
        B_ = self.SCR[1][:, 0:1024]
        fw.dma(fw.sp, G, self.lnp_d[2 * idx].partition_broadcast(128), writes=[self.SCRb[0]])
        fw.dma(fw.sp, B_, self.lnp_d[2 * idx + 1].partition_broadcast(128), writes=[self.SCRb[1]])
        if scale is not None:
            self.act(G, G, AF.Copy, [self.SCRb[0]], [self.SCRb[0]], scale=scale)
            self.act(B_, B_, AF.Copy, [self.SCRb[1]], [self.SCRb[1]], scale=scale)
        return G, B_

    def _ln_stats(self, tt):
        nc = self.nc
        xs = self.X[:, tt, :]
        st_, stb = self.statr.next()
        xb = [self.Xb[tt]]
        self.dve(lambda: nc.vector.bn_stats(st_[:, 0:6], xs[:, 0:512]), xb, [stb])
        self.dve(lambda: nc.vector.bn_stats(st_[:, 6:12], xs[:, 512:1024]), xb, [stb])
        self.dve(lambda: nc.vector.bn_aggr(st_[:, 12:14], st_[:, 0:12]), [stb], [stb])
        self.dve(lambda: nc.vector.tensor_scalar(st_[:, 14:15], st_[:, 13:14], LN_EPS, None, ALU.add), [stb], [stb])
        self.act(st_[:, 14:15], st_[:, 14:15], AF.Ln, [stb], [stb])
        self.act(st_[:, 14:15], st_[:, 14:15], AF.Exp, [stb], [stb], scale=-0.5)
        return (tt, st_, stb)

    def _ln_apply(self, tt, st_, stb, G, B_):
        nc = self.nc
        xs = self.X[:, tt, :]
        xb = [self.Xb[tt]]
        self.dve(lambda: nc.vector.scalar_tensor_tensor(xs, xs, st_[:, 12:13], G, ALU.subtract, ALU.mult), xb + [stb, self.SCRb[0]], xb)
        self.dve(lambda: nc.vector.scalar_tensor_tensor(xs, xs, st_[:, 14:15], B_, ALU.mult, ALU.add), xb + [stb, self.SCRb[1]], xb)

    def _ln_tile(self, tt, G, B_):
        self._ln_apply(*self._ln_stats(tt), G, B_)

    def _load_wout(self, L):
        fw = self.fw
        for i in range(2):
            src = self.wout[L].rearrange("(c p) n -> p c n", p=128)[:, i * 4:(i + 1) * 4, :]
            dst = self.Wblk[3 + i].rearrange("p (c n) -> p c n", c=4)
            fw.dma(fw.pool, dst, src, writes=[self.Wb[3 + i]])

    def _mid(self, L):
        nc, fw = self.nc, self.fw
        MIX = self.XT
        inv = 1.0 / ALPHA
        Wo = [self.Wblk[3].rearrange("p (c n) -> p c n", c=4), self.Wblk[4].rearrange("p (c n) -> p c n", c=4)]
        W0 = self._expert_load(L, 0)
        G, B_ = self._load_ln(2 * L, scale=ALPHA)
        xsrc = self.x_d if L == 0 else self.spill_d
        for tt in range(NT):
            fw.dma(fw.sp, self.X[:, tt, :], xsrc[tt * 128:(tt + 1) * 128, :], writes=[self.Xb[tt]])
        psr = self.psring([5, 6])
        tpr = self.psring([0, 1])
        RB = [(self.PS[i], self.PSb[i]) for i in (2, 3, 4, 7)]

        def op_tile(tt):
            for half in range(2):
                ps, psb = psr.next()
                for fc in range(8):
                    self.mm(ps, MIX[:, fc, tt * 128:(tt + 1) * 128], Wo[fc // 4][:, fc % 4, half * 512:(half + 1) * 512], fc == 0, fc == 7,
                            [self.XTb[tt], self.Wb[3 + fc // 4]], [psb])
                xs = self.X[:, tt, half * 512:(half + 1) * 512]
                self.dve(lambda: nc.vector.scalar_tensor_tensor(xs, xs, ALPHA, ps, ALU.mult, ALU.add), [self.Xb[tt], psb], [self.Xb[tt]])
            st = self._ln_stats(tt)
            ln_flush()
            ln_pend.append(st)

        ln_pend = []

        def ln_flush():
            while ln_pend:
                self._ln_apply(*ln_pend.pop(0), G, B_)

        def xt_T(tb, dc):
            ps, psb = tpr.next()
            for j in range(4):
                tt = tb * 4 + j
                self.tr(ps[:, j * 128:(j + 1) * 128], self.X[:, tt, dc * 128:(dc + 1) * 128], self.ident_f,
                        [self.Xb[tt], self.Bc], [psb], inc=(j == 3))
            xf, xfb = self.tmpf.next()
            self.act(xf, ps, AF.Copy, [psb], [xfb], scale=inv)
            self.act(self.XT[:, dc, tb * 512:(tb + 1) * 512], ps, AF.Copy, [psb], self.XTb[tb * 4:tb * 4 + 4], scale=inv)
            return (tb, dc, xf, xfb)

        def xt_R(tb, dc, xf, xfb):
            for j in range(4):
                self.mm(RB[j][0][:, 0:NE], xf[:, j * 128:(j + 1) * 128], self.wr[:, dc, :], dc == 0, dc == 7, [self.Bc, xfb], [RB[j][1]])
            if dc == 7:
                for j in range(4):
                    tt = tb * 4 + j
                    self.act(self.lgt[:, tt * NE:(tt + 1) * NE], RB[j][0][:, 0:NE], AF.Copy, [RB[j][1]], [self.Brt])

        pend = []

        def xt_step(tb, dc):
            pend.append(xt_T(tb, dc))
            if len(pend) > 1:
                xt_R(*pend.pop(0))

        for tt in range(4):
            op_tile(tt)
        for tb in range(4):
            for j in range(4):
                if tb + 1 < 4:
                    op_tile((tb + 1) * 4 + j)
                else:
                    ln_flush()
                xt_step(tb, 2 * j)
                xt_step(tb, 2 * j + 1)
        while pend:
            xt_R(*pend.pop(0))
        return W0

    def _routing(self):
        nc = self.nc
        def wt():
            t, tb_ = self.tmpf.next()
            return t[:, 0:256], tb_
        aff, affb = wt()
        self.act(aff, self.lgt, AF.Sigmoid, [self.Brt], [affb])
        sel, selb = wt()
        v3 = lambda a: a.rearrange("p (t e) -> p t e", t=NT)
        v4 = lambda a: a.rearrange("p (t g j) -> p t g j", t=NT, g=4)
        g3 = lambda a: a.rearrange("p (t g) -> p t g", t=NT)
        self.dve(lambda: nc.vector.tensor_tensor(v3(sel), v3(aff), self.rbias.unsqueeze(1).broadcast_to([128, NT, NE]), ALU.add), [affb, self.Bc], [selb])
        sm = self.small
        m1 = sm[:, 8:8 + 64] if False else None
        m1t, m1b = self.tmpf.next()
        m1 = m1t[:, 0:64]
        m2 = m1t[:, 64:128]
        gs = m1t[:, 128:192]
        gmax = m1t[:, 192:208]
        gmask = m1t[:, 208:272]
        gsum = m1t[:, 272:288]
        self.dve(lambda: nc.vector.tensor_reduce(m1, sel.rearrange("p (tg j) -> p tg j", j=4), AX.X, ALU.max), [selb], [m1b])
        eq, eqb = wt()
        m1bc = m1.unsqueeze(2).broadcast_to([128, 64, 4])
        self.dve(lambda: nc.vector.tensor_tensor(eq.rearrange("p (tg j) -> p tg j", j=4), sel.rearrange("p (tg j) -> p tg j", j=4), m1bc, ALU.is_equal), [selb, m1b], [eqb])
        self.dve(lambda: nc.vector.scalar_tensor_tensor(eq, eq, -1.0e9, sel, ALU.mult, ALU.add), [eqb, selb], [eqb])
        self.dve(lambda: nc.vector.tensor_reduce(m2, eq.rearrange("p (tg j) -> p tg j", j=4), AX.X, ALU.max), [eqb], [m1b])
        self.dve(lambda: nc.vector.tensor_tensor(gs, m1, m2, ALU.add), [m1b], [m1b])
        self.dve(lambda: nc.vector.tensor_reduce(gmax, g3(gs), AX.X, ALU.max), [m1b], [m1b])
        self.dve(lambda: nc.vector.tensor_tensor(g3(gmask), g3(gs), gmax.unsqueeze(2).broadcast_to([128, NT, 4]), ALU.is_equal), [m1b], [m1b])
        ge, geb = wt()
        m2bc = m2.unsqueeze(2).broadcast_to([128, 64, 4])
        gmbc = gmask.unsqueeze(2).broadcast_to([128, 64, 4])
        j4 = lambda a: a.rearrange("p (tg j) -> p tg j", j=4)
        self.dve(lambda: nc.vector.tensor_tensor(j4(ge), j4(sel), m2bc, ALU.is_ge), [selb, m1b], [geb])
        self.dve(lambda: nc.vector.tensor_tensor(j4(ge), j4(ge), gmbc, ALU.mult), [geb, m1b], [geb])
        self.dve(lambda: nc.vector.tensor_tensor(ge, ge, aff, ALU.mult), [geb, affb], [geb])
        self.dve(lambda: nc.vector.tensor_reduce(gsum, v3(ge), AX.X, ALU.add), [geb], [m1b])
        self.dve(lambda: nc.vector.reciprocal(gsum, gsum), [m1b], [m1b])
        self.dve(lambda: nc.vector.tensor_tensor(v3(self.comb), v3(ge), gsum.unsqueeze(2).broadcast_to([128, NT, NE]), ALU.mult), [geb, m1b], [self.Bcomb])

    def _expert_load(self, L, e):
        fw = self.fw
        s = e % 2
        Wg = self.Wblk[3 * s].rearrange("p (c n) -> p c n", c=8)
        Wu = self.Wblk[3 * s + 1].rearrange("p (c n) -> p c n", c=8)
        Wd = self.Wblk[3 * s + 2].rearrange("p (c n) -> p c n", c=4)
        fw.dma(fw.pool, Wg, self.wg[L][e].rearrange("(c p) n -> p c n", p=128), writes=[self.Wb[3 * s]])
        fw.dma(fw.pool, Wu, self.wu[L][e].rearrange("(c p) n -> p c n", p=128), writes=[self.Wb[3 * s + 1]])
        fw.dma(fw.pool, Wd, self.wd[L][e].rearrange("(c p) n -> p c n", p=128), writes=[self.Wb[3 * s + 2]])
        return (Wg, Wu, Wd, s)

    def _experts(self, L, W0):
        nc, fw = self.nc, self.fw
        gr = self.psring([0, 1])
        ur = self.psring([2, 3])
        yr = self.psring([4, 5, 6, 7])
        comb3 = self.comb.rearrange("p (t e) -> p t e", t=NT)

        def load(e):
            s = e % 2
            Wg = self.Wblk[3 * s].rearrange("p (c n) -> p c n", c=8)
            Wu = self.Wblk[3 * s + 1].rearrange("p (c n) -> p c n", c=8)
            Wd = self.Wblk[3 * s + 2].rearrange("p (c n) -> p c n", c=4)
            fw.dma(fw.pool, Wg, self.wg[L][e].rearrange("(c p) n -> p c n", p=128), writes=[self.Wb[3 * s]])
            fw.dma(fw.pool, Wu, self.wu[L][e].rearrange("(c p) n -> p c n", p=128), writes=[self.Wb[3 * s + 1]])
            fw.dma(fw.pool, Wd, self.wd[L][e].rearrange("(c p) n -> p c n", p=128), writes=[self.Wb[3 * s + 2]])
            return (Wg, Wu, Wd, s)

        G_, B_ = self._load_ln(2 * L + 1)

        lnp = []

        def ln_fin():
            while lnp:
                st = lnp.pop(0)
                self._ln_apply(*st, G_, B_)
                if L == 1:
                    tt = st[0]
                    fw.dma(fw.sp, self.out_d[tt * 128:(tt + 1) * 128, :], self.X[:, tt, :], reads=[self.Xb[tt]])

        def ln_out(tt):
            st = self._ln_stats(tt)
            ln_fin()
            lnp.append(st)

        def stage_gu(e, tb, W, ln_tiles=()):
            Wg, Wu, Wd, s = W
            H, Hb = self.htr.next()
            xb4 = self.XTb[tb * 4:tb * 4 + 4]
            for fc in range(4):
                g, gb = gr.next()
                u, ub = ur.next()
                for dc in range(8):
                    self.mm(g, Wg[:, dc, fc * 128:(fc + 1) * 128], self.XT[:, dc, tb * 512:(tb + 1) * 512], dc == 0, dc == 7, [self.Wb[3 * s]] + xb4, [gb])
                for dc in range(8):
                    self.mm(u, Wu[:, dc, fc * 128:(fc + 1) * 128], self.XT[:, dc, tb * 512:(tb + 1) * 512], dc == 0, dc == 7, [self.Wb[3 * s + 1]] + xb4, [ub])
                sg, sgb = self.tmpf.next()
                self.act(sg, g, AF.Silu, [gb], [sgb])
                self.dve(lambda: nc.vector.tensor_tensor(H[:, fc, :], u, sg, ALU.mult), [ub, sgb], [Hb])
                if fc < len(ln_tiles):
                    ln_out(ln_tiles[fc])
            return (e, tb, W, H, Hb)

        def stage_dn(e, tb, W, H, Hb):
            Wg, Wu, Wd, s = W
            for t4 in range(4):
                tt = tb * 4 + t4
                for half in range(2):
                    y, yb = yr.next()
                    for fc in range(4):
                        self.mm(y, H[:, fc, t4 * 128:(t4 + 1) * 128], Wd[:, fc, half * 512:(half + 1) * 512], fc == 0, fc == 3, [Hb, self.Wb[3 * s + 2]], [yb])
                    xs = self.X[:, tt, half * 512:(half + 1) * 512]
                    self.dve(lambda: nc.vector.scalar_tensor_tensor(xs, y, comb3[:, tt, e:e + 1], xs, ALU.mult, ALU.add), [yb, self.Bcomb, self.Xb[tt]], [self.Xb[tt]])

        Wn = W0
        prev = None
        for e in range(NE):
            W = Wn
            for tb in range(4):
                if e == NE - 1:
                    stage_dn(*prev)
                    done = list(range((tb - 1) * 4, tb * 4)) if tb > 0 else []
                    cur = stage_gu(e, tb, W, done)
                else:
                    cur = stage_gu(e, tb, W)
                    if prev is not None:
                        stage_dn(*prev)
                prev = cur
                if tb == 0 and e + 1 < NE:
                    Wn = self._expert_load(L, e + 1)
                if tb == 0 and e + 1 == NE and L == 0:
                    self.pref_win = {g: self._load_w_in(1, g, self._win_slot(1)[g]) for g in (3, 0, 1)}
        stage_dn(*prev)
        for tt in range(12, 16):
            ln_out(tt)
        ln_fin()

    def _final_ln(self, L):
        pass


def _constants():
    j = np.arange(128)[:, None]
    k = np.arange(128)[None, :]
    ident = (j == k).astype(np.float32)
    tri = (j >= k).astype(np.float32)
    mle = (j <= k).astype(np.float32)
    mlt = (j < k).astype(np.float32)
    m = np.arange(128)
    perm = np.where((m % 64) < 32, m + 32, m - 32)
    pm = np.zeros((128, 128), np.float32)
    pm[perm, m] = 1.0
    invc = np.zeros((128, 4, 16), np.float32)
    for g, w in enumerate(POOL_WINDOWS):
        invc[:, g, :] = 1.0 / np.minimum(np.arange(16) + 1, w)
    cst = np.concatenate([ident, tri, mle, mlt, pm, invc.reshape(128, 64)], axis=1).astype(np.float32)
    inv = (10000.0 ** (-np.arange(0, 64, 2, dtype=np.float32) / 64)).astype(np.float32)
    ang = np.arange(S, dtype=np.float32)[:, None] * inv[None, :]
    ang = np.concatenate([ang, ang], axis=-1)
    cos = np.cos(ang).astype(np.float32).T
    sin = np.sin(ang).astype(np.float32).T
    sign = np.where(np.arange(64) < 32, -1.0, 1.0).astype(np.float32)[:, None]
    sins = sin * sign
    rope = np.stack([np.concatenate([cos, cos], 0), np.concatenate([sins, sins], 0)], 0).astype(np.float32)
    return cst, np.ascontiguousarray(rope)


def _pack_inputs(inp):
    f = lambda a: np.ascontiguousarray(np.asarray(a, dtype=np.float32))
    cst, rope = _constants()
    pp = np.zeros((128, 144), np.float32)
    pp[:, 0:124] = f(inp["l0_conv_w"]).T.reshape(4, 128, 31).transpose(1, 0, 2).reshape(128, 124)
    v4 = lambda a: f(a).reshape(4, 128).T
    pp[:, 124:128] = v4(inp["l0_conv_b"])
    pp[:, 128:132] = v4(inp["l0_conv_ln_g"])
    pp[:, 132:136] = v4(inp["l0_conv_ln_b"])
    pp[:, 136] = f(inp["l0_subln_g"])
    pp[:, 137:141] = v4(inp["l1_pool_scale"])
    rowp = np.concatenate([f(inp["l0_lambda_q1"]), f(inp["l0_lambda_q2"]), f(inp["l0_lambda_k1"]), f(inp["l0_lambda_k2"]),
                           f(inp["router_bias"])]).astype(np.float32)
    lnp = np.stack([f(inp["l0_ln_mix_g"]), f(inp["l0_ln_mix_b"]), f(inp["l0_ln_ffn_g"]), f(inp["l0_ln_ffn_b"]),
                    f(inp["l1_ln_mix_g"]), f(inp["l1_ln_mix_b"]), f(inp["l1_ln_ffn_g"]), f(inp["l1_ln_ffn_b"])], 0)
    shared = {
        "router_w": f(inp["router_w"]),
        "w_in0": f(inp["l0_in_proj"]), "w_in1": f(inp["l1_in_proj"]),
        "w_out0": f(inp["l0_out_proj"]), "w_out1": f(inp["l1_out_proj"]),
        "w_gate0": f(inp["l0_w_gate"]), "w_gate1": f(inp["l1_w_gate"]),
        "w_up0": f(inp["l0_w_up"]), "w_up1": f(inp["l1_w_up"]),
        "w_down0": f(inp["l0_w_down"]), "w_down1": f(inp["l1_w_down"]),
        "pool_w": f(inp["l1_pool_w"]),
        "pp": pp, "rowp": rowp, "lnp": np.ascontiguousarray(lnp), "cst": cst, "rope": rope,
    }
    x = f(inp["x"])
    return [dict(shared, x=np.ascontiguousarray(x[c])) for c in range(x.shape[0])]


def kernel(**inputs):
    in_maps = _pack_inputs(inputs)
    nc = Prog().build()
    res = run_bass_kernel_spmd(nc, in_maps, core_ids=list(range(8)))
    return np.stack([np.asarray(r["out"], dtype=np.float32) for r in res.results], axis=0)
```

```python
import contextlib
import math
import numpy as np
import concourse.bass as bass
import concourse.mybir as mybir
from concourse.alu_op_type import AluOpType as ALU
from concourse.bass_utils import run_bass_kernel_spmd

F32 = mybir.dt.float32
BF16 = mybir.dt.bfloat16
AF = mybir.ActivationFunctionType
AX = mybir.AxisListType

S = 2048
D = 1024
NT = 16
NE = 16
ALPHA = 4.0 ** 0.25
LN_EPS = 1e-5
RMS_EPS = 1e-5
LAM_INIT0 = 0.8 - 0.6 * math.exp(0.0)
POOL_WINDOWS = (2, 4, 8, 16)
NFILL = 3


import os as _os
TRACE = bool(_os.environ.get("KTRACE"))


class Sem:
    def __init__(self, h):
        self.h = h
        self.cnt = 0


class Buf:
    __slots__ = ("last_w", "readers", "name", "excl")

    def __init__(self, name="", excl=False):
        self.last_w = None
        self.readers = []
        self.name = name
        self.excl = excl


class Eng:
    def __init__(self, name, obj, sem, same_wait=True):
        self.name = name
        self.obj = obj
        self.sem = sem
        self.waited = {}
        self.same_wait = same_wait


class FW:
    def __init__(self, nc, stack, n_dma_sems=32):
        self.nc = nc
        def mk(n):
            s_ = Sem(stack.enter_context(nc.semaphore(n)))
            s_.name = n
            return s_
        self.pe = Eng("pe", nc.tensor, mk("s_pe"), same_wait=False)
        self.act = Eng("act", nc.scalar, mk("s_act"))
        self.dve = Eng("dve", nc.vector, mk("s_dve"))
        self.pool = Eng("pool", nc.gpsimd, mk("s_pool"))
        self.sp = Eng("sp", nc.sync, mk("s_sp"))
        self.engs = [self.pe, self.act, self.dve, self.pool, self.sp]
        self.dma_sems = [mk(f"s_dma{i}") for i in range(n_dma_sems)]
        self.dma_i = 0
        self.dma_qi = [0, 0]
        self.n_instr = 0
        self.pe_pending = False

    def _wait(self, eng, tok):
        sem, val = tok
        if sem is eng.sem and not eng.same_wait:
            return
        if eng.waited.get(id(sem), 0) >= val:
            return
        eng.obj.wait_ge(sem.h, val)
        eng.waited[id(sem)] = val
        if TRACE:
            print("   wait", eng.name, "on", getattr(sem, "name", "?"), val)

    def _deps(self, eng, reads, writes):
        for b in reads:
            if b.last_w is not None:
                self._wait(eng, b.last_w)
            if b.excl:
                for t in b.readers:
                    if t[0] is not eng.sem:
                        self._wait(eng, t)
        for b in writes:
            if b.last_w is not None:
                self._wait(eng, b.last_w)
            for t in b.readers:
                self._wait(eng, t)

    @staticmethod
    def _commit(tok, reads, writes):
        for b in reads:
            if len(b.readers) > 24:
                best = {}
                for s_, v_ in b.readers:
                    if best.get(id(s_), (None, -1))[1] < v_:
                        best[id(s_)] = (s_, v_)
                b.readers = list(best.values())
            b.readers.append(tok)
        for b in writes:
            b.last_w = tok
            b.readers = []

    def op(self, eng, fn, reads=(), writes=(), inc=True):
        if TRACE:
            print("op", eng.name, "cnt", eng.sem.cnt, "reads", [b.name for b in reads], "writes", [b.name for b in writes], "inc", inc)
        self._deps(eng, reads, writes)
        ins = fn()
        self.n_instr += 1
        if inc:
            eng.sem.cnt += 1
            ins.then_inc(eng.sem.h, 1)
            tok = (eng.sem, eng.sem.cnt)
            if eng is self.pe:
                self.pe_pending = False
        else:
            assert eng is self.pe
            tok = (eng.sem, eng.sem.cnt + 1)
            self.pe_pending = True
        self._commit(tok, reads, writes)
        return ins

    def dma(self, q, out, in_, reads=(), writes=(), **kw):
        half = len(self.dma_sems) // 2
        k = 0 if q is self.sp else 1
        sem = self.dma_sems[k * half + self.dma_qi[k] % half]
        self.dma_qi[k] += 1
        self.dma_i += 1
        if sem.cnt:
            self._wait(q, (sem, sem.cnt))
        self._deps(q, reads, writes)
        ins = q.obj.dma_start(out=out, in_=in_, **kw)
        sem.cnt += 16
        ins.then_inc(sem.h, 16)
        self.n_instr += 1
        tok = (sem, sem.cnt)
        self._commit(tok, reads, writes)
        return tok

    def barrier(self):
        assert not self.pe_pending
        for e in self.engs:
            for o in self.engs:
                if o is not e and o.sem.cnt:
                    self._wait(e, (o.sem, o.sem.cnt))
            for s in self.dma_sems:
                if s.cnt:
                    self._wait(e, (s, s.cnt))


class Ring:
    def __init__(self, aps, name="r"):
        self.items = [(ap, Buf(f"{name}{i}")) for i, ap in enumerate(aps)]
        self.i = 0

    def next(self):
        it = self.items[self.i % len(self.items)]
        self.i += 1
        return it


class Prog:
    def __init__(self, stop=None, taps=()):
        self.stop = stop
        self.taps = set(taps)
        self.dbg_specs = {}

    def build(self):
        nc = bass.Bass("TRN2", target_bir_lowering=False)
        self.nc = nc
        di = lambda n, s: nc.dram_tensor(n, list(s), F32, kind="ExternalInput").ap()
        self.x_d = di("x", [S, D])
        self.router_w = di("router_w", [D, NE])
        self.win = [di("w_in0", [D, 2560]), di("w_in1", [D, 2048])]
        self.wout = [di("w_out0", [D, D]), di("w_out1", [D, D])]
        self.wg = [di("w_gate0", [NE, D, 512]), di("w_gate1", [NE, D, 512])]
        self.wu = [di("w_up0", [NE, D, 512]), di("w_up1", [NE, D, 512])]
        self.wd = [di("w_down0", [NE, 512, D]), di("w_down1", [NE, 512, D])]
        self.pool_w = di("pool_w", [4, 128, 128])
        self.pp_d = di("pp", [128, 144])
        self.rowp_d = di("rowp", [272])
        self.lnp_d = di("lnp", [8, D])
        self.cst_d = di("cst", [128, 128 * 5 + 64])
        self.rope_d = di("rope", [2, 128, S])
        self.out_d = nc.dram_tensor("out", [S, D], F32, kind="ExternalOutput").ap()
        self.spill_d = nc.dram_tensor("xspill", [S, D], F32, kind="Internal").ap()
        with contextlib.ExitStack() as st:
            self.st = st
            self.fw = FW(nc, st)
            self._alloc()
            self._run()
        return nc

    def _alloc(self):
        nc, st = self.nc, self.st
        sb = lambda n, s, d: st.enter_context(nc.sbuf_tensor("sb_" + n, s, d))
        self.XT = sb("xt", [128, 8 * S], BF16)[:].rearrange("p (c t) -> p c t", c=8)
        self.XTb = [Buf(f"xt{t}") for t in range(NT)]
        bigf = sb("big", [128, 16640], F32)[:]
        self.X = bigf[:, 0:16384].rearrange("p (t d) -> p t d", t=NT)
        self.Xb = [Buf(f"x{t}") for t in range(NT)]
        bigb = bigf.bitcast(BF16)
        self.QK = bigb[:, 0:16384].rearrange("p (c t) -> p c t", c=8)
        self.QKb = [[Buf(f"qk{c}_{tb}") for tb in range(4)] for c in range(8)]
        self.V = bigb[:, 16384:24576].rearrange("p (t f) -> p t f", t=NT)
        self.Vb = [Buf(f"v{t}") for t in range(NT)]
        self.AU = bigb[:, 24576:32896].rearrange("p (c t) -> p c t", c=4)
        self.AUb = [Buf(f"au{c}") for c in range(4)]
        wb = sb("w", [128, 24576], BF16)[:]
        self.Wblk = [wb[:, i * 4096:(i + 1) * 4096] for i in range(6)]
        self.Wb = [Buf(f"w{i}") for i in range(6)]
        scr = sb("scr", [128, 4096], F32)[:]
        self.SCR = [scr[:, 0:2048], scr[:, 2048:4096]]
        self.SCRb = [Buf("scrA"), Buf("scrB")]
        tf = sb("tmpf", [128, 8 * 512], F32)[:]
        self.tmpf = Ring([tf[:, i * 512:(i + 1) * 512] for i in range(8)], "tf")
        tb_ = sb("tmpb", [128, 10 * 512], BF16)[:]
        self.tmpb = Ring([tb_[:, i * 512:(i + 1) * 512] for i in range(6)], "tb")
        self.tmpb2 = Ring([tb_[:, i * 512:(i + 1) * 512] for i in range(6, 10)], "tb2")
        self.tf_full, self.tb_full = tf, tb_
        self.ht = sb("ht", [128, 2 * 2048], BF16)[:]
        self.htr = Ring([self.ht[:, i * 2048:(i + 1) * 2048].rearrange("p (c t) -> p c t", c=4) for i in range(2)], "ht")
        self.cstf = sb("cstf", [128, 128 * 5 + 64], F32)[:]
        self.cstb = sb("cstb", [128, 128 * 7], BF16)[:]
        self.Bc = Buf("const")
        self.pp = sb("pp", [128, 144], F32)[:]
        self.rowp = sb("rowp", [128, 272], F32)[:]
        self.small = sb("small", [128, 64], F32)[:]
        self.Bsmall = Buf("small")
        self.comb = sb("comb", [128, NT * NE], F32)[:]
        self.Bcomb = Buf("comb")
        self.zeros_b = sb("zeros", [128, 128], BF16)[:]
        self.rt_halves = [scr[0:16, 1024:2048], scr[0:16, 3072:4096]]
        self.lgt = scr[:, 1024:1024 + NT * NE]
        self.Brt = Buf("rt")
        stt = sb("stats", [128, 4 * 16], F32)[:]
        self.statr = Ring([stt[:, i * 16:(i + 1) * 16] for i in range(4)], "stat")
        self.wr = sb("wr", [128, 8 * NE], F32)[:].rearrange("p (c e) -> p c e", c=8)
        self.pwb = sb("pwb", [128, 4 * 128], BF16)[:].rearrange("p (g d) -> p g d", g=4)
        self.PS = []
        self.PSb = []
        self.PSpair = []
        for k in range(4):
            pair = st.enter_context(nc.psum_tensor(f"pp{k}", [128, 1024], F32))[:]
            self.PSpair.append(pair)
            for h in range(2):
                self.PS.append(pair[:, h * 512:(h + 1) * 512])
                self.PSb.append(Buf(f"ps{2 * k + h}", excl=True))

    def psring(self, idxs):
        r = Ring([self.PS[i] for i in idxs])
        r.items = [(self.PS[i], self.PSb[i]) for i in idxs]
        return r

    def mm(self, out, lhsT, rhs, start, stop, reads, writes, inc=None):
        nc = self.nc
        inc = stop if inc is None else inc
        self.fw.op(self.fw.pe, lambda: nc.tensor.matmul(out, lhsT, rhs, start=start, stop=stop), reads, writes, inc=inc)

    def tr(self, out, in_, ident, reads, writes, inc=True):
        nc = self.nc
        self.fw.op(self.fw.pe, lambda: nc.tensor.transpose(out, in_, ident), reads, writes, inc=inc)

    def act(self, out, in_, func, reads, writes, **kw):
        nc = self.nc
        self.fw.op(self.fw.act, lambda: nc.scalar.activation(out, in_, func, **kw), reads, writes)

    def dve(self, fn, reads, writes):
        self.fw.op(self.fw.dve, fn, reads, writes)

    def pool(self, fn, reads, writes):
        self.fw.op(self.fw.pool, fn, reads, writes)

    def tap(self, name, ap, bufs, dtype=F32):
        if name not in self.taps:
            return
        nc = self.nc
        shape = list(ap.shape)
        d = nc.dram_tensor("dbg_" + name, shape, dtype, kind="ExternalOutput").ap()
        self.dbg_specs[name] = (shape, dtype)
        self.fw.barrier()
        self.fw.dma(self.fw.sp, d, ap, reads=bufs)
        self.fw.barrier()

    def _run(self):
        self._consts()
        for L in range(2):
            if L == 0:
                self._xt_from_dram()
            else:
                self._xt_from_x(router=False, spill=True)
                self.fw.barrier()
            if self.stop == f"l{L}_xt":
                return self._finish()
            self._in_proj(L)
            if self.stop in (f"l{L}_inproj", "l0_glu", "l0_qk"):
                return self._finish()
            self._load_wout(L)
            if L == 0:
                if self.stop == "l0_conv":
                    self._conv_branch()
                    return self._finish()
                self._diff_attn(self._conv_pe())
                self._conv_ln()
            else:
                self._pool_mix()
                self._sb_attn()
            if self.stop == f"l{L}_mix":
                return self._finish()
            self.fw.barrier()
            W0 = self._mid(L)
            self._routing()
            if self.stop == f"l{L}_route":
                return self._finish()
            self._experts(L, W0)
            self._final_ln(L)
            if self.stop == f"l{L}_moe":
                return self._finish()
        self._finish()

    def _finish(self):
        self.fw.barrier()
        allb = self.XTb + self.Xb + self.Vb + self.AUb + [b for r in self.QKb for b in r] + [self.Bcomb, self.Brt, self.Bsmall]
        self.tap("XT", self.XT, allb, BF16)
        self.tap("QK", self.QK, allb, BF16)
        self.tap("V", self.V, allb, BF16)
        self.tap("AU", self.AU, allb, BF16)
        self.tap("X", self.X, allb, F32)
        self.tap("comb", self.comb, allb, F32)
        self.tap("small", self.small, allb, F32)
        self.fw.barrier()

    def _consts(self):
        nc, fw = self.nc, self.fw
        fw.dma(fw.sp, self.cstf, self.cst_d[:, :], writes=[self.Bc])
        fw.dma(fw.sp, self.pp, self.pp_d[:, :], writes=[self.Bc])
        fw.dma(fw.sp, self.rowp, self.rowp_d.partition_broadcast(128), writes=[self.Bc])
        fw.dma(fw.sp, self.wr, self.router_w.rearrange("(c p) e -> p c e", p=128), writes=[self.Bc])
        fw.dma(fw.pool, self.pwb, self.pool_w.rearrange("g c d -> c g d"), writes=[self.Bc])
        self.dve(lambda: nc.vector.tensor_copy(self.cstb[:, 0:640], self.cstf[:, 0:640]), [self.Bc], [self.Bc])
        self.dve(lambda: nc.vector.memset(self.cstb[:, 640:768], 1.0), [], [self.Bc])
        self.dve(lambda: nc.vector.tensor_scalar(self.cstb[:, 768:896], self.cstf[:, 128:256], -8.0, None, ALU.mult), [self.Bc], [self.Bc])
        self.ident_f = self.cstf[:, 0:128]
        self.ident_b = self.cstb[:, 0:128]
        self.tri_b = self.cstb[:, 128:256]
        self.mle_b = self.cstb[:, 256:384]
        self.mlt_b = self.cstb[:, 384:512]
        self.pm_b = self.cstb[:, 512:640]
        self.ones_b = self.cstb[:, 640:768]
        self.ntri_b = self.cstb[:, 768:896]
        self.invc = self.cstf[:, 640:704].rearrange("p (g t) -> p g t", g=4)
        self.cw = self.pp[:, 0:124].rearrange("p (c w) -> p c w", c=4)
        self.conv_b = self.pp[:, 124:128]
        self.cln_g = self.pp[:, 128:132]
        self.cln_b = self.pp[:, 132:136]
        self.subln = self.pp[:, 136:137]
        self.pscale = self.pp[:, 137:141]
        sm = self.small
        lam4 = self.rowp[:, 0:256].rearrange("p (a d) -> p a d", a=4)
        tmp, tmpb_ = self.tmpf.next()
        self.dve(lambda: nc.vector.tensor_tensor(tmp[:, 0:128].rearrange("p (a d) -> p a d", a=2), lam4[:, 0:2, :], lam4[:, 2:4, :], ALU.mult), [self.Bc], [tmpb_])
        self.dve(lambda: nc.vector.tensor_reduce(sm[:, 2:4], tmp[:, 0:128].rearrange("p (a d) -> p a d", a=2), AX.X, ALU.add), [tmpb_], [self.Bsmall])
        self.act(sm[:, 4:6], sm[:, 2:4], AF.Exp, [self.Bsmall], [self.Bsmall])
        self.dve(lambda: nc.vector.tensor_tensor(sm[:, 6:7], sm[:, 5:6], sm[:, 4:5], ALU.subtract), [self.Bsmall], [self.Bsmall])
        self.dve(lambda: nc.vector.tensor_scalar(sm[:, 0:1], sm[:, 6:7], -LAM_INIT0, None, ALU.add), [self.Bsmall], [self.Bsmall])
        self.dve(lambda: nc.vector.tensor_scalar(sm[:, 1:2], self.subln, 1.0 - LAM_INIT0, None, ALU.mult), [self.Bc], [self.Bsmall])
        self.neglam = sm[:, 0:1]
        self.gsc = sm[:, 1:2]
        self.rbias = self.rowp[:, 256:272]

    def _xt_from_dram(self):
        nc, fw = self.nc, self.fw
        psr = self.psring([0, 1, 2, 3])
        k = 0
        for tt in range(NT):
            half = tt % 2
            for hh in range(2):
                pass
            xt_ap = self.SCR[half][:, 0:1024]
            fw.dma(fw.sp, xt_ap, self.x_d[tt * 128:(tt + 1) * 128, :], writes=[self.SCRb[half]])
            for dg in range(2):
                ps, psb = psr.next()
                for j in range(4):
                    dc = dg * 4 + j
                    self.tr(ps[:, j * 128:(j + 1) * 128], xt_ap[:, dc * 128:(dc + 1) * 128], self.ident_f,
                            [self.SCRb[half], self.Bc], [psb], inc=(j == 3))
                dst = self.XT[:, dg * 4:(dg + 1) * 4, tt * 128:(tt + 1) * 128]
                src = ps.rearrange("p (c t) -> p c t", c=4)
                if k % 2 == 0:
                    self.act(dst, src, AF.Copy, [psb], [self.XTb[tt]])
                else:
                    self.dve(lambda: nc.vector.tensor_copy(dst, src), [psb], [self.XTb[tt]])
                k += 1

    def _xt_from_x(self, router, spill, inv=1.0):
        nc, fw = self.nc, self.fw
        psr = self.psring([0, 1])
        lg_ps, lg_b = self.PS[2], self.PSb[2]
        if spill:
            for tt in range(NT):
                fw.dma(fw.sp, self.spill_d[tt * 128:(tt + 1) * 128, :], self.X[:, tt, :], reads=[self.Xb[tt]])
        for tb in range(4):
            for dc in range(8):
                ps, psb = psr.next()
                for j in range(4):
                    tt = tb * 4 + j
                    self.tr(ps[:, j * 128:(j + 1) * 128], self.X[:, tt, dc * 128:(dc + 1) * 128], self.ident_f,
                            [self.Xb[tt], self.Bc], [psb], inc=(j == 3))
                dst = self.XT[:, dc, tb * 512:(tb + 1) * 512]
                self.act(dst, ps, AF.Copy, [psb], self.XTb[tb * 4:tb * 4 + 4], scale=inv)
                if router:
                    xf, xfb = self.tmpf.next()
                    self.act(xf, ps, AF.Copy, [psb], [xfb], scale=inv)
                    self.mm(lg_ps[0:16, :], self.wr[:, dc, :], xf, dc == 0, dc == 7, [self.Bc, xfb], [lg_b])
            if router:
                self.dve(lambda: nc.vector.tensor_copy(self.rt_halves[tb // 2][:, (tb % 2) * 512:(tb % 2 + 1) * 512], lg_ps[0:16, :]), [lg_b], [self.Brt])

    @staticmethod
    def _win_slot(L):
        return {0: 0, 1: 1, 2: 2, 3: 3, 4: 4} if L == 0 else {3: 0, 0: 1, 1: 2, 2: 3}

    def _load_w_in(self, L, g, slot):
        fw = self.fw
        src = self.win[L].rearrange("(c p) n -> p c n", p=128)[:, :, g * 512:(g + 1) * 512]
        dst = self.Wblk[slot].rearrange("p (c n) -> p c n", c=8)
        fw.dma(fw.pool, dst, src, writes=[self.Wb[slot]])
        return dst

    def _proj_fm(self, W, wbuf, c, tb, ps, psb):
        for dc in range(8):
            self.mm(ps, W[:, dc, c * 128:(c + 1) * 128], self.XT[:, dc, tb * 512:(tb + 1) * 512], dc == 0, dc == 7,
                    [wbuf] + self.XTb[tb * 4:tb * 4 + 4], [psb])

    def _proj_v(self, W, wbuf):
        nc = self.nc
        psr = self.psring([0, 1, 2, 3])
        for tt in range(NT):
            ps, psb = psr.next()
            for dc in range(8):
                self.mm(ps, self.XT[:, dc, tt * 128:(tt + 1) * 128], W[:, dc, :], dc == 0, dc == 7, [wbuf, self.XTb[tt]], [psb])
            if tt % 2 == 0:
                self.act(self.V[:, tt, :], ps, AF.Copy, [psb], [self.Vb[tt]])
            else:
                self.dve(lambda: nc.vector.tensor_copy(self.V[:, tt, :], ps), [psb], [self.Vb[tt]])

    def _in_proj(self, L):
        nc, fw = self.nc, self.fw
        ng = 5 if L == 0 else 4
        pref = getattr(self, "pref_win", {}) if L == 1 else {}
        slot_of = self._win_slot(L)
        Ws = [pref[g] if g in pref else self._load_w_in(L, g, slot_of[g]) for g in range(ng)]
        Wbs = [self.Wb[slot_of[g]] for g in range(ng)]
        psr = self.psring([0, 1, 2, 3])
        if L == 0:
            fw.dma(fw.sp, self.SCR[0], self.rope_d[0], writes=[self.SCRb[0]])
            fw.dma(fw.sp, self.SCR[1], self.rope_d[1], writes=[self.SCRb[1]])
            for c in range(4):
                self.pool(lambda: nc.gpsimd.memset(self.AU[:, c, 0:32], 0.0), [], [self.AUb[c]])
            for c in range(4):
                for tb in range(4):
                    pv, pvb = psr.next()
                    pg, pgb = psr.next()
                    self._proj_fm(Ws[0], self.Wb[0], c, tb, pv, pvb)
                    self._proj_fm(Ws[1], self.Wb[1], c, tb, pg, pgb)
                    sg, sgb = self.tmpf.next()
                    self.act(sg, pg, AF.Sigmoid, [pgb], [sgb])
                    dst = self.AU[:, c, 32 + tb * 512:32 + (tb + 1) * 512]
                    self.dve(lambda: nc.vector.tensor_tensor(dst, pv, sg, ALU.mult), [pvb, sgb], [self.AUb[c]])
            if self.stop == "l0_glu":
                return
            self._conv_diag()
            pmr = self.psring([4, 5])
            pending = None

            import os
            KD = int(os.environ.get("KDBG", "0"))

            def rope_tail(item):
                c8, tb, p1, p1b, qb, qbb = item
                if KD == 1:
                    return
                if KD == 2:
                    p2, p2b = pmr.next()
                    self.mm(p2, self.pm_b, qb, True, True, [self.Bc, qbb], [p2b])
                    return
                if KD == 3:
                    t1, t1b = self.tmpf.next()
                    sl = slice(tb * 512, (tb + 1) * 512)
                    self.dve(lambda: nc.vector.tensor_tensor(t1, p1, self.SCR[0][:, sl], ALU.mult), [p1b, self.SCRb[0]], [t1b])
                    return
                if KD == 6:
                    t1, t1b = self.tmpf.next()
                    self.dve(lambda: nc.vector.tensor_tensor(t1, self.cstf[:, 0:512], self.cstf[:, 0:512], ALU.mult), [self.Bc], [t1b])
                    return
                if KD == 7:
                    t1, t1b = self.tmpf.next()
                    self.dve(lambda: nc.vector.tensor_copy(t1, p1), [p1b], [t1b])
                    return
                if KD == 8:
                    t1, t1b = self.tmpf.next()
                    self.dve(lambda: nc.vector.tensor_tensor(qb, p1, self.cstf[:, 0:512], ALU.mult), [p1b, self.Bc], [qbb])
                    return
                if KD == 4:
                    t1, t1b = self.tmpf.next()
                    self.dve(lambda: nc.vector.tensor_tensor(t1, p1, self.cstf[:, 0:512], ALU.mult), [p1b, self.Bc], [t1b])
                    return
                if KD == 5:
                    t1, t1b = self.tmpf.next()
                    sl = slice(tb * 512, (tb + 1) * 512)
                    self.dve(lambda: nc.vector.tensor_tensor(t1, p1, self.SCR[0][:, sl], ALU.mult), [p1b, self.SCRb[0]], [t1b])
                    if c8 == 0 and tb == 1:
                        raise StopIteration
                    return
                p2, p2b = pmr.next()
                self.mm(p2, self.pm_b, qb, True, True, [self.Bc, qbb], [p2b])
                t1, t1b = self.tmpf.next()
                t2, t2b = self.tmpf.next()
                sl = slice(tb * 512, (tb + 1) * 512)
                self.dve(lambda: nc.vector.tensor_tensor(t1, p1, self.SCR[0][:, sl], ALU.mult), [p1b, self.SCRb[0]], [t1b])
                self.dve(lambda: nc.vector.tensor_tensor(t2, p2, self.SCR[1][:, sl], ALU.mult), [p2b, self.SCRb[1]], [t2b])
                self.dve(lambda: nc.vector.tensor_tensor(self.QK[:, c8, sl], t1, t2, ALU.add), [t1b, t2b], [self.QKb[c8][tb]])

            try:
                for g in (2, 3):
                    for c in range(4):
                        for tb in range(4):
                            p1, p1b = psr.next()
                            self._proj_fm(Ws[g], self.Wb[g], c, tb, p1, p1b)
                            qb, qbb = self.tmpb.next()
                            self.act(qb, p1, AF.Copy, [p1b], [qbb])
                            if pending is not None:
                                rope_tail(pending)
                            pending = ((g - 2) * 4 + c, tb, p1, p1b, qb, qbb)
                rope_tail(pending)
            except StopIteration:
                pass
            if self.stop == "l0_qk":
                return
            self._proj_v(Ws[4], self.Wb[4])
        else:
            for c in range(4):
                self.pool(lambda: nc.gpsimd.memset(self.AU[:, c, 0:16], 0.0), [], [self.AUb[c]])
            for c in range(4):
                for tb in range(4):
                    p1, p1b = psr.next()
                    self._proj_fm(Ws[3], Wbs[3], c, tb, p1, p1b)
                    self.act(self.AU[:, c, 16 + tb * 512:16 + (tb + 1) * 512], p1, AF.Copy, [p1b], [self.AUb[c]])
            self._pool_prefix()
            for g in (0, 1):
                for c in range(4):
                    for tb in range(4):
                        p1, p1b = psr.next()
                        self._proj_fm(Ws[g], Wbs[g], c, tb, p1, p1b)
                        self.act(self.QK[:, g * 4 + c, tb * 512:(tb + 1) * 512], p1, AF.Copy, [p1b], [self.QKb[g * 4 + c][tb]])
            self._proj_v(Ws[2], Wbs[2])

    def _conv_diag(self):
        nc = self.nc
        homes = [(self.Wblk[0], [self.Wb[0]]), (self.Wblk[1], [self.Wb[1]]), (self.Wblk[5], [self.Wb[5]]),
                 (self.ht, [self.htr.items[0][1], self.htr.items[1][1]])]
        self.Dg = []
        for c in range(4):
            home, bufs = homes[c]
            D3 = home[:, 0:31 * 128].rearrange("p (w j) -> p w j", w=31)
            self.dve(lambda: nc.vector.tensor_tensor(D3, self.ident_b.unsqueeze(1).broadcast_to([128, 31, 128]),
                                                     self.cw[:, c, :].unsqueeze(2).broadcast_to([128, 31, 128]), ALU.mult),
                     [self.Bc], bufs)
            self.Dg.append((D3, bufs))

    def _conv_pe(self):
        MIX = self.XT
        ps, psb = self.PS[3], self.PSb[3]
        for c in range(4):
            D3, dbufs = self.Dg[c]
            for tb in range(4):
                for w in range(31):
                    o = 2 + w + tb * 512
                    self.mm(ps, D3[:, w, :], self.AU[:, c, o:o + 512], w == 0, w == 30, dbufs + [self.AUb[c]], [psb])
                    yield
                self.act(MIX[:, c, tb * 512:(tb + 1) * 512], ps, AF.Identity, [psb, self.Bc], self.XTb[tb * 4:tb * 4 + 4],
                         bias=self.conv_b[:, c:c + 1])
                yield

    def _conv_ln(self):
        nc = self.nc
        MIX = self.XT
        sls = [slice(tb * 512, (tb + 1) * 512) for tb in range(4)]
        for tb in range(4):
            sl = sls[tb]
            xb4 = self.XTb[tb * 4:tb * 4 + 4]
            sum_ps, sum_b = self.PS[tb], self.PSb[tb]
            sq_ps, sq_b = self.PS[4 + tb], self.PSb[4 + tb]
            for c in range(4):
                sq, sqb = self.tmpb.next()
                self.act(sq, MIX[:, c, sl], AF.Square, xb4, [sqb])
                self.mm(sum_ps, self.ones_b, MIX[:, c, sl], c == 0, c == 3, [self.Bc] + xb4, [sum_b])
                self.mm(sq_ps, self.ones_b, sq, c == 0, c == 3, [self.Bc, sqb], [sq_b])
        for tb in range(4):
            sl = sls[tb]
            mean, var = self.SCR[0][:, sl], self.SCR[1][:, sl]
            self.act(mean, self.PS[tb], AF.Copy, [self.PSb[tb]], [self.SCRb[0]], scale=1.0 / 512)
            nmsq, nmsqb = self.tmpf.next()
            self.dve(lambda: nc.vector.scalar_tensor_tensor(nmsq, mean, -1.0, mean, ALU.mult, ALU.mult), [self.SCRb[0]], [nmsqb])
            self.dve(lambda: nc.vector.scalar_tensor_tensor(var, self.PS[4 + tb], 1.0 / 512, nmsq, ALU.mult, ALU.add), [self.PSb[4 + tb], nmsqb], [self.SCRb[1]])
            self.dve(lambda: nc.vector.tensor_scalar(var, var, LN_EPS, None, ALU.add), [self.SCRb[1]], [self.SCRb[1]])
            self.act(var, var, AF.Ln, [self.SCRb[1]], [self.SCRb[1]])
            self.act(var, var, AF.Exp, [self.SCRb[1]], [self.SCRb[1]], scale=-0.5)
        for tb in range(4):
            sl = sls[tb]
            xb4 = self.XTb[tb * 4:tb * 4 + 4]
            mean, var = self.SCR[0][:, sl], self.SCR[1][:, sl]
            for c in range(4):
                d, db = self.tmpf.next()
                self.dve(lambda: nc.vector.tensor_tensor(d, MIX[:, c, sl], mean, ALU.subtract), xb4 + [self.SCRb[0]], [db])
                self.dve(lambda: nc.vector.tensor_tensor(d, d, var, ALU.mult), [db, self.SCRb[1]], [db])
                self.act(MIX[:, c, sl], d, AF.Silu, [db, self.Bc], xb4, scale=self.cln_g[:, c:c + 1], bias=self.cln_b[:, c:c + 1])

    def _conv_branch(self):
        for _ in self._conv_pe():
            pass
        self._conv_ln()

    def _diff_attn(self, side=None):
        nc = self.nc
        MIX = self.XT
        QK, V = self.QK, self.V
        scr = self.psring([0, 1, 2])
        O = [(self.PS[4], self.PSb[4]), (self.PS[6], self.PSb[6])]

        def pull(n):
            if side is not None:
                for _ in range(n):
                    next(side, None)
        Dn = [(self.PS[5], self.PSb[5]), (self.PS[7], self.PSb[7])]
        deferred = []
        for h in range(4):
            for qb in range(4):
                nkt = 4 * (qb + 1)
                q0 = qb * 512
                steps = [(kt, j) for kt in range(nkt) for j in range(2)]

                def stage_a(kt, j):
                    i = kt - 4 * qb
                    c0 = 128 * i if i > 0 else 0
                    sp_, spb = scr.next()
                    pr = slice(64 * j, 64 * j + 64)
                    self.mm(sp_[:, c0:512], QK[pr, 4 + h, kt * 128:(kt + 1) * 128], QK[pr, h, q0 + c0:q0 + 512], True, True,
                            [self.QKb[4 + h][kt // 4], self.QKb[h][qb]], [spb])
                    pt, ptb = self.tmpb.next()
                    self.act(pt[:, c0:512], sp_[:, c0:512], AF.Exp, [spb], [ptb], scale=0.125)
                    if i >= 0:
                        self.pool(lambda: nc.gpsimd.tensor_tensor(pt[:, c0:c0 + 128], pt[:, c0:c0 + 128], self.mle_b, ALU.mult), [ptb, self.Bc], [ptb])
                    return (kt, j, c0, pt, ptb)

                def stage_b(kt, j, c0, pt, ptb):
                    self.mm(O[j][0][:, c0:512], V[:, kt, h * 128:(h + 1) * 128], pt[:, c0:512], kt == 0, kt == nkt - 1,
                            [self.Vb[kt], ptb], [O[j][1]])
                    self.mm(Dn[j][0][:, c0:512], self.ones_b, pt[:, c0:512], kt == 0, kt == nkt - 1, [self.Bc, ptb], [Dn[j][1]])

                pend = []
                for si, (kt, j) in enumerate(steps):
                    pend.append(stage_a(kt, j))
                    if len(pend) > 2:
                        stage_b(*pend.pop(0))
                    pull(1)
                    while deferred and deferred[0][0] <= si:
                        deferred.pop(0)[1]()
                while pend:
                    stage_b(*pend.pop(0))
                while deferred:
                    deferred.pop(0)[1]()
                os_ = []
                for j in range(2):
                    r, rb = self.tmpf.next()
                    self.act(r, Dn[j][0], AF.Ln, [Dn[j][1]], [rb])
                    self.act(r, r, AF.Exp, [rb], [rb], scale=-1.0)
                    o, ob = self.tmpf.next()
                    self.dve(lambda: nc.vector.tensor_tensor(o, O[j][0], r, ALU.mult), [O[j][1], rb], [ob])
                    os_.append((o, ob))
                (o1, o1b), (o2, o2b) = os_
                self.dve(lambda: nc.vector.scalar_tensor_tensor(o1, o2, self.neglam, o1, ALU.mult, ALU.add), [o2b, o1b, self.Bsmall], [o1b])
                st = {}

                def fin_a(o1=o1, o1b=o1b, st=st):
                    st["osq"], st["osqb"] = self.tmpb.next()
                    self.act(st["osq"], o1, AF.Square, [o1b], [st["osqb"]])

                def fin_b(st=st):
                    ss, ssb = scr.next()
                    self.mm(ss, self.ones_b, st["osq"], True, True, [self.Bc, st["osqb"]], [ssb])
                    st["rs"], st["rsb"] = self.tmpf.next()
                    rs = st["rs"]
                    self.dve(lambda: nc.vector.tensor_scalar(rs, ss, 1.0 / 128, RMS_EPS, ALU.mult, ALU.add), [ssb], [st["rsb"]])

                def fin_c(o1=o1, o1b=o1b, st=st, h=h, q0=q0, qb=qb):
                    rs, rsb = st["rs"], st["rsb"]
                    self.act(rs, rs, AF.Ln, [rsb], [rsb])
                    self.act(rs, rs, AF.Exp, [rsb], [rsb], scale=-0.5)
                    self.dve(lambda: nc.vector.scalar_tensor_tensor(MIX[:, 4 + h, q0:q0 + 512], o1, self.gsc, rs, ALU.mult, ALU.mult),
                             [o1b, rsb, self.Bsmall], self.XTb[qb * 4:qb * 4 + 4])

                deferred = [(2, fin_a), (4, fin_b), (6, fin_c)]
                pull(12)
        while deferred:
            deferred.pop(0)[1]()
        if side is not None:
            for _ in side:
                pass

    def _pool_prefix(self):
        nc = self.nc
        for g in range(4):
            U = self.AU[:, g, :]
            bufs = [(self.SCR[0], self.SCRb[0]), (self.SCR[1], self.SCRb[1])]
            cur, curb = bufs[0]
            self.dve(lambda: nc.vector.tensor_tensor(cur, U[:, 16:16 + S], U[:, 15:15 + S], ALU.add), [self.AUb[g]], [curb])
            k = 0
            sh = 2
            while sh < POOL_WINDOWS[g]:
                nxt, nxtb = bufs[(k + 1) % 2]
                self.dve(lambda: nc.vector.tensor_tensor(nxt[:, sh:S], cur[:, sh:S], cur[:, 0:S - sh], ALU.add), [curb], [nxtb])
                self.pool(lambda: nc.gpsimd.tensor_copy(nxt[:, 0:sh], cur[:, 0:sh]), [curb], [nxtb])
                cur, curb = nxt, nxtb
                k += 1
                sh *= 2
            win = POOL_WINDOWS[g]
            t, tb_ = self.tmpf.next()
            self.dve(lambda: nc.vector.tensor_tensor(t[:, 0:16], cur[:, 0:16], self.invc[:, g, :], ALU.mult), [curb, self.Bc], [tb_])
            self.dve(lambda: nc.vector.tensor_tensor(t[:, 0:16], t[:, 0:16], U[:, 16:32], ALU.subtract), [tb_, self.AUb[g]], [tb_])
            for tb in range(4):
                sl = slice(tb * 512, (tb + 1) * 512)
                usl = U[:, 16 + tb * 512:16 + (tb + 1) * 512]
                self.dve(lambda: nc.vector.scalar_tensor_tensor(usl, cur[:, sl], 1.0 / win, usl, ALU.mult, ALU.subtract),
                         [curb, self.AUb[g]], [self.AUb[g]])
            self.dve(lambda: nc.vector.tensor_copy(U[:, 16:32], t[:, 0:16]), [tb_], [self.AUb[g]])

    def _pool_mix(self):
        nc = self.nc
        MIX = self.XT
        pr = self.psring([4, 5, 6, 7])
        for g in range(4):
            for tb in range(4):
                sl = slice(tb * 512, (tb + 1) * 512)
                pm_ps, pm_b = pr.next()
                self.mm(pm_ps, self.pwb[:, g, :], self.AU[:, g, 16 + tb * 512:16 + (tb + 1) * 512], True, True, [self.Bc, self.AUb[g]], [pm_b])
                self.dve(lambda: nc.vector.tensor_scalar(MIX[:, 4 + g, sl], pm_ps, self.pscale[:, g:g + 1], None, ALU.mult),
                         [pm_b, self.Bc], self.XTb[tb * 4:tb * 4 + 4])

    def _sb_attn(self):
        nc = self.nc
        MIX = self.XT
        QK, V = self.QK, self.V
        self.fw.barrier()
        tf, tb_ = self.tf_full, self.tb_full
        fpr = Ring([tf[:, i * 1024:(i + 1) * 1024] for i in range(4)], "tfp")
        spr = Ring([tb_[:, i * 1024:(i + 1) * 1024] for i in range(3)], "tbp")
        apr = Ring([tb_[:, 3072 + i * 1024:3072 + (i + 1) * 1024] for i in range(2)], "tap")
        rpr = Ring([self.SCR[i // 2][:, (i % 2) * 1024:(i % 2 + 1) * 1024] for i in range(4)], "trp")
        zpairs = [(self.PSpair[0], [self.PSb[0], self.PSb[1]]), (self.PSpair[1], [self.PSb[2], self.PSb[3]])]
        zi = [0]
        Rp, Rb = self.PSpair[2], [self.PSb[4], self.PSb[5]]
        Or = self.psring([6, 7])
        zeros = self.zeros_b
        self.pool(lambda: nc.gpsimd.memset(zeros, 0.0), [], [self.Bsmall])
        w = lambda ap, c0: ap.rearrange("p (h q) -> p h q", h=2)[:, :, c0:512]
        hv = lambda ap, hh: ap[:, hh * 512:(hh + 1) * 512]
        mask2 = self.mlt_b.unsqueeze(1).broadcast_to([128, 2, 128])

        steps = []
        for c in range(4):
            for qb in range(4):
                nkt = 4 * (qb + 1)
                for idx, kt in enumerate(range(nkt - 1, -1, -1)):
                    steps.append((c, qb, nkt, idx, kt))

        def a_pe(c, qb, nkt, idx, kt, o_ps, o_b):
            q0 = qb * 512
            i = kt - 4 * qb
            c0 = 128 * i if i > 0 else 0
            zp, zb = zpairs[zi[0] % 2]
            zi[0] += 1
            for hh in range(2):
                pr = slice(64 * hh, 64 * hh + 64)
                self.mm(hv(zp, hh)[:, c0:512], QK[pr, 4 + c, kt * 128:(kt + 1) * 128], QK[pr, c, q0 + c0:q0 + 512], True, True,
                        [self.QKb[4 + c][kt // 4], self.QKb[c][qb]], [zb[hh]])
            return dict(c=c, qb=qb, q0=q0, nkt=nkt, idx=idx, kt=kt, i=i, c0=c0, zp=zp, zb=zb, o_ps=o_ps, o_b=o_b)

        def a_act(st):
            c0 = st["c0"]
            e, eb = fpr.next()
            self.act(w(e, c0), w(st["zp"], c0), AF.Exp, st["zb"], [eb], scale=0.125)
            sp_, spb = spr.next()
            self.act(w(sp_, c0), w(e, c0), AF.Ln, [eb], [spb], bias=1.0)
            if st["i"] >= 0:
                blk = sp_.rearrange("p (h q) -> p h q", h=2)[:, :, c0:c0 + 128]
                self.pool(lambda: nc.gpsimd.tensor_tensor(blk, blk, mask2, ALU.mult), [spb, self.Bc], [spb])
            st["sp"], st["spb"] = sp_, spb

        def r_init(st):
            for hh in range(2):
                self.mm(hv(Rp, hh), zeros, QK[:, st["c"], st["q0"]:st["q0"] + 512], True, True,
                        [self.Bsmall, self.QKb[st["c"]][st["qb"]]], [Rb[hh]], inc=True)

        def b_copy(st):
            c0 = st["c0"]
            rsb_, rsbb = rpr.next()
            self.dve(lambda: nc.vector.tensor_copy(w(rsb_, c0), w(Rp, c0)), Rb, [rsbb])
            st["rsb"], st["rsbb"] = rsb_, rsbb

        def b_pe(st):
            c0 = st["c0"]
            for hh in range(2):
                sph = hv(st["sp"], hh)[:, c0:512]
                self.mm(hv(st["zp"], hh)[:, c0:512], self.ntri_b, sph, False, True, [self.Bc, st["spb"]], [st["zb"][hh]], inc=True)
            if st["idx"] < st["nkt"] - 1:
                for hh in range(2):
                    sph = hv(st["sp"], hh)[:, c0:512]
                    self.mm(hv(Rp, hh)[:, c0:512], self.ones_b, sph, False, True, [self.Bc, st["spb"]], [Rb[hh]], inc=True)

        def b_dve(st):
            c0 = st["c0"]
            t, tbf = fpr.next()
            self.dve(lambda: nc.vector.scalar_tensor_tensor(w(t, c0), w(st["zp"], c0), 0.125, w(st["rsb"], c0), ALU.mult, ALU.subtract),
                     st["zb"] + [st["rsbb"]], [tbf])
            st["t"], st["tbf"] = t, tbf

        def b_act(st):
            c0 = st["c0"]
            a, ab = apr.next()
            self.act(w(a, c0), w(st["t"], c0), AF.Exp, [st["tbf"]], [ab])
            if st["i"] >= 0:
                blk = a.rearrange("p (h q) -> p h q", h=2)[:, :, c0:c0 + 128]
                self.pool(lambda: nc.gpsimd.tensor_tensor(blk, blk, mask2, ALU.mult), [ab, self.Bc], [ab])
            st["a"], st["ab"] = a, ab
            return st

        def stage_c(st):
            c0, kt, idx, nkt = st["c0"], st["kt"], st["idx"], st["nkt"]
            o_ps, o_b = st["o_ps"], st["o_b"]
            for hh in range(2):
                pr = slice(64 * hh, 64 * hh + 64)
                h = 2 * st["c"] + hh
                self.mm(o_ps[pr, c0:512], V[:, kt, h * 64:(h + 1) * 64], hv(st["a"], hh)[:, c0:512], idx == 0, idx == nkt - 1,
                        [self.Vb[kt], st["ab"]], [o_b], inc=True)
            if idx == nkt - 1:
                qb = st["qb"]
                self.act(MIX[:, st["c"], st["q0"]:st["q0"] + 512], o_ps, AF.Copy, [o_b], self.XTb[qb * 4:qb * 4 + 4])

        pa = None
        pb = None
        pc = None
        obank = None
        for n in range(len(steps) + 3):
            cur = None
            if n < len(steps):
                c, qb, nkt, idx, kt = steps[n]
                if idx == 0:
                    obank = Or.next()
                cur = a_pe(c, qb, nkt, idx, kt, obank[0], obank[1])
            if pa is not None:
                b_pe(pa)
                b_dve(pa)
            if cur is not None:
                if cur["idx"] == 0:
                    r_init(cur)
                b_copy(cur)
            if pc is not None:
                stage_c(pc)
            if cur is not None:
                a_act(cur)
            nc_ = b_act(pb) if pb is not None else None
            pa, pb, pc = cur, pa, nc_

    def _load_ln(self, idx, scale=None):
        nc, fw = self.nc, self.fw
        G = self.SCR[0][:, 0:1024]
        B_ = self.SCR[1][:, 0:1024]
        fw.dma(fw.sp, G, self.lnp_d[2 * idx].partition_broadcast(128), writes=[self.SCRb[0]])
        fw.dma(fw.sp, B_, self.lnp_d[2 * idx + 1].partition_broadcast(128), writes=[self.SCRb[1]])
        if scale is not None:
            self.act(G, G, AF.Copy, [self.SCRb[0]], [self.SCRb[0]], scale=scale)
            self.act(B_, B_, AF.Copy, [self.SCRb[1]], [self.SCRb[1]], scale=scale)
        return G, B_

    def _ln_stats(self, tt):
        nc = self.nc
        xs = self.X[:, tt, :]
        st_, stb = self.statr.next()
        xb = [self.Xb[tt]]
        self.dve(lambda: nc.vector.bn_stats(st_[:, 0:6], xs[:, 0:512]), xb, [stb])
        self.dve(lambda: nc.vector.bn_stats(st_[:, 6:12], xs[:, 512:1024]), xb, [stb])
        self.dve(lambda: nc.vector.bn_aggr(st_[:, 12:14], st_[:, 0:12]), [stb], [stb])
        self.dve(lambda: nc.vector.tensor_scalar(st_[:, 14:15], st_[:, 13:14], LN_EPS, None, ALU.add), [stb], [stb])
        self.act(st_[:, 14:15], st_[:, 14:15], AF.Ln, [stb], [stb])
        self.act(st_[:, 14:15], st_[:, 14:15], AF.Exp, [stb], [stb], scale=-0.5)
        return (tt, st_, stb)

    def _ln_apply(self, tt, st_, stb, G, B_):
        nc = self.nc
        xs = self.X[:, tt, :]
        xb = [self.Xb[tt]]
        self.dve(lambda: nc.vector.scalar_tensor_tensor(xs, xs, st_[:, 12:13], G, ALU.subtract, ALU.mult), xb + [stb, self.SCRb[0]], xb)
        self.dve(lambda: nc.vector.scalar_tensor_tensor(xs, xs, st_[:, 14:15], B_, ALU.mult, ALU.add), xb + [stb, self.SCRb[1]], xb)

    def _ln_tile(self, tt, G, B_):
        self._ln_apply(*self._ln_stats(tt), G, B_)

    def _load_wout(self, L):
        fw = self.fw
        for i in range(2):
            src = self.wout[L].rearrange("(c p) n -> p c n", p=128)[:, i * 4:(i + 1) * 4, :]
            dst = self.Wblk[3 + i].rearrange("p (c n) -> p c n", c=4)
            fw.dma(fw.pool, dst, src, writes=[self.Wb[3 + i]])

    def _mid(self, L):
        nc, fw = self.nc, self.fw
        MIX = self.XT
        inv = 1.0 / ALPHA
        Wo = [self.Wblk[3].rearrange("p (c n) -> p c n", c=4), self.Wblk[4].rearrange("p (c n) -> p c n", c=4)]
        W0 = self._expert_load(L, 0)
        G, B_ = self._load_ln(2 * L, scale=ALPHA)
        xsrc = self.x_d if L == 0 else self.spill_d
        for tt in range(NT):
            fw.dma(fw.sp, self.X[:, tt, :], xsrc[tt * 128:(tt + 1) * 128, :], writes=[self.Xb[tt]])
        psr = self.psring([5, 6])
        tpr = self.psring([0, 1])
        RB = [(self.PS[i], self.PSb[i]) for i in (2, 3, 4, 7)]

        def op_tile(tt):
            for half in range(2):
                ps, psb = psr.next()
                for fc in range(8):
                    self.mm(ps, MIX[:, fc, tt * 128:(tt + 1) * 128], Wo[fc // 4][:, fc % 4, half * 512:(half + 1) * 512], fc == 0, fc == 7,
                            [self.XTb[tt], self.Wb[3 + fc // 4]], [psb])
                xs = self.X[:, tt, half * 512:(half + 1) * 512]
                self.dve(lambda: nc.vector.scalar_tensor_tensor(xs, xs, ALPHA, ps, ALU.mult, ALU.add), [self.Xb[tt], psb], [self.Xb[tt]])
            st = self._ln_stats(tt)
            ln_flush()
            ln_pend.append(st)

        ln_pend = []

        def ln_flush():
            while ln_pend:
                self._ln_apply(*ln_pend.pop(0), G, B_)

        def xt_T(tb, dc):
            ps, psb = tpr.next()
            for j in range(4):
                tt = tb * 4 + j
                self.tr(ps[:, j * 128:(j + 1) * 128], self.X[:, tt, dc * 128:(dc + 1) * 128], self.ident_f,
                        [self.Xb[tt], self.Bc], [psb], inc=(j == 3))
            xf, xfb = self.tmpf.next()
            self.act(xf, ps, AF.Copy, [psb], [xfb], scale=inv)
            self.act(self.XT[:, dc, tb * 512:(tb + 1) * 512], ps, AF.Copy, [psb], self.XTb[tb * 4:tb * 4 + 4], scale=inv)
            return (tb, dc, xf, xfb)

        def xt_R(tb, dc, xf, xfb):
            for j in range(4):
                self.mm(RB[j][0][:, 0:NE], xf[:, j * 128:(j + 1) * 128], self.wr[:, dc, :], dc == 0, dc == 7, [self.Bc, xfb], [RB[j][1]])
            if dc == 7:
                for j in range(4):
                    tt = tb * 4 + j
                    self.act(self.lgt[:, tt * NE:(tt + 1) * NE], RB[j][0][:, 0:NE], AF.Copy, [RB[j][1]], [self.Brt])

        pend = []

        def xt_step(tb, dc):
            pend.append(xt_T(tb, dc))
            if len(pend) > 1:
                xt_R(*pend.pop(0))

        for tt in range(4):
            op_tile(tt)
        for tb in range(4):
            for j in range(4):
                if tb + 1 < 4:
                    op_tile((tb + 1) * 4 + j)
                else:
                    ln_flush()
                xt_step(tb, 2 * j)
                xt_step(tb, 2 * j + 1)
        while pend:
            xt_R(*pend.pop(0))
        return W0

    def _routing(self):
        nc = self.nc
        def wt():
            t, tb_ = self.tmpf.next()
            return t[:, 0:256], tb_
        aff, affb = wt()
        self.act(aff, self.lgt, AF.Sigmoid, [self.Brt], [affb])
        sel, selb = wt()
        v3 = lambda a: a.rearrange("p (t e) -> p t e", t=NT)
        v4 = lambda a: a.rearrange("p (t g j) -> p t g j", t=NT, g=4)
        g3 = lambda a: a.rearrange("p (t g) -> p t g", t=NT)
        self.dve(lambda: nc.vector.tensor_tensor(v3(sel), v3(aff), self.rbias.unsqueeze(1).broadcast_to([128, NT, NE]), ALU.add), [affb, self.Bc], [selb])
        sm = self.small
        m1 = sm[:, 8:8 + 64] if False else None
        m1t, m1b = self.tmpf.next()
        m1 = m1t[:, 0:64]
        m2 = m1t[:, 64:128]
        gs = m1t[:, 128:192]
        gmax = m1t[:, 192:208]
        gmask = m1t[:, 208:272]
        gsum = m1t[:, 272:288]
        self.dve(lambda: nc.vector.tensor_reduce(m1, sel.rearrange("p (tg j) -> p tg j", j=4), AX.X, ALU.max), [selb], [m1b])
        eq, eqb = wt()
        m1bc = m1.unsqueeze(2).broadcast_to([128, 64, 4])
        self.dve(lambda: nc.vector.tensor_tensor(eq.rearrange("p (tg j) -> p tg j", j=4), sel.rearrange("p (tg j) -> p tg j", j=4), m1bc, ALU.is_equal), [selb, m1b], [eqb])
        self.dve(lambda: nc.vector.scalar_tensor_tensor(eq, eq, -1.0e9, sel, ALU.mult, ALU.add), [eqb, selb], [eqb])
        self.dve(lambda: nc.vector.tensor_reduce(m2, eq.rearrange("p (tg j) -> p tg j", j=4), AX.X, ALU.max), [eqb], [m1b])
        self.dve(lambda: nc.vector.tensor_tensor(gs, m1, m2, ALU.add), [m1b], [m1b])
        self.dve(lambda: nc.vector.tensor_reduce(gmax, g3(gs), AX.X, ALU.max), [m1b], [m1b])
        self.dve(lambda: nc.vector.tensor_tensor(g3(gmask), g3(gs), gmax.unsqueeze(2).broadcast_to([128, NT, 4]), ALU.is_equal), [m1b], [m1b])
        ge, geb = wt()
        m2bc = m2.unsqueeze(2).broadcast_to([128, 64, 4])
        gmbc = gmask.unsqueeze(2).broadcast_to([128, 64, 4])
        j4 = lambda a: a.rearrange("p (tg j) -> p tg j", j=4)
        self.dve(lambda: nc.vector.tensor_tensor(j4(ge), j4(sel), m2bc, ALU.is_ge), [selb, m1b], [geb])
        self.dve(lambda: nc.vector.tensor_tensor(j4(ge), j4(ge), gmbc, ALU.mult), [geb, m1b], [geb])
        self.dve(lambda: nc.vector.tensor_tensor(ge, ge, aff, ALU.mult), [geb, affb], [geb])
        self.dve(lambda: nc.vector.tensor_reduce(gsum, v3(ge), AX.X, ALU.add), [geb], [m1b])
        self.dve(lambda: nc.vector.reciprocal(gsum, gsum), [m1b], [m1b])
        self.dve(lambda: nc.vector.tensor_tensor(v3(self.comb), v3(ge), gsum.unsqueeze(2).broadcast_to([128, NT, NE]), ALU.mult), [geb, m1b], [self.Bcomb])

    def _expert_load(self, L, e):
        fw = self.fw
        s = e % 2
        Wg = self.Wblk[3 * s].rearrange("p (c n) -> p c n", c=8)
        Wu = self.Wblk[3 * s + 1].rearrange("p (c n) -> p c n", c=8)
        Wd = self.Wblk[3 * s + 2].rearrange("p (c n) -> p c n", c=4)
        fw.dma(fw.pool, Wg, self.wg[L][e].rearrange("(c p) n -> p c n", p=128), writes=[self.Wb[3 * s]])
        fw.dma(fw.pool, Wu, self.wu[L][e].rearrange("(c p) n -> p c n", p=128), writes=[self.Wb[3 * s + 1]])
        fw.dma(fw.pool, Wd, self.wd[L][e].rearrange("(c p) n -> p c n", p=128), writes=[self.Wb[3 * s + 2]])
        return (Wg, Wu, Wd, s)

    def _experts(self, L, W0):
        nc, fw = self.nc, self.fw
        gr = self.psring([0, 1])
        ur = self.psring([2, 3])
        yr = self.psring([4, 5, 6, 7])
        comb3 = self.comb.rearrange("p (t e) -> p t e", t=NT)

        def load(e):
            s = e % 2
            Wg = self.Wblk[3 * s].rearrange("p (c n) -> p c n", c=8)
            Wu = self.Wblk[3 * s + 1].rearrange("p (c n) -> p c n", c=8)
            Wd = self.Wblk[3 * s + 2].rearrange("p (c n) -> p c n", c=4)
            fw.dma(fw.pool, Wg, self.wg[L][e].rearrange("(c p) n -> p c n", p=128), writes=[self.Wb[3 * s]])
            fw.dma(fw.pool, Wu, self.wu[L][e].rearrange("(c p) n -> p c n", p=128), writes=[self.Wb[3 * s + 1]])
            fw.dma(fw.pool, Wd, self.wd[L][e].rearrange("(c p) n -> p c n", p=128), writes=[self.Wb[3 * s + 2]])
            return (Wg, Wu, Wd, s)

        G_, B_ = self._load_ln(2 * L + 1)

        lnp = []

        def ln_fin():
            while lnp:
                st = lnp.pop(0)
                self._ln_apply(*st, G_, B_)
                if L == 1:
                    tt = st[0]
                    fw.dma(fw.sp, self.out_d[tt * 128:(tt + 1) * 128, :], self.X[:, tt, :], reads=[self.Xb[tt]])

        def ln_out(tt):
            st = self._ln_stats(tt)
            ln_fin()
            lnp.append(st)

        def stage_gu(e, tb, W, ln_tiles=()):
            Wg, Wu, Wd, s = W
            H, Hb = self.htr.next()
            xb4 = self.XTb[tb * 4:tb * 4 + 4]
            for fc in range(4):
                g, gb = gr.next()
                u, ub = ur.next()
                for dc in range(8):
                    self.mm(g, Wg[:, dc, fc * 128:(fc + 1) * 128], self.XT[:, dc, tb * 512:(tb + 1) * 512], dc == 0, dc == 7, [self.Wb[3 * s]] + xb4, [gb])
                for dc in range(8):
                    self.mm(u, Wu[:, dc, fc * 128:(fc + 1) * 128], self.XT[:, dc, tb * 512:(tb + 1) * 512], dc == 0, dc == 7, [self.Wb[3 * s + 1]] + xb4, [ub])
                sg, sgb = self.tmpf.next()
                self.act(sg, g, AF.Silu, [gb], [sgb])
                self.dve(lambda: nc.vector.tensor_tensor(H[:, fc, :], u, sg, ALU.mult), [ub, sgb], [Hb])
                if fc < len(ln_tiles):
                    ln_out(ln_tiles[fc])
            return (e, tb, W, H, Hb)

        def stage_dn(e, tb, W, H, Hb):
            Wg, Wu, Wd, s = W
            for t4 in range(4):
                tt = tb * 4 + t4
                for half in range(2):
                    y, yb = yr.next()
                    for fc in range(4):
                        self.mm(y, H[:, fc, t4 * 128:(t4 + 1) * 128], Wd[:, fc, half * 512:(half + 1) * 512], fc == 0, fc == 3, [Hb, self.Wb[3 * s + 2]], [yb])
                    xs = self.X[:, tt, half * 512:(half + 1) * 512]
                    self.dve(lambda: nc.vector.scalar_tensor_tensor(xs, y, comb3[:, tt, e:e + 1], xs, ALU.mult, ALU.add), [yb, self.Bcomb, self.Xb[tt]], [self.Xb[tt]])

        Wn = W0
        prev = None
        for e in range(NE):
            W = Wn
            for tb in range(4):
                if e == NE - 1:
                    stage_dn(*prev)
                    done = list(range((tb - 1) * 4, tb * 4)) if tb > 0 else []
                    cur = stage_gu(e, tb, W, done)
                else:
                    cur = stage_gu(e, tb, W)
                    if prev is not None:
                        stage_dn(*prev)
                prev = cur
                if tb == 0 and e + 1 < NE:
                    Wn = self._expert_load(L, e + 1)
                if tb == 0 and e + 1 == NE and L == 0:
                    self.pref_win = {g: self._load_w_in(1, g, self._win_slot(1)[g]) for g in (3, 0, 1)}
        stage_dn(*prev)
        for tt in range(12, 16):
            ln_out(tt)
        ln_fin()

    def _final_ln(self, L):
        pass


def _constants():
    j = np.arange(128)[:, None]
    k = np.arange(128)[None, :]
    ident = (j == k).astype(np.float32)
    tri = (j >= k).astype(np.float32)
    mle = (j <= k).astype(np.float32)
    mlt = (j < k).astype(np.float32)
    m = np.arange(128)
    perm = np.where((m % 64) < 32, m + 32, m - 32)
    pm = np.zeros((128, 128), np.float32)
    pm[perm, m] = 1.0
    invc = np.zeros((128, 4, 16), np.float32)
    for g, w in enumerate(POOL_WINDOWS):
        invc[:, g, :] = 1.0 / np.minimum(np.arange(16) + 1, w)
    cst = np.concatenate([ident, tri, mle, mlt, pm, invc.reshape(128, 64)], axis=1).astype(np.float32)
    inv = (10000.0 ** (-np.arange(0, 64, 2, dtype=np.float32) / 64)).astype(np.float32)
    ang = np.arange(S, dtype=np.float32)[:, None] * inv[None, :]
    ang = np.concatenate([ang, ang], axis=-1)
    cos = np.cos(ang).astype(np.float32).T
    sin = np.sin(ang).astype(np.float32).T
    sign = np.where(np.arange(64) < 32, -1.0, 1.0).astype(np.float32)[:, None]
    sins = sin * sign
    rope = np.stack([np.concatenate([cos, cos], 0), np.concatenate([sins, sins], 0)], 0).astype(np.float32)
    return cst, np.ascontiguousarray(rope)


def _pack_inputs(inp):
    f = lambda a: np.ascontiguousarray(np.asarray(a, dtype=np.float32))
    cst, rope = _constants()
    pp = np.zeros((128, 144), np.float32)
    pp[:, 0:124] = f(inp["l0_conv_w"]).T.reshape(4, 128, 31).transpose(1, 0, 2).reshape(128, 124)
    v4 = lambda a: f(a).reshape(4, 128).T
    pp[:, 124:128] = v4(inp["l0_conv_b"])
    pp[:, 128:132] = v4(inp["l0_conv_ln_g"])
    pp[:, 132:136] = v4(inp["l0_conv_ln_b"])
    pp[:, 136] = f(inp["l0_subln_g"])
    pp[:, 137:141] = v4(inp["l1_pool_scale"])
    rowp = np.concatenate([f(inp["l0_lambda_q1"]), f(inp["l0_lambda_q2"]), f(inp["l0_lambda_k1"]), f(inp["l0_lambda_k2"]),
                           f(inp["router_bias"])]).astype(np.float32)
    lnp = np.stack([f(inp["l0_ln_mix_g"]), f(inp["l0_ln_mix_b"]), f(inp["l0_ln_ffn_g"]), f(inp["l0_ln_ffn_b"]),
                    f(inp["l1_ln_mix_g"]), f(inp["l1_ln_mix_b"]), f(inp["l1_ln_ffn_g"]), f(inp["l1_ln_ffn_b"])], 0)
    shared = {
        "router_w": f(inp["router_w"]),
        "w_in0": f(inp["l0_in_proj"]), "w_in1": f(inp["l1_in_proj"]),
        "w_out0": f(inp["l0_out_proj"]), "w_out1": f(inp["l1_out_proj"]),
        "w_gate0": f(inp["l0_w_gate"]), "w_gate1": f(inp["l1_w_gate"]),
        "w_up0": f(inp["l0_w_up"]), "w_up1": f(inp["l1_w_up"]),
        "w_down0": f(inp["l0_w_down"]), "w_down1": f(inp["l1_w_down"]),
        "pool_w": f(inp["l1_pool_w"]),
        "pp": pp, "rowp": rowp, "lnp": np.ascontiguousarray(lnp), "cst": cst, "rope": rope,
    }
    x = f(inp["x"])
    return [dict(shared, x=np.ascontiguousarray(x[c])) for c in range(x.shape[0])]


def kernel(**inputs):
    in_maps = _pack_inputs(inputs)
    nc = Prog().build()
    res = run_bass_kernel_spmd(nc, in_maps, core_ids=list(range(8)))
    return np.stack([np.asarray(r["out"], dtype=np.float32) for r in res.results], axis=0)
```
